# Optimizing a Trainium2 kernel written in Bass

```python
import math
import jax, jax.numpy as jnp
from jax import lax
import numpy as np

D_MODEL = 1024
BATCH = 16
SEQ = 4096
DEPTH = 4

A_WIDTH = D_MODEL
A_GROUPS = 8
A_GROUP_DIM = A_WIDTH // A_GROUPS
A_CHUNK = 128
B_WIDTH = D_MODEL
B_HEADS = 4
B_HEAD_DIM = B_WIDTH // B_HEADS
B_CONV = 4
MLSTM_CHUNK = 128
EVEN_IN = 3 * A_WIDTH + 3 * B_WIDTH + 2 * B_HEADS
EVEN_OUT = A_WIDTH + B_WIDTH
EVEN_SPLITS = (A_WIDTH, 2 * A_WIDTH, 3 * A_WIDTH, 3 * A_WIDTH + B_WIDTH,
               3 * A_WIDTH + 2 * B_WIDTH, 3 * A_WIDTH + 2 * B_WIDTH + B_HEADS,
               3 * A_WIDTH + 2 * B_WIDTH + 2 * B_HEADS)
C_WIDTH = 2 * D_MODEL
C_HEADS = 16
C_HEAD_DIM = C_WIDTH // (2 * C_HEADS)
ODD_IN = 4 * C_WIDTH
Q_BLOCK = 128
ROPE_THETA = 10000.0
NORM_EPS = 1e-6

kernel_name = "hybrid_gmlp_mlstm_diffattn"


def rmsnorm(x, g):
    xf = x.astype(jnp.float32)
    y = xf * lax.rsqrt(jnp.mean(xf * xf, axis=-1, keepdims=True) + NORM_EPS)
    return (y * g.astype(jnp.float32)).astype(x.dtype)


def layernorm(x, g):
    xf = x.astype(jnp.float32)
    xc = xf - jnp.mean(xf, axis=-1, keepdims=True)
    y = xc * lax.rsqrt(jnp.mean(xc * xc, axis=-1, keepdims=True) + NORM_EPS)
    return (y * g.astype(jnp.float32)).astype(x.dtype)


def causal_depthwise_conv(x, w, b):
    k = w.shape[0]
    y = lax.conv_general_dilated(x, w[:, None, :].astype(x.dtype), window_strides=(1,),
                                 padding=((k - 1, 0),), dimension_numbers=("NWC", "WIO", "NWC"),
                                 feature_group_count=x.shape[-1])
    return y + b


def rope_tables(seq, dim):
    inv = ROPE_THETA ** (-jnp.arange(0, dim, 2, dtype=jnp.float32) / dim)
    ang = jnp.arange(seq, dtype=jnp.float32)[:, None] * inv[None, :]
    return jnp.cos(ang), jnp.sin(ang)


def apply_rope(x, cos, sin):
    shape = (x.shape[1],) + (1,) * (x.ndim - 3) + (x.shape[-1] // 2,)
    c, s = cos.reshape(shape), sin.reshape(shape)
    x1, x2 = jnp.split(x.astype(jnp.float32), 2, axis=-1)
    return jnp.concatenate([x1 * c - x2 * s, x1 * s + x2 * c], axis=-1).astype(x.dtype)


def mlstm_chunkwise(q, k, v, i_pre, f_pre):
    bsz, nh, seq, dk = q.shape
    dv = v.shape[-1]
    L = MLSTM_CHUNK
    nc = seq // L

    def chunks(t):
        t = t.astype(jnp.float32)
        return jnp.moveaxis(t.reshape((bsz, nh, nc, L) + t.shape[3:]), 2, 0)

    logf = jax.nn.log_sigmoid(f_pre.astype(jnp.float32))
    causal = jnp.tril(jnp.ones((L, L), dtype=bool))

    def body(carry, xs):
        C, n, m = carry
        qc, kc, vc, ic, lfc = xs
        b = jnp.cumsum(lfc, axis=-1)
        a = b + m[..., None]
        D = jnp.where(causal, b[..., :, None] - b[..., None, :] + ic[..., None, :], -jnp.inf)
        m_t = jnp.maximum(a, jnp.max(D, axis=-1))
        sc = jnp.einsum('bhtd,bhsd->bhts', qc, kc) * jnp.exp(D - m_t[..., None])
        w_inter = jnp.exp(a - m_t)
        num = jnp.einsum('bhts,bhse->bhte', sc, vc) + w_inter[..., None] * jnp.einsum('bhtd,bhde->bhte', qc, C)
        den = jnp.sum(sc, axis=-1) + w_inter * jnp.einsum('bhtd,bhd->bht', qc, n)
        h = num / jnp.maximum(jnp.abs(den), jnp.exp(-m_t))[..., None]
        bl = b[..., -1]
        g = bl[..., None] - b + ic
        m_new = jnp.maximum(bl + m, jnp.max(g, axis=-1))
        wk = jnp.exp(g - m_new[..., None])
        decay = jnp.exp(bl + m - m_new)
        C_new = decay[..., None, None] * C + jnp.einsum('bhs,bhsd,bhse->bhde', wk, kc, vc)
        n_new = decay[..., None] * n + jnp.einsum('bhs,bhsd->bhd', wk, kc)
        return (C_new, n_new, m_new), h

    init = (jnp.zeros((bsz, nh, dk, dv), jnp.float32), jnp.zeros((bsz, nh, dk), jnp.float32),
            jnp.zeros((bsz, nh), jnp.float32))
    _, hs = lax.scan(body, init, (chunks(q), chunks(k) * dk ** -0.5, chunks(v), chunks(i_pre), chunks(logf)))
    return jnp.moveaxis(hs, 0, 2).reshape(bsz, nh, seq, dv).astype(v.dtype)


def even_layer(h, w_in, w_out, a_ln_g, a_ws, a_bs, b_conv_w, b_conv_b, b_wq, b_wk, b_wv,
               b_ig_b, b_fg_b, b_gn_g, b_skip):
    bsz, seq, _ = h.shape
    u, va, za, xm, og, ig, fg, zb = jnp.split(h @ w_in, EVEN_SPLITS, axis=-1)
    u = jax.nn.gelu(u)
    va = layernorm(jax.nn.gelu(va), a_ln_g)
    vg = va.reshape(bsz, seq // A_CHUNK, A_CHUNK, A_GROUPS, A_GROUP_DIM)
    sg = jnp.einsum('gts,bcsgd->bctgd', jnp.tril(a_ws), vg) + a_bs.T[:, :, None]
    y_a = u * sg.reshape(bsz, seq, A_WIDTH) * jax.nn.silu(za)
    xc = jax.nn.silu(causal_depthwise_conv(xm, b_conv_w, b_conv_b))
    xch = xc.reshape(bsz, seq, B_HEADS, B_HEAD_DIM)
    xmh = xm.reshape(bsz, seq, B_HEADS, B_HEAD_DIM)
    q = jnp.einsum('bshd,hde->bhse', xch, b_wq)
    k = jnp.einsum('bshd,hde->bhse', xch, b_wk)
    v = jnp.einsum('bshd,hde->bhse', xmh, b_wv)
    ht = mlstm_chunkwise(q, k, v, jnp.swapaxes(ig + b_ig_b, 1, 2), jnp.swapaxes(fg + b_fg_b, 1, 2))
    ht = jnp.swapaxes(ht, 1, 2) * jax.nn.sigmoid(og).reshape(bsz, seq, B_HEADS, B_HEAD_DIM)
    ht = layernorm(ht, b_gn_g.reshape(B_HEADS, B_HEAD_DIM))
    y_b = (ht.reshape(bsz, seq, B_WIDTH) + b_skip * xc) * jax.nn.silu(zb)
    return jnp.concatenate([y_a, y_b], axis=-1) @ w_out


def diff_attention(q1, q2, k1, k2, v, lam):
    seq, d = q1.shape[1], q1.shape[-1]
    scale = d ** -0.5
    outs = []
    for j in range(seq // Q_BLOCK):
        lo, hi = j * Q_BLOCK, (j + 1) * Q_BLOCK
        mask = jnp.arange(hi)[None, :] <= jnp.arange(lo, hi)[:, None]

        def probs(qa, ka):
            s = jnp.einsum('bqhd,bkhd->bhqk', qa[:, lo:hi], ka[:, :hi]).astype(jnp.float32) * scale
            return jax.nn.softmax(jnp.where(mask, s, -jnp.inf), axis=-1)

        p = probs(q1, k1) - lam * probs(q2, k2)
        outs.append(jnp.einsum('bhqk,bkhe->bqhe', p.astype(v.dtype), v[:, :hi]))
    return jnp.concatenate(outs, axis=1)


def odd_layer(h, w_in, w_out, lq1, lk1, lq2, lk2, subln_g, lam_init, cos, sin):
    bsz, seq, _ = h.shape
    q, k, v, z = jnp.split(h @ w_in, 4, axis=-1)
    q = apply_rope(q.reshape(bsz, seq, C_HEADS, 2, C_HEAD_DIM), cos, sin)
    k = apply_rope(k.reshape(bsz, seq, C_HEADS, 2, C_HEAD_DIM), cos, sin)
    v = v.reshape(bsz, seq, C_HEADS, 2 * C_HEAD_DIM)
    f32 = jnp.float32
    lam = (jnp.exp(jnp.sum(lq1.astype(f32) * lk1.astype(f32))) -
           jnp.exp(jnp.sum(lq2.astype(f32) * lk2.astype(f32))) + lam_init)
    o = diff_attention(q[..., 0, :], q[..., 1, :], k[..., 0, :], k[..., 1, :], v, lam)
    o = rmsnorm(o, subln_g) * (1.0 - lam_init)
    o = o.reshape(bsz, seq, C_WIDTH) * jax.nn.silu(z)
    return o @ w_out


def lambda_init(layer):
    return 0.8 - 0.6 * math.exp(-0.3 * layer)


def setup_inputs(seed: int = 0) -> dict:
    key = jax.random.key(seed)
    ks = jax.random.split(key, 24)
    ne, no = (DEPTH + 1) // 2, DEPTH // 2

    def nrm(k, shape, scale):
        return jax.random.normal(k, shape, jnp.float32) * scale

    return {
        "x": nrm(ks[0], (BATCH, SEQ, D_MODEL), 1.0),
        "norm_g": 1.0 + nrm(ks[1], (DEPTH, D_MODEL), 0.02),
        "ev_w_in": nrm(ks[2], (ne, D_MODEL, EVEN_IN), D_MODEL ** -0.5),
        "ev_w_out": nrm(ks[3], (ne, EVEN_OUT, D_MODEL), EVEN_OUT ** -0.5),
        "a_ln_g": 1.0 + nrm(ks[4], (ne, A_WIDTH), 0.02),
        "a_ws": nrm(ks[5], (ne, A_GROUPS, A_CHUNK, A_CHUNK), A_CHUNK ** -0.5),
        "a_bs": 1.0 + nrm(ks[6], (ne, A_GROUPS, A_CHUNK), 0.1),
        "b_conv_w": nrm(ks[7], (ne, B_CONV, B_WIDTH), B_CONV ** -0.5),
        "b_conv_b": nrm(ks[8], (ne, B_WIDTH), 0.02),
        "b_wq": nrm(ks[9], (ne, B_HEADS, B_HEAD_DIM, B_HEAD_DIM), B_HEAD_DIM ** -0.5),
        "b_wk": nrm(ks[10], (ne, B_HEADS, B_HEAD_DIM, B_HEAD_DIM), B_HEAD_DIM ** -0.5),
        "b_wv": nrm(ks[11], (ne, B_HEADS, B_HEAD_DIM, B_HEAD_DIM), B_HEAD_DIM ** -0.5),
        "b_ig_b": nrm(ks[12], (ne, B_HEADS), 0.1),
        "b_fg_b": jnp.linspace(3.0, 6.0, B_HEADS, dtype=jnp.float32) + nrm(ks[13], (ne, B_HEADS), 0.1),
        "b_gn_g": 1.0 + nrm(ks[14], (ne, B_WIDTH), 0.02),
        "b_skip": 1.0 + nrm(ks[15], (ne, B_WIDTH), 0.02),
        "od_w_in": nrm(ks[16], (no, D_MODEL, ODD_IN), D_MODEL ** -0.5),
        "od_w_out": nrm(ks[17], (no, C_WIDTH, D_MODEL), C_WIDTH ** -0.5),
        "c_lam_q1": nrm(ks[18], (no, C_HEAD_DIM), 0.1),
        "c_lam_k1": nrm(ks[19], (no, C_HEAD_DIM), 0.1),
        "c_lam_q2": nrm(ks[20], (no, C_HEAD_DIM), 0.1),
        "c_lam_k2": nrm(ks[21], (no, C_HEAD_DIM), 0.1),
        "c_subln_g": 1.0 + nrm(ks[22], (no, 2 * C_HEAD_DIM), 0.02),
        "final_g": 1.0 + nrm(ks[23], (D_MODEL,), 0.02),
    }


def reference(x, norm_g, ev_w_in, ev_w_out, a_ln_g, a_ws, a_bs, b_conv_w, b_conv_b, b_wq, b_wk, b_wv,
              b_ig_b, b_fg_b, b_gn_g, b_skip, od_w_in, od_w_out, c_lam_q1, c_lam_k1, c_lam_q2,
              c_lam_k2, c_subln_g, final_g):
    cos, sin = rope_tables(x.shape[1], C_HEAD_DIM)
    for layer in range(DEPTH):
        hn = rmsnorm(x, norm_g[layer])
        if layer % 2 == 0:
            e = layer // 2
            x = x + even_layer(hn, ev_w_in[e], ev_w_out[e], a_ln_g[e], a_ws[e], a_bs[e], b_conv_w[e],
                               b_conv_b[e], b_wq[e], b_wk[e], b_wv[e], b_ig_b[e], b_fg_b[e], b_gn_g[e],
                               b_skip[e])
        else:
            o = layer // 2
            x = x + odd_layer(hn, od_w_in[o], od_w_out[o], c_lam_q1[o], c_lam_k1[o], c_lam_q2[o],
                              c_lam_k2[o], c_subln_g[o], lambda_init(layer), cos, sin)
    return rmsnorm(x, final_g)
```

```python
import math
from contextlib import ExitStack

import numpy as np
import concourse.bass as bass
import concourse.mybir as mybir
from concourse.bass_utils import run_bass_kernel_spmd

F32 = mybir.dt.float32
BF16 = mybir.dt.bfloat16
AF = mybir.ActivationFunctionType
ALU = mybir.AluOpType
AX = mybir.AxisListType

D = 1024
NCORES = 8
EPS = 1e-6
EVEN_IN = 6152
OD_EXT = 12288
N_DMA_SEMS = 84
FULL_LAYERS = (("even", 0, 0), ("odd", 0, 1), ("even", 1, 2), ("odd", 1, 3))


class Sched:
    ENGS = ("pe", "act", "dve", "pool", "sp")

    def __init__(self, nc, es):
        self.nc = nc
        self.es = es
        self.prog = {e: [] for e in self.ENGS}
        self.idx = {e: 0 for e in self.ENGS}
        self.seen = {e: {} for e in self.ENGS}
        self.cnt = {}
        self.last_w = {}
        self.readers = {}
        self.eng_sem = {}
        for e in ("pe", "act", "dve", "pool"):
            self.eng_sem[e] = self.new_sem("c_" + e)
        self.dma_sems = [self.new_sem(f"dq{i}") for i in range(N_DMA_SEMS)]
        self.dma_i = 0
        self.n_ops = 0

    def new_sem(self, name):
        s = self.es.enter_context(self.nc.semaphore(name))
        self.cnt[s] = 0
        return s

    def get_dma_sem(self):
        s = self.dma_sems[self.dma_i % N_DMA_SEMS]
        self.dma_i += 1
        return s

    def op(self, eng, fn, reads=(), writes=(), dma_sem=None, ndma=1):
        deps = []
        for k in reads:
            t = self.last_w.get(k)
            if t is not None:
                deps.append(t)
        for k in writes:
            t = self.last_w.get(k)
            if t is not None:
                deps.append(t)
            deps.extend(self.readers.get(k, ()))
        need = {}
        seen = self.seen[eng]
        cur = self.idx[eng]
        for (sem, val, teng, tidx, is_dma) in deps:
            if teng == eng and not is_dma:
                if eng == "pe":
                    continue
            if seen.get(sem, 0) >= val:
                continue
            if need.get(sem, 0) < val:
                need[sem] = val
        if dma_sem is not None:
            prev = self.cnt[dma_sem]
            if prev > 0 and seen.get(dma_sem, 0) < prev:
                need[dma_sem] = prev
        for sem, val in need.items():
            self.prog[eng].append(("w", sem, val))
            seen[sem] = val
        if dma_sem is None:
            sem = self.eng_sem[eng]
            self.cnt[sem] += 1
            is_dma = False
        else:
            sem = dma_sem
            self.cnt[sem] += 16 * ndma
            is_dma = True
        tok = (sem, self.cnt[sem], eng, cur, is_dma)
        self.prog[eng].append(("o", fn, sem))
        for k in writes:
            self.last_w[k] = tok
            self.readers[k] = []
        for k in reads:
            self.readers.setdefault(k, []).append(tok)
        self.idx[eng] = cur + 1
        self.n_ops += 1
        return tok

    def barrier(self, engs=None):
        for eng in (engs or self.ENGS):
            for sem, val in self.cnt.items():
                if val > 0 and self.seen[eng].get(sem, 0) < val:
                    self.prog[eng].append(("w", sem, val))
                    self.seen[eng][sem] = val
        if engs is None:
            self.last_w = {}
            self.readers = {}

    def emit(self):
        nc = self.nc
        engobj = {"pe": "tensor", "act": "scalar", "dve": "vector", "pool": "gpsimd", "sp": "sync"}
        with nc.Block() as block:
            for e in self.ENGS:
                prog = self.prog[e]

                def body(eng, prog=prog):
                    for it in prog:
                        if it[0] == "w":
                            eng.wait_ge(it[1], it[2])
                        else:
                            it[1](eng, it[2])

                getattr(block, engobj[e])(body)


class Tl:
    def __init__(self, t, sem=None):
        self.t = t
        self.sem = sem

    def __getitem__(self, k):
        return self.t[k]


def rope_tables_T(S):
    inv = (10000.0 ** (-np.arange(0, 64, 2, dtype=np.float32) / np.float32(64))).astype(np.float32)
    ang = (np.arange(S, dtype=np.float32)[:, None] * inv[None, :]).astype(np.float32)
    cos, sin = np.cos(ang).astype(np.float32), np.sin(ang).astype(np.float32)
    cosT = np.zeros((128, S), np.float32)
    sinT = np.zeros((128, S), np.float32)
    for p in range(128):
        d = p % 64
        cosT[p] = cos[:, d % 32]
        sinT[p] = -sin[:, d % 32] if d < 32 else sin[:, d % 32]
    return cosT, sinT


def lambda_init(layer):
    return 0.8 - 0.6 * math.exp(-0.3 * layer)


def build_program(NS, S, layers=FULL_LAYERS, dbg=False):
    T = NS * S
    NT = T // 128
    NC5 = T // 512
    CPS = S // 128
    n_layers = len(layers)
    nc = bass.Bass("TRN2", target_bir_lowering=False)
    es = ExitStack()

    def din(name, shape, dt=F32):
        return nc.dram_tensor(name, list(shape), dt, kind="ExternalInput").ap()

    def dscr(name, shape, dt):
        if dbg:
            return nc.dram_tensor(name, list(shape), dt, kind="ExternalOutput").ap()
        return nc.dram_tensor(name, list(shape), dt).ap()

    x_in = din("x", [T, D])
    gvec = din("gvec", [128, 5, D])
    ident_d = din("ident", [128, 128])
    tri_d = din("tri", [128, 128])
    cosT_d = din("cosT", [128, S])
    sinT_d = din("sinT", [128, S])
    ev_w_in = din("ev_w_in", [2, D, EVEN_IN])
    ev_w_out = din("ev_w_out", [2, 2048, D])
    od_w_in = din("od_w_in", [2, D, OD_EXT])
    od_w_out = din("od_w_out", [2, 2048, D])
    a_ln_g = din("a_ln_g", [2, 128, D])
    a_wsT = din("a_wsT", [2, 8, 128, 128])
    a_bsT = din("a_bsT", [2, 128, 8])
    convw = din("convw", [2, 128, 8, 4])
    colv = din("colv", [2, 128, 3, 8])
    b_wqkv = din("b_wqkv", [2, 3, 4, 256, 256])
    gate_b = din("gate_b", [2, 128, 8])
    lamv = din("lamv", [2, 128, 4, 64])
    subln = din("subln", [2, 128, 1])
    out = nc.dram_tensor("out", [T, D], F32, kind="ExternalOutput").ap()

    xs = dscr("xs", [T, D], F32)
    hnT = dscr("hnT", [8, 128, T], BF16)
    yT = dscr("yT", [16, 128, T], BF16)
    qT_s = dscr("qT_s", [16, 128, T], BF16)
    kT_s = dscr("kT_s", [16, 128, T], BF16)
    zT_s = dscr("zT_s", [16, 128, T], BF16)
    v_s = dscr("v_s", [T, 2048], BF16)
    u_s = dscr("u_s", [T, D], BF16)
    va_s = dscr("va_s", [T, D], BF16)
    og_s = dscr("og_s", [T, D], BF16)
    gt_s = dscr("gt_s", [T, 8], F32)
    zaT_s = dscr("zaT_s", [8, 128, T], BF16)
    xmT_s = dscr("xmT_s", [8, 128, T], BF16)
    zbT_s = dscr("zbT_s", [8, 128, T], BF16)

    with es:
        sc = Sched(nc, es)

        def I(eng, f, reads=(), writes=()):
            sc.op(eng, lambda e, s: f(e).then_inc(s, 1), reads, writes)

        def DMA(eng, tl, f, reads=(), writes=(), n=1):
            def g(e, s):
                r = f(e)
                assert len(r) == n
                for x in r:
                    x.then_inc(s, 16)
            sc.op(eng, g, reads, writes, dma_sem=tl.sem, ndma=n)

        uid = [0]

        def sbt(st, name, shape, dt, dma=True):
            uid[0] += 1
            t = st.enter_context(nc.sbuf_tensor(f"{name}_{uid[0]}", list(shape), dt))
            return Tl(t, sc.get_dma_sem() if dma else None)

        def pool(st, name, shape, dt, n, dma=True):
            return [sbt(st, f"{name}{i}", shape, dt, dma) for i in range(n)]

        ident_f = sbt(es, "ident_f", [128, 128], F32)
        ident_b = sbt(es, "ident_b", [128, 128], BF16)
        tri_f = sbt(es, "tri_f", [128, 128], F32)
        tri_b = sbt(es, "tri_b", [128, 128], BF16)
        ones_f = sbt(es, "ones_f", [128, 128], F32)
        psF = [Tl(es.enter_context(nc.psum_tensor(f"psF{i}", [128, 512], F32))) for i in range(7)]
        psB = Tl(es.enter_context(nc.psum_tensor("psB", [128, 1024], BF16)))
        ps_i = [0]

        def ps_next():
            p = psF[ps_i[0] % len(psF)]
            ps_i[0] += 1
            return p

        DMA("sp", ident_f, lambda e: [e.dma_start(out=ident_f[:], in_=ident_d[:, :])], writes=[ident_f])
        DMA("sp", tri_f, lambda e: [e.dma_start(out=tri_f[:], in_=tri_d[:, :])], writes=[tri_f])
        I("dve", lambda e: e.tensor_copy(out=ident_b[:], in_=ident_f[:]), [ident_f], [ident_b])
        I("dve", lambda e: e.tensor_copy(out=tri_b[:], in_=tri_f[:]), [tri_f], [tri_b])
        I("dve", lambda e: e.memset(ones_f[:], 1.0), [], [ones_f])

        def rstd_chain(st, ss_col, scale, out_col):
            I("dve", lambda e: e.tensor_scalar(out=st[:, 6:7], in0=st[:, ss_col:ss_col + 1], scalar1=scale,
                                               scalar2=EPS, op0=ALU.mult, op1=ALU.add),
              [(st, ss_col)], [(st, 6)])
            I("act", lambda e: e.activation(out=st[:, 7:8], in_=st[:, 6:7], func=AF.Ln), [(st, 6)], [(st, 7)])
            I("act", lambda e: e.activation(out=st[:, out_col:out_col + 1], in_=st[:, 7:8], func=AF.Exp, scale=-0.5),
              [(st, 7)], [(st, out_col)])

        def norm_to_hnT(st_pools, xt, tt, gt_, hT, final=False):
            junk, stat, hnp, otp = st_pools
            jt = junk[tt % len(junk)]
            st = stat[tt % len(stat)]
            I("act", lambda e: e.activation(out=jt[:], in_=xt[:], func=AF.Square, accum_out=st[:, 0:1]),
              [xt], [jt, (st, 0)])
            rstd_chain(st, 0, 1.0 / D, 1)
            if final:
                ot = otp[tt % len(otp)]
                I("dve", lambda e: e.scalar_tensor_tensor(out=ot[:], in0=xt[:], scalar=st[:, 1:2], in1=gt_[:],
                                                          op0=ALU.mult, op1=ALU.mult), [xt, (st, 1), gt_], [ot])
                DMA("sp", ot, lambda e: [e.dma_start(out=out[tt * 128:(tt + 1) * 128, :], in_=ot[:])], reads=[ot])
                return
            hn = hnp[tt % len(hnp)]
            I("dve", lambda e: e.scalar_tensor_tensor(out=hn[:], in0=xt[:], scalar=st[:, 1:2], in1=gt_[:],
                                                      op0=ALU.mult, op1=ALU.mult), [xt, (st, 1), gt_], [hn])
            for kc in range(8):
                I("pe", lambda e, kc=kc: e.transpose(out=psB[:, kc * 128:(kc + 1) * 128],
                                                     in_=hn[:, kc * 128:(kc + 1) * 128], identity=ident_b[:]),
                  [hn, ident_b], [psB])
            j = tt % 4
            I("act", lambda e: e.activation(out=hT[:, :, j * 128:(j + 1) * 128],
                                            in_=psB[:].rearrange("p (k t) -> p k t", k=8), func=AF.Copy),
              [], [psB, (hT, j)])
            if j == 3:
                c = tt // 4
                DMA("sp", hT, lambda e: [e.dma_start(out=hnT[:, :, c * 512:(c + 1) * 512].rearrange("k p t -> p k t"),
                                                     in_=hT[:])],
                    reads=[(hT, 0), (hT, 1), (hT, 2), (hT, 3)])

        def phase_norm0(gidx):
            with ExitStack() as st:
                xp = pool(st, "n0x", [128, D], F32, 3)
                pools = (pool(st, "n0j", [128, D], F32, 2, False), pool(st, "n0s", [128, 8], F32, 4, False),
                         pool(st, "n0h", [128, D], BF16, 2, False), None)
                hTp = pool(st, "n0T", [128, 8, 512], BF16, 2)
                gt_ = sbt(st, "gt0", [128, D], F32)
                DMA("sp", gt_, lambda e: [e.dma_start(out=gt_[:], in_=gvec[:, gidx, :])], writes=[gt_])
                for tt in range(NT):
                    xt = xp[tt % 3]
                    DMA("sp", xt, lambda e, xt=xt, tt=tt: [e.dma_start(out=xt[:], in_=x_in[tt * 128:(tt + 1) * 128, :])],
                        writes=[xt])
                    DMA("sp", xt, lambda e, xt=xt, tt=tt: [e.dma_start(out=xs[tt * 128:(tt + 1) * 128, :], in_=xt[:])],
                        reads=[xt])
                    norm_to_hnT(pools, xt, tt, gt_, hTp[(tt // 4) % 2])
                sc.barrier()

        def load_w_group(wt, w_dram_2d, c0, ncols, stage):
            DMA("sp", stage, lambda e: [e.dma_start(out=stage[:, :, 0:ncols],
                                                    in_=w_dram_2d[:, c0:c0 + ncols].rearrange("(kc p) c -> p kc c", p=128))],
                writes=[stage])
            I("pool", lambda e: e.tensor_copy(out=wt[:, :, 0:ncols], in_=stage[:, :, 0:ncols]), [stage], [wt])

        def load_hT(hT, c):
            DMA("sp", hT, lambda e: [e.dma_start(out=hT[:], in_=hnT[:, :, c * 512:(c + 1) * 512].rearrange("k p t -> p k t"))],
                writes=[hT])

        def proj_fm(wt, units, evac, hTp):
            load_hT(hTp[0], 0)
            for c in range(NC5):
                hT = hTp[c % len(hTp)]
                if c + 1 < NC5:
                    load_hT(hTp[(c + 1) % len(hTp)], c + 1)
                for ui, cols in enumerate(units):
                    pss = []
                    for co in cols:
                        ps = ps_next()
                        for kc in range(8):
                            I("pe", lambda e, ps=ps, kc=kc, co=co, hT=hT: e.matmul(
                                ps[:], lhsT=wt[:, kc, co:co + 128], rhs=hT[:, kc, :], start=(kc == 0), stop=(kc == 7)),
                              [wt, hT], [ps])
                        pss.append(ps)
                    evac(ui, c, pss)

        def proj_tm(wt, ncols, evac, hTp):
            load_hT(hTp[0], 0)
            for c in range(NC5):
                hT = hTp[c % len(hTp)]
                if c + 1 < NC5:
                    load_hT(hTp[(c + 1) % len(hTp)], c + 1)
                for j in range(4):
                    tt = c * 4 + j
                    for half in range((ncols + 511) // 512):
                        w = min(512, ncols - half * 512)
                        ps = ps_next()
                        for kc in range(8):
                            I("pe", lambda e, ps=ps, kc=kc, half=half, w=w, j=j, hT=hT: e.matmul(
                                ps[:, 0:w], lhsT=hT[:, kc, j * 128:(j + 1) * 128],
                                rhs=wt[:, kc, half * 512:half * 512 + w], start=(kc == 0), stop=(kc == 7)),
                              [wt, hT], [ps])
                        evac(tt, half, ps, w)

        def phase_out(li, w_out_2d):
            final = (li == n_layers - 1)
            gidx = 4 if final else layers[li + 1][2]
            with ExitStack() as st:
                wo = sbt(st, "wo", [128, 16, D], BF16, False)
                wos = sbt(st, "wos", [128, 8, D], F32)
                for hf in range(2):
                    DMA("sp", wos, lambda e, hf=hf: [e.dma_start(
                        out=wos[:], in_=w_out_2d[hf * 1024:(hf + 1) * 1024, :].rearrange("(kc p) c -> p kc c", p=128))], writes=[wos])
                    I("pool", lambda e, hf=hf: e.tensor_copy(out=wo[:, hf * 8:(hf + 1) * 8, :], in_=wos[:]), [wos], [(wo, hf)])
                gt_ = sbt(st, "gt", [128, D], F32)
                DMA("sp", gt_, lambda e: [e.dma_start(out=gt_[:], in_=gvec[:, gidx, :])], writes=[gt_])
                yp = pool(st, "poy", [128, 16, 512], BF16, 2)
                xp = pool(st, "pox", [128, D], F32, 4)
                pools = (pool(st, "poj", [128, D], F32, 2, False), pool(st, "pos", [128, 8], F32, 4, False),
                         pool(st, "poh", [128, D], BF16, 2, False), pool(st, "poo", [128, D], F32, 2))
                hTp = pool(st, "poT", [128, 8, 512], BF16, 2)
                def load_y(c):
                    yt = yp[c % 2]
                    DMA("sp", yt, lambda e: [e.dma_start(
                        out=yt[:, hf * 8:(hf + 1) * 8, :], in_=yT[hf * 8:(hf + 1) * 8, :, c * 512:(c + 1) * 512].rearrange("k p t -> p k t"))
                        for hf in range(2)], writes=[yt], n=2)

                load_y(0)

                def stage_a(tt):
                    c, j = tt // 4, tt % 4
                    yt = yp[c % 2]
                    if j == 1 and c + 1 < NC5:
                        load_y(c + 1)
                    xt = xp[tt % 4]
                    DMA("sp", xt, lambda e: [e.dma_start(out=xt[:], in_=xs[tt * 128:(tt + 1) * 128, :])], writes=[xt])
                    for half in range(2):
                        ps = ps_next()
                        for kc in range(16):
                            I("pe", lambda e, ps=ps, kc=kc, half=half: e.matmul(
                                ps[:], lhsT=yt[:, kc, j * 128:(j + 1) * 128],
                                rhs=wo[:, kc, half * 512:(half + 1) * 512], start=(kc == 0), stop=(kc == 15)),
                              [(wo, 0), (wo, 1), yt], [ps])
                        I("dve", lambda e, ps=ps, half=half: e.tensor_tensor(
                            out=xt[:, half * 512:(half + 1) * 512], in0=ps[:], in1=xt[:, half * 512:(half + 1) * 512],
                            op=ALU.add), [], [ps, xt])
                    if not final:
                        DMA("sp", xt, lambda e: [e.dma_start(out=xs[tt * 128:(tt + 1) * 128, :], in_=xt[:])], reads=[xt])

                def stage_b(tt):
                    norm_to_hnT(pools, xp[tt % 4], tt, gt_, hTp[(tt // 4) % 2], final=final)

                stage_a(0)
                for tt in range(NT):
                    if tt + 1 < NT:
                        stage_a(tt + 1)
                    stage_b(tt)
                sc.barrier()

        def phase_odd_inproj(o):
            W = od_w_in[o]
            with ExitStack() as st:
                wtp = pool(st, "oiw", [128, 8, 1024], BF16, 2, False)
                wstage = sbt(st, "oiws", [128, 8, 1024], F32)
                hTp = pool(st, "oih", [128, 8, 512], BF16, 3)
                cosT = sbt(st, "cosT", [128, S], F32)
                sinT = sbt(st, "sinT", [128, S], F32)
                DMA("sp", cosT, lambda e: [e.dma_start(out=cosT[:], in_=cosT_d[:, :])], writes=[cosT])
                DMA("sp", sinT, lambda e: [e.dma_start(out=sinT[:], in_=sinT_d[:, :])], writes=[sinT])
                t1p = pool(st, "oit1", [128, 512], F32, 2, False)
                t2p = pool(st, "oit2", [128, 512], F32, 2, False)
                stg = pool(st, "oist", [128, 512], BF16, 4)
                cnt = [0]
                for g in range(12):
                    wt = wtp[g % 2]
                    load_w_group(wt, W, g * 1024, 1024, wstage)
                    if g < 8:
                        dst = qT_s if g < 4 else kT_s
                        units = [(h * 256, h * 256 + 128) for h in range(4)]

                        def evac(ui, c, pss, g=g, dst=dst):
                            i = cnt[0]
                            cnt[0] += 1
                            t1, t2, sg = t1p[i % 2], t2p[i % 2], stg[i % 4]
                            p0 = (c * 512) % S
                            I("dve", lambda e: e.tensor_tensor(out=t1[:], in0=pss[0][:], in1=cosT[:, p0:p0 + 512], op=ALU.mult),
                              [cosT], [pss[0], t1])
                            I("dve", lambda e: e.tensor_tensor(out=t2[:], in0=pss[1][:], in1=sinT[:, p0:p0 + 512], op=ALU.mult),
                              [sinT], [pss[1], t2])
                            I("pool", lambda e: e.tensor_tensor(out=sg[:], in0=t1[:], in1=t2[:], op=ALU.add), [t1, t2], [sg])
                            h = (g % 4) * 4 + ui
                            DMA("sp", sg, lambda e: [e.dma_start(out=dst[h, :, c * 512:(c + 1) * 512], in_=sg[:])], reads=[sg])
                        proj_fm(wt, units, evac, hTp)
                    elif g < 10:
                        units = [(j * 128,) for j in range(8)]

                        def evac(ui, c, pss, g=g):
                            i = cnt[0]
                            cnt[0] += 1
                            sg = stg[i % 4]
                            I("act", lambda e: e.activation(out=sg[:], in_=pss[0][:], func=AF.Silu), [], [pss[0], sg])
                            h = (g - 8) * 8 + ui
                            DMA("sp", sg, lambda e: [e.dma_start(out=zT_s[h, :, c * 512:(c + 1) * 512], in_=sg[:])], reads=[sg])
                        proj_fm(wt, units, evac, hTp)
                    else:
                        def evac(tt, half, ps, w, g=g):
                            i = cnt[0]
                            cnt[0] += 1
                            sg = stg[i % 4]
                            I("act", lambda e: e.activation(out=sg[:], in_=ps[:], func=AF.Copy), [], [ps, sg])
                            c0 = (g - 10) * 1024 + half * 512
                            DMA("sp", sg, lambda e: [e.dma_start(out=v_s[tt * 128:(tt + 1) * 128, c0:c0 + 512], in_=sg[:])],
                                reads=[sg])
                        proj_tm(wt, 1024, evac, hTp)
                sc.barrier()

        def phase_attn(o, layer_no):
            li = lambda_init(layer_no)
            with ExitStack() as st:
                lv = sbt(st, "lv", [128, 4, 64], F32)
                sl = sbt(st, "sl", [128, 8], F32, False)
                gcol = sbt(st, "gcol", [128, 1], F32)
                lj = sbt(st, "lj", [128, 64], F32, False)
                DMA("sp", lv, lambda e: [e.dma_start(out=lv[:], in_=lamv[o])], writes=[lv])
                DMA("sp", gcol, lambda e: [e.dma_start(out=gcol[:], in_=subln[o])], writes=[gcol])
                for i2 in range(2):
                    I("dve", lambda e, i2=i2: e.tensor_tensor(out=lj[:], in0=lv[:, 2 * i2, :], in1=lv[:, 2 * i2 + 1, :], op=ALU.mult),
                      [lv], [lj])
                    I("dve", lambda e, i2=i2: e.reduce_sum(out=sl[:, i2:i2 + 1], in_=lj[:], axis=AX.X), [lj], [(sl, i2)])
                    I("act", lambda e, i2=i2: e.activation(out=sl[:, 2 + i2:3 + i2], in_=sl[:, i2:i2 + 1], func=AF.Exp),
                      [(sl, i2)], [(sl, 2 + i2)])
                I("dve", lambda e: e.tensor_tensor(out=sl[:, 5:6], in0=sl[:, 3:4], in1=sl[:, 2:3], op=ALU.subtract),
                  [(sl, 2), (sl, 3)], [(sl, 5)])
                I("dve", lambda e: e.tensor_scalar(out=sl[:, 4:5], in0=sl[:, 5:6], scalar1=-li, scalar2=None, op0=ALU.add),
                  [(sl, 5)], [(sl, 4)])
                I("dve", lambda e: e.tensor_scalar(out=gcol[:], in0=gcol[:], scalar1=1.0 - li, scalar2=None, op0=ALU.mult),
                  [gcol], [gcol])

                NG = S // 256
                qp = pool(st, "aqq", [128, NG, 2, 256], BF16, 2)
                for b_ in range(2):
                    I("dve", lambda e, b_=b_: e.memset(qp[b_][64:128, :, 0, :], 0.0), [], [(qp[b_], "z0")])
                    I("dve", lambda e, b_=b_: e.memset(qp[b_][0:64, :, 1, :], 0.0), [], [(qp[b_], "z1")])
                kp = pool(st, "ak", [128, S], BF16, 2)
                zp = pool(st, "az", [128, S], BF16, 2)
                vp = pool(st, "av", [128, CPS, 132], BF16, 2)
                yp = pool(st, "ay", [128, S], BF16, 2)
                pp = pool(st, "ap", [128, 512], BF16, 6, False)
                stp = pool(st, "ast", [128, 8], F32, 4, False)
                o1p = pool(st, "ao1", [128, 128], F32, 4, False)
                o2p = pool(st, "ao2", [128, 128], F32, 4, False)
                jkp = pool(st, "ajk", [128, 128], F32, 4, False)
                onp = pool(st, "aon", [128, 128], BF16, 4, False)
                for v_ in vp:
                    I("dve", lambda e, v_=v_: e.memset(v_[:, :, 128:129], 1.0), [], [(v_, "ones")])
                it = 0
                qb = 0
                epi_i = [0]
                gstep = [0]
                pending = []

                def defer(delay, fn):
                    pending.append([gstep[0] + delay, fn])

                def run_due(force=False):
                    keep = []
                    for item in list(pending):
                        if force or item[0] <= gstep[0]:
                            item[1]()
                        else:
                            keep.append(item)
                    pending[:] = keep

                def issue_loads(idx):
                    s_, h = idx // 16, idx % 16
                    qq, k, z, v = qp[idx % 2], kp[idx % 2], zp[idx % 2], vp[idx % 2]
                    t0 = s_ * S
                    DMA("sp", qq, lambda e: [
                        e.dma_start(out=qq[0:64, :, 0, :], in_=qT_s[h, 0:64, t0:t0 + S].rearrange("p (g q) -> p g q", q=256)),
                        e.dma_start(out=qq[64:128, :, 1, :], in_=qT_s[h, 64:128, t0:t0 + S].rearrange("p (g q) -> p g q", q=256))],
                        writes=[qq], n=2)
                    DMA("sp", k, lambda e: [e.dma_start(out=k[:], in_=kT_s[h, :, t0:t0 + S])], writes=[k])
                    DMA("sp", z, lambda e: [e.dma_start(out=z[:], in_=zT_s[h, :, t0:t0 + S])], writes=[z])
                    nvp = (CPS + 7) // 8
                    DMA("sp", v, lambda e: [e.dma_start(
                        out=v[:, j8 * 8:min(CPS, j8 * 8 + 8), 0:128],
                        in_=v_s[t0 + j8 * 1024:min(t0 + S, t0 + j8 * 1024 + 1024), h * 128:(h + 1) * 128].rearrange(
                            "(c p) e -> p c e", p=128)) for j8 in range(nvp)], writes=[v], n=nvp)

                issue_loads(0)
                for s in range(NS):
                    for h in range(16):
                        qq, k, z, v, y = qp[it % 2], kp[it % 2], zp[it % 2], vp[it % 2], yp[it % 2]
                        it += 1
                        t0 = s * S
                        steps = [(G, i) for G in range(CPS // 2) for i in range(2 * G + 2)]
                        info = {}

                        def emit_qk(n, qq=qq, k=k):
                            nonlocal qb
                            G, i = steps[n]
                            r = 1 if i == 2 * G + 1 else 0
                            sps = psF[4 + qb % 3]
                            pt = pp[qb % 4]
                            qb += 1
                            info[n] = (r, sps, pt)
                            qdeps = [k, qq, (qq, "z0"), (qq, "z1")]
                            if r == 0:
                                I("pe", lambda e, sps=sps, i=i, G=G: e.matmul(
                                    sps[:, 0:512], lhsT=k[:, i * 128:(i + 1) * 128],
                                    rhs=qq[:, G, :, :].rearrange("p c q -> p (c q)"), start=True, stop=True), qdeps, [sps])
                            else:
                                for cmp_ in range(2):
                                    I("pe", lambda e, sps=sps, cmp_=cmp_, i=i, G=G: e.matmul(
                                        sps[:, cmp_ * 256:cmp_ * 256 + 128], lhsT=k[:, i * 128:(i + 1) * 128],
                                        rhs=qq[:, G, cmp_, 128:256], start=True, stop=True), qdeps, [sps])

                        def emit_rest(n, v=v):
                            G, i = steps[n]
                            r, sps, pt = info.pop(n)
                            if r == 0:
                                I("act", lambda e, sps=sps, pt=pt: e.activation(out=pt[:], in_=sps[:], func=AF.Exp, scale=0.125),
                                  [], [sps, pt])
                            else:
                                I("act", lambda e, sps=sps, pt=pt: e.activation(
                                    out=pt[:].rearrange("p (c q) -> p c q", c=2)[:, :, 0:128],
                                    in_=sps[:].rearrange("p (c q) -> p c q", c=2)[:, :, 0:128], func=AF.Exp, scale=0.125),
                                  [], [sps, pt])
                            if i >= 2 * G:
                                for cmp_ in range(2):
                                    ds_ = slice(cmp_ * 256, cmp_ * 256 + 128)
                                    I("dve", lambda e, pt=pt, ds_=ds_: e.tensor_tensor(
                                        out=pt[:, ds_], in0=pt[:, ds_], in1=tri_b[:], op=ALU.mult), [tri_b], [pt])
                            for ql in range(r, 2):
                                for cmp_ in range(2):
                                    acc = psF[(G % 2) * 2 + ql]
                                    col = cmp_ * 256 + (ql - r) * 128
                                    first = (i == 0 and cmp_ == 0)
                                    last = (i == 2 * G + ql)
                                    I("pe", lambda e, acc=acc, pt=pt, col=col, i=i, v=v, first=first, last=last, cmp_=cmp_: e.matmul(
                                        acc[:, cmp_ * 256:cmp_ * 256 + 129], lhsT=pt[:, col:col + 128], rhs=v[:, i, 0:129],
                                        start=first, stop=last, skip_group_check=True),
                                      [pt, v, (v, "ones")], [acc])

                        def emit_epilogue(G, y=y, z=z):
                            par = G % 2
                            bufs = []
                            for ql in range(2):
                                ab = psF[par * 2 + ql]
                                bi = epi_i[0] % 4
                                epi_i[0] += 1
                                stt, o1, o2, jk, on = stp[bi], o1p[bi], o2p[bi], jkp[bi], onp[bi]
                                bufs.append((stt, o2, on))
                                I("dve", lambda e, ab=ab, stt=stt: e.reciprocal(out=stt[:, 0:1], in_=ab[:, 128:129]), [], [ab, (stt, 0)])
                                I("dve", lambda e, ab=ab, stt=stt: e.reciprocal(out=stt[:, 2:3], in_=ab[:, 384:385]), [], [ab, (stt, 2)])
                                I("dve", lambda e, stt=stt: e.tensor_tensor(out=stt[:, 3:4], in0=stt[:, 2:3], in1=sl[:, 4:5], op=ALU.mult),
                                  [(stt, 2), (sl, 4)], [(stt, 3)])
                                I("dve", lambda e, ab=ab, stt=stt, o1=o1: e.tensor_scalar(
                                    out=o1[:], in0=ab[:, 0:128], scalar1=stt[:, 0:1], scalar2=None, op0=ALU.mult),
                                  [(stt, 0)], [ab, o1])
                                I("dve", lambda e, ab=ab, stt=stt, o1=o1, o2=o2: e.scalar_tensor_tensor(
                                    out=o2[:], in0=ab[:, 256:384], scalar=stt[:, 3:4], in1=o1[:], op0=ALU.mult, op1=ALU.add),
                                  [(stt, 3), o1], [ab, o2])
                                I("dve", lambda e, o2=o2, jk=jk, stt=stt: e.scalar_tensor_tensor(
                                    out=jk[:], in0=o2[:], scalar=1.0, in1=o2[:], op0=ALU.mult, op1=ALU.mult, accum_out=stt[:, 4:5]),
                                  [o2], [jk, (stt, 4)])
                                I("dve", lambda e, stt=stt: e.tensor_scalar(out=stt[:, 6:7], in0=stt[:, 4:5], scalar1=1.0 / 128,
                                                                            scalar2=EPS, op0=ALU.mult, op1=ALU.add), [(stt, 4)], [(stt, 6)])

                            def s2():
                                for (stt, o2, on) in bufs:
                                    I("act", lambda e, stt=stt: e.activation(out=stt[:, 7:8], in_=stt[:, 6:7], func=AF.Ln), [(stt, 6)], [(stt, 7)])
                                    I("act", lambda e, stt=stt: e.activation(out=stt[:, 5:6], in_=stt[:, 7:8], func=AF.Exp, scale=-0.5),
                                      [(stt, 7)], [(stt, 5)])

                            def s3():
                                for (stt, o2, on) in bufs:
                                    I("dve", lambda e, o2=o2, on=on, stt=stt: e.tensor_scalar(
                                        out=on[:], in0=o2[:], scalar1=stt[:, 5:6], scalar2=None, op0=ALU.mult), [o2, (stt, 5)], [on])

                            def s4():
                                for ql, (stt, o2, on) in enumerate(bufs):
                                    I("pe", lambda e, on=on, ql=ql: e.transpose(out=psB[:, ql * 128:(ql + 1) * 128], in_=on[:],
                                                                                identity=ident_b[:]), [on, ident_b], [psB])

                            def s5():
                                qcol = 2 * G * 128
                                I("dve", lambda e: e.scalar_tensor_tensor(
                                    out=y[:, qcol:qcol + 256], in0=psB[:, 0:256], scalar=gcol[:, 0:1], in1=z[:, qcol:qcol + 256],
                                    op0=ALU.mult, op1=ALU.mult), [gcol, z], [psB, y])

                            defer(2, s2)
                            defer(3, s3)
                            defer(4, s4)
                            defer(5, s5)

                        LA = 2
                        for n0 in range(min(LA, len(steps))):
                            emit_qk(n0)
                        for n in range(len(steps)):
                            if n + LA < len(steps):
                                emit_qk(n + LA)
                            emit_rest(n)
                            gstep[0] += 1
                            run_due()
                            if n == min(7, len(steps) - 1) and it < NS * 16:
                                issue_loads(it)
                            G, i = steps[n]
                            if i == 2 * G + 1:
                                emit_epilogue(G)
                        defer(6, lambda y=y, h=h, t0=t0: DMA(
                            "sp", y, lambda e: [e.dma_start(out=yT[h, :, t0:t0 + S], in_=y[:])], reads=[y]))
                run_due(force=True)
                sc.barrier()


        def gelu_evac(ps, w, dstt, tmps):
            t1, t2 = tmps
            I("act", lambda e: e.activation(out=t1[:, 0:w], in_=ps[:, 0:w], func=AF.Square), [], [ps, t1])
            I("dve", lambda e: e.tensor_scalar(out=t1[:, 0:w], in0=t1[:, 0:w], scalar1=0.044715, scalar2=1.0,
                                               op0=ALU.mult, op1=ALU.add), [t1], [t1])
            I("dve", lambda e: e.tensor_tensor(out=t2[:, 0:w], in0=ps[:, 0:w], in1=t1[:, 0:w], op=ALU.mult), [t1], [ps, t2])
            I("act", lambda e: e.activation(out=t1[:, 0:w], in_=t2[:, 0:w], func=AF.Sigmoid, scale=1.5957691216057308),
              [t2], [t1])
            I("dve", lambda e: e.tensor_tensor(out=dstt[:, 0:w], in0=ps[:, 0:w], in1=t1[:, 0:w], op=ALU.mult), [t1], [ps, dstt])

        def phase_even_inproj(ei):
            W = ev_w_in[ei]
            with ExitStack() as st:
                wtp = pool(st, "eiw", [128, 8, 1024], BF16, 2, False)
                wstage = sbt(st, "eiws", [128, 8, 1024], F32)
                hTp = pool(st, "eih", [128, 8, 512], BF16, 3)
                t1p = pool(st, "eit1", [128, 512], F32, 2, False)
                t2p = pool(st, "eit2", [128, 512], F32, 2, False)
                stg = pool(st, "eist", [128, 512], BF16, 4)
                stgf = pool(st, "eisf", [128, 8], F32, 3)
                cnt = [0]
                groups = [(0, 1024, "tm", "gelu", u_s), (1024, 1024, "tm", "gelu", va_s), (2048, 1024, "fm", "silu", zaT_s),
                          (3072, 1024, "fm", "copy", xmT_s), (4096, 1024, "tm", "sigm", og_s), (5128, 1024, "fm", "silu", zbT_s),
                          (5120, 8, "tm", "gate", gt_s)]
                for gi, (c0, ncols, mode, kind, dst) in enumerate(groups):
                    wt = wtp[gi % 2]
                    load_w_group(wt, W, c0, ncols, wstage)
                    if mode == "fm":
                        units = [(j * 128,) for j in range(8)]

                        def evac(ui, c, pss, kind=kind, dst=dst):
                            i = cnt[0]
                            cnt[0] += 1
                            sg = stg[i % 4]
                            fn = AF.Silu if kind == "silu" else AF.Copy
                            I("act", lambda e: e.activation(out=sg[:], in_=pss[0][:], func=fn), [], [pss[0], sg])
                            DMA("sp", sg, lambda e: [e.dma_start(out=dst[ui, :, c * 512:(c + 1) * 512], in_=sg[:])], reads=[sg])
                        proj_fm(wt, units, evac, hTp)
                    else:
                        def evac(tt, half, ps, w, kind=kind, dst=dst):
                            i = cnt[0]
                            cnt[0] += 1
                            if kind == "gate":
                                sf = stgf[i % 3]
                                I("act", lambda e: e.activation(out=sf[:], in_=ps[:, 0:8], func=AF.Copy), [], [ps, sf])
                                DMA("sp", sf, lambda e: [e.dma_start(out=dst[tt * 128:(tt + 1) * 128, :], in_=sf[:])], reads=[sf])
                                return
                            sg = stg[i % 4]
                            if kind == "gelu":
                                gelu_evac(ps, w, sg, (t1p[i % 2], t2p[i % 2]))
                            else:
                                I("act", lambda e: e.activation(out=sg[:], in_=ps[:], func=AF.Sigmoid), [], [ps, sg])
                            DMA("sp", sg, lambda e: [e.dma_start(out=dst[tt * 128:(tt + 1) * 128, half * 512:(half + 1) * 512],
                                                                 in_=sg[:])], reads=[sg])
                        proj_tm(wt, ncols, evac, hTp)
                sc.barrier()

        def phase_even_mix(ei):
            with ExitStack() as st:
                alg = sbt(st, "alg", [128, D], F32)
                wsf = sbt(st, "wsf", [128, 8, 128], F32)
                wsb = sbt(st, "wsb", [128, 8, 128], BF16, False)
                bsT = sbt(st, "bsT", [128, 8], F32)
                cw = sbt(st, "cw", [128, 8, 4], F32)
                cv = sbt(st, "cv", [128, 3, 8], F32)
                gb = sbt(st, "gb", [128, 8], F32)
                Wb = sbt(st, "Wb", [128, 3, 8, 256], BF16, False)
                GN = sbt(st, "GN", [128, 8, 128], F32, False)
                SK = sbt(st, "SK", [128, 8, 128], F32, False)
                tri4 = sbt(st, "tri4", [128, 4, 128], F32, False)
                DMA("sp", alg, lambda e: [e.dma_start(out=alg[:], in_=a_ln_g[ei])], writes=[alg])
                DMA("sp", wsf, lambda e: [e.dma_start(out=wsf[:], in_=a_wsT[ei].rearrange("g s t -> s g t"))], writes=[wsf])
                DMA("sp", bsT, lambda e: [e.dma_start(out=bsT[:], in_=a_bsT[ei])], writes=[bsT])
                DMA("sp", cw, lambda e: [e.dma_start(out=cw[:], in_=convw[ei])], writes=[cw])
                DMA("sp", cv, lambda e: [e.dma_start(out=cv[:], in_=colv[ei])], writes=[cv])
                DMA("sp", gb, lambda e: [e.dma_start(out=gb[:], in_=gate_b[ei])], writes=[gb])
                Wbs = sbt(st, "Wbs", [128, 8, 256], F32)
                for j in range(3):
                    DMA("sp", Wbs, lambda e, j=j: [e.dma_start(out=Wbs[:], in_=b_wqkv[ei, j].rearrange("h (dc p) e -> p (h dc) e", p=128))],
                        writes=[Wbs])
                    I("pool", lambda e, j=j: e.tensor_copy(out=Wb[:, j, :, :], in_=Wbs[:]), [Wbs], [(Wb, j)])
                Wb_keys = [(Wb, j) for j in range(3)]
                for g in range(8):
                    I("dve", lambda e, g=g: e.tensor_tensor(out=wsb[:, g, :], in0=wsf[:, g, :], in1=tri_f[:], op=ALU.mult),
                      [wsf, tri_f], [(wsb, g)])
                    I("dve", lambda e, g=g: e.tensor_scalar(out=GN[:, g, :], in0=ones_f[:], scalar1=cv[:, 1, g:g + 1], scalar2=None,
                                                            op0=ALU.mult), [ones_f, cv], [(GN, g)])
                    I("dve", lambda e, g=g: e.tensor_scalar(out=SK[:, g, :], in0=ones_f[:], scalar1=cv[:, 2, g:g + 1], scalar2=None,
                                                            op0=ALU.mult), [ones_f, cv], [(SK, g)])
                for h in range(4):
                    I("dve", lambda e, h=h: e.tensor_copy(out=tri4[:, h, :], in_=tri_f[:]), [tri_f], [(tri4, h)])
                wsb_keys = [(wsb, g) for g in range(8)]
                GN_keys = [(GN, g) for g in range(8)]
                SK_keys = [(SK, g) for g in range(8)]
                tri4_keys = [(tri4, h) for h in range(4)]

                tmps = {}

                def tmp(name, shape, dt, n=2):
                    if name not in tmps:
                        tmps[name] = [pool(st, "t" + name, shape, dt, n, False), 0]
                    p = tmps[name]
                    t = p[0][p[1] % n]
                    p[1] += 1
                    return t

                Cst = sbt(st, "Cst", [128, 4, 2, 260], F32, False)
                Cb = sbt(st, "Cb", [128, 4, 2, 260], BF16, False)

                up = pool(st, "mu", [128, D], BF16, 2)
                vap = pool(st, "mva", [128, D], BF16, 2)
                ogp = pool(st, "mog", [128, D], BF16, 2)
                gtp = pool(st, "mgt", [128, 8], F32, 2)
                zap = pool(st, "mza", [128, 8, 128], BF16, 2)
                zbp = pool(st, "mzb", [128, 8, 128], BF16, 2)
                xmp = pool(st, "mxm", [128, 8, 131], BF16, 2)
                yap = pool(st, "mya", [128, 8, 128], BF16, 2)
                ybp = pool(st, "myb", [128, 8, 128], BF16, 2)
                def tile_loads(tt):
                    c = tt % CPS
                    tok0 = tt * 128
                    b2 = tt % 2
                    ut, vat, ogt, gtt, zat, zbt, xmt = up[b2], vap[b2], ogp[b2], gtp[b2], zap[b2], zbp[b2], xmp[b2]
                    rows = slice(tok0, tok0 + 128)
                    DMA("sp", ut, lambda e, ut=ut, rows=rows: [e.dma_start(out=ut[:], in_=u_s[rows, :])], writes=[ut])
                    DMA("sp", vat, lambda e, vat=vat, rows=rows: [e.dma_start(out=vat[:], in_=va_s[rows, :])], writes=[vat])
                    DMA("sp", ogt, lambda e, ogt=ogt, rows=rows: [e.dma_start(out=ogt[:], in_=og_s[rows, :])], writes=[ogt])
                    DMA("sp", gtt, lambda e, gtt=gtt, rows=rows: [e.dma_start(out=gtt[:], in_=gt_s[rows, :])], writes=[gtt])
                    DMA("sp", zat, lambda e, zat=zat, rows=rows: [e.dma_start(
                        out=zat[:], in_=zaT_s[:, :, rows].rearrange("k p t -> p k t"))], writes=[zat])
                    DMA("sp", zbt, lambda e, zbt=zbt, rows=rows: [e.dma_start(
                        out=zbt[:], in_=zbT_s[:, :, rows].rearrange("k p t -> p k t"))], writes=[zbt])
                    if c == 0:
                        I("dve", lambda e, xmt=xmt: e.memset(xmt[:, :, 0:3], 0.0), [], [xmt])
                        DMA("sp", xmt, lambda e, xmt=xmt, rows=rows: [e.dma_start(
                            out=xmt[:, :, 3:131], in_=xmT_s[:, :, rows].rearrange("k p t -> p k t"))], writes=[xmt])
                    else:
                        DMA("sp", xmt, lambda e, xmt=xmt, tok0=tok0: [e.dma_start(
                            out=xmt[:], in_=xmT_s[:, :, tok0 - 3:tok0 + 128].rearrange("k p t -> p k t"))], writes=[xmt])


                def do_tile(tt):
                    c = tt % CPS
                    tok0 = tt * 128
                    b2 = tt % 2
                    ut, vat, ogt, gtt, zat, zbt, xmt = up[b2], vap[b2], ogp[b2], gtp[b2], zap[b2], zbp[b2], xmp[b2]
                    rows = slice(tok0, tok0 + 128)
                    if tt + 1 < NT:
                        tile_loads(tt + 1)
                    bst = tmp("bst", [128, 2, 6], F32)
                    mv = tmp("mv", [128, 8], F32)
                    for hf in range(2):
                        I("dve", lambda e, hf=hf, bst=bst, vat=vat: e.bn_stats(out=bst[:, hf, :], in_=vat[:, hf * 512:(hf + 1) * 512]),
                          [vat], [(bst, hf)])
                    I("dve", lambda e, bst=bst, mv=mv: e.bn_aggr(out=mv[:, 0:2], in_=bst[:].rearrange("p a b -> p (a b)")),
                      [(bst, 0), (bst, 1)], [(mv, 0), (mv, 1)])
                    rstd_chain(mv, 1, 1.0, 2)
                    vn = tmp("vn", [128, D], F32, 1)
                    vnb = tmp("vnb", [128, D], BF16)
                    I("dve", lambda e, vn=vn, vat=vat, mv=mv: e.tensor_scalar(out=vn[:], in0=vat[:], scalar1=mv[:, 0:1], scalar2=mv[:, 2:3],
                                                                              op0=ALU.subtract, op1=ALU.mult), [vat, (mv, 0), (mv, 2)], [vn])
                    I("pool", lambda e, vn=vn, vnb=vnb: e.tensor_tensor(out=vnb[:], in0=vn[:], in1=alg[:], op=ALU.mult), [vn, alg], [vnb])
                    psA = [ps_next(), ps_next()]
                    for g in range(8):
                        pg = psA[g // 4]
                        I("pe", lambda e, g=g, pg=pg, vnb=vnb: e.matmul(pg[:, (g % 4) * 128:(g % 4 + 1) * 128], lhsT=wsb[:, g, :],
                                                                        rhs=vnb[:, g * 128:(g + 1) * 128], start=True, stop=True),
                          [(wsb, g), vnb], [pg])
                    ya = tmp("ya", [128, D], BF16)
                    for g in range(8):
                        pg = psA[g // 4]
                        I("dve", lambda e, g=g, pg=pg, ya=ya, ut=ut: e.scalar_tensor_tensor(
                            out=ya[:, g * 128:(g + 1) * 128], in0=pg[:, (g % 4) * 128:(g % 4 + 1) * 128], scalar=bsT[:, g:g + 1],
                            in1=ut[:, g * 128:(g + 1) * 128], op0=ALU.add, op1=ALU.mult), [bsT, ut], [pg, (ya, g)])
                    for g in range(8):
                        I("pe", lambda e, g=g, ya=ya: e.transpose(out=psB[:, g * 128:(g + 1) * 128], in_=ya[:, g * 128:(g + 1) * 128],
                                                                  identity=ident_b[:]), [(ya, g), ident_b], [psB])
                    yat = yap[b2]
                    I("dve", lambda e, yat=yat, zat=zat: e.tensor_tensor(out=yat[:], in0=psB[:].rearrange("p (k t) -> p k t", k=8),
                                                                         in1=zat[:], op=ALU.mult), [zat], [psB, yat])
                    DMA("sp", yat, lambda e, yat=yat, rows=rows: [e.dma_start(out=yT[0:8, :, rows].rearrange("k p t -> p k t"), in_=yat[:])],
                        reads=[yat])

                    xa = tmp("xa", [128, 8, 128], F32, 1)
                    for j in range(8):
                        I("dve", lambda e, j=j, xa=xa, xmt=xmt: e.tensor_scalar(
                            out=xa[:, j, :], in0=xmt[:, j, 0:128], scalar1=cw[:, j, 0:1], scalar2=cv[:, 0, j:j + 1],
                            op0=ALU.mult, op1=ALU.add), [xmt, cw, cv], [(xa, j)])
                    for k in range(1, 4):
                        for j in range(8):
                            I("dve", lambda e, j=j, k=k, xa=xa, xmt=xmt: e.scalar_tensor_tensor(
                                out=xa[:, j, :], in0=xmt[:, j, k:k + 128], scalar=cw[:, j, k:k + 1], in1=xa[:, j, :],
                                op0=ALU.mult, op1=ALU.add), [xmt, cw, (xa, j)], [(xa, j)])
                    xc = tmp("xc", [128, 8, 128], BF16)
                    I("act", lambda e, xa=xa, xc=xc: e.activation(out=xc[:], in_=xa[:], func=AF.Silu), [(xa, j) for j in range(8)], [xc])
                    qb_ = tmp("qb", [128, 4, 2, 128], BF16)
                    kb_ = tmp("kb", [128, 4, 2, 128], BF16)
                    qs_ = tmp("qs", [128, 4, 2, 128], BF16)
                    psq = [ps_next(), ps_next()]
                    for (wi, pss, dstb) in ((0, psq, qb_), (1, [ps_next(), ps_next()], kb_)):
                        for h in range(4):
                            pg = pss[h // 2]
                            for ec in range(2):
                                o0 = (h % 2) * 256 + ec * 128
                                for dc in range(2):
                                    I("pe", lambda e, wi=wi, pg=pg, h=h, ec=ec, dc=dc, o0=o0, xc=xc: e.matmul(
                                        pg[:, o0:o0 + 128], lhsT=Wb[:, wi, h * 2 + dc, ec * 128:(ec + 1) * 128], rhs=xc[:, h * 2 + dc, :],
                                        start=(dc == 0), stop=(dc == 1)), Wb_keys + [xc], [pg])
                        for bk in range(2):
                            I("act", lambda e, pss=pss, bk=bk, dstb=dstb, wi=wi: e.activation(
                                out=dstb[:, bk * 2:bk * 2 + 2, :, :], in_=pss[bk][:].rearrange("p (h c t) -> p h c t", h=2, c=2),
                                func=AF.Copy, scale=(1.0 if wi == 0 else 0.0625)), [], [pss[bk], (dstb, bk)])
                    psk = [ps_next(), ps_next()]
                    for h in range(4):
                        for dc in range(2):
                            I("pe", lambda e, h=h, dc=dc, xc=xc: e.matmul(
                                psk[h // 2][:, (h % 2) * 256:(h % 2 + 1) * 256], lhsT=xc[:, h * 2 + dc, :], rhs=Wb[:, 1, h * 2 + dc, :],
                                start=(dc == 0), stop=(dc == 1)), Wb_keys + [xc], [psk[h // 2]])
                    ktm = tmp("ktm", [128, 4, 256], BF16)
                    for bk in range(2):
                        I("act", lambda e, bk=bk, ktm=ktm: e.activation(
                            out=ktm[:, bk * 2:bk * 2 + 2, :], in_=psk[bk][:].rearrange("p (h e) -> p h e", h=2), func=AF.Copy,
                            scale=0.0625), [], [psk[bk], (ktm, bk)])
                    va_ = tmp("vaug", [128, 4, 260], BF16)
                    I("pool", lambda e, va_=va_: e.memset(va_[:, :, 256:257], 1.0), [], [(va_, "o")])
                    for bk in range(2):
                        psv = ps_next()
                        for hh in range(2):
                            h = bk * 2 + hh
                            for dc in range(2):
                                I("pe", lambda e, h=h, hh=hh, dc=dc, psv=psv, xmt=xmt: e.matmul(
                                    psv[:, hh * 256:(hh + 1) * 256], lhsT=xmt[:, h * 2 + dc, 3:131], rhs=Wb[:, 2, h * 2 + dc, :],
                                    start=(dc == 0), stop=(dc == 1)), Wb_keys + [xmt], [psv])
                        I("act", lambda e, psv=psv, bk=bk, va_=va_: e.activation(
                            out=va_[:, bk * 2:bk * 2 + 2, 0:256], in_=psv[:].rearrange("p (h e) -> p h e", h=2), func=AF.Copy),
                          [], [psv, (va_, bk)])
                    gs = tmp("gs", [128, 40], F32)
                    I("dve", lambda e, gs=gs, gtt=gtt: e.tensor_tensor(out=gs[:, 0:8], in0=gtt[:], in1=gb[:], op=ALU.add), [gtt, gb], [(gs, 0)])
                    I("act", lambda e, gs=gs: e.activation(out=gs[:, 8:12], in_=gs[:, 4:8], func=AF.Exp, scale=-1.0), [(gs, 0)], [(gs, 1)])
                    I("dve", lambda e, gs=gs: e.tensor_scalar(out=gs[:, 12:16], in0=gs[:, 8:12], scalar1=1.0, scalar2=None, op0=ALU.add),
                      [(gs, 1)], [(gs, 2)])
                    I("act", lambda e, gs=gs: e.activation(out=gs[:, 16:20], in_=gs[:, 12:16], func=AF.Ln), [(gs, 2)], [(gs, 3)])
                    psg = ps_next()
                    I("pe", lambda e, gs=gs, psg=psg: e.matmul(psg[:, 0:4], lhsT=tri_f[:], rhs=gs[:, 16:20], start=True, stop=True),
                      [tri_f, (gs, 3)], [psg])
                    I("dve", lambda e, gs=gs, psg=psg: e.tensor_copy(out=gs[:, 20:24], in_=psg[:, 0:4]), [], [psg, (gs, 4)])
                    I("dve", lambda e, gs=gs: e.tensor_tensor(out=gs[:, 24:28], in0=gs[:, 0:4], in1=gs[:, 20:24], op=ALU.add),
                      [(gs, 0), (gs, 4)], [(gs, 5)])
                    dg = tmp("dg", [128, 4, 128], F32, 1)
                    for h in range(4):
                        I("dve", lambda e, h=h, dg=dg, gs=gs: e.tensor_scalar(out=dg[:, h, :], in0=ident_f[:], scalar1=gs[:, 20 + h:21 + h],
                                                                              scalar2=-1.0, op0=ALU.mult, op1=ALU.mult),
                          [ident_f, (gs, 4)], [(dg, h)])
                    psr = ps_next()
                    I("pe", lambda e, psr=psr, dg=dg: e.matmul(psr[:], lhsT=ones_f[:], rhs=dg[:].rearrange("p h t -> p (h t)"),
                                                               start=True, stop=True), [ones_f] + [(dg, h) for h in range(4)], [psr])
                    PT = tmp("PT", [128, 4, 128], F32, 1)
                    for h in range(4):
                        I("act", lambda e, h=h, PT=PT, psr=psr, gs=gs: e.activation(
                            out=PT[:, h, :], in_=psr[:, h * 128:(h + 1) * 128], func=AF.Exp, bias=gs[:, 24 + h:25 + h]),
                          [(gs, 5)], [psr, (PT, h)])
                    EB = tmp("EB", [128, 4, 128], F32, 1)
                    I("act", lambda e, EB=EB, psr=psr: e.activation(out=EB[:].rearrange("p h t -> p (h t)"), in_=psr[:], func=AF.Exp),
                      [], [psr, EB])
                    I("dve", lambda e, gs=gs, psr=psr: e.tensor_copy(
                        out=gs[:, 28:32], in_=psr[:].rearrange("p (h t) -> p h t", h=4)[:, :, 127]), [], [psr, (gs, 6)])
                    I("pool", lambda e, PT=PT: e.tensor_tensor(out=PT[:], in0=PT[:], in1=tri4[:], op=ALU.mult),
                      tri4_keys + [(PT, h) for h in range(4)], [(PT, h) for h in range(4)])
                    if c > 0:
                        for bk in range(2):
                            for ec in range(2):
                                I("dve", lambda e, bk=bk, ec=ec, qs_=qs_, EB=EB, qb_=qb_: e.tensor_tensor(
                                    out=qs_[:, bk * 2:bk * 2 + 2, ec, :], in0=qb_[:, bk * 2:bk * 2 + 2, ec, :],
                                    in1=EB[:, bk * 2:bk * 2 + 2, :], op=ALU.mult), [EB, (qb_, bk)], [(qs_, bk, ec)])
                    psqk = ps_next()
                    for h in range(4):
                        for ec in range(2):
                            I("pe", lambda e, h=h, ec=ec, psqk=psqk, kb_=kb_, qb_=qb_: e.matmul(
                                psqk[:, h * 128:(h + 1) * 128], lhsT=kb_[:, h, ec, :], rhs=qb_[:, h, ec, :],
                                start=(ec == 0), stop=(ec == 1)), [(kb_, 0), (kb_, 1), (qb_, 0), (qb_, 1)], [psqk])
                    scT = tmp("scT", [128, 4, 128], BF16)
                    I("dve", lambda e, scT=scT, psqk=psqk, PT=PT: e.tensor_tensor(
                        out=scT[:].rearrange("p h t -> p (h t)"), in0=psqk[:], in1=PT[:].rearrange("p h t -> p (h t)"), op=ALU.mult),
                      [(PT, h) for h in range(4)], [psqk, scT])
                    htn = tmp("htn", [128, D], BF16)
                    for h in range(4):
                        acc = ps_next()
                        I("pe", lambda e, h=h, acc=acc, scT=scT, va_=va_: e.matmul(
                            acc[:, 0:257], lhsT=scT[:, h, :], rhs=va_[:, h, 0:257], start=True, stop=(c == 0)),
                          [scT, (va_, "o"), (va_, h // 2)], [acc])
                        if c > 0:
                            for dc in range(2):
                                I("pe", lambda e, h=h, dc=dc, acc=acc, qs_=qs_: e.matmul(
                                    acc[:, 0:257], lhsT=qs_[:, h, dc, :], rhs=Cb[:, h, dc, 0:257], start=False, stop=(dc == 1)),
                                  [(qs_, h // 2, dc), (Cb, h, dc)], [acc])
                        hs = tmp("hs", [128, 16], F32)
                        ht = tmp("ht", [128, 256], F32)
                        I("act", lambda e, hs=hs, acc=acc: e.activation(out=hs[:, 8:9], in_=acc[:, 256:257], func=AF.Abs),
                          [], [acc, (hs, 8)])
                        I("dve", lambda e, hs=hs: e.tensor_scalar(out=hs[:, 10:11], in0=hs[:, 8:9], scalar1=1.0, scalar2=None,
                                                                  op0=ALU.max), [(hs, 8)], [(hs, 10)])
                        I("dve", lambda e, hs=hs: e.reciprocal(out=hs[:, 9:10], in_=hs[:, 10:11]), [(hs, 10)], [(hs, 9)])
                        I("dve", lambda e, hs=hs, acc=acc, ht=ht, h=h, ogt=ogt: e.scalar_tensor_tensor(
                            out=ht[:], in0=acc[:, 0:256], scalar=hs[:, 9:10], in1=ogt[:, h * 256:(h + 1) * 256],
                            op0=ALU.mult, op1=ALU.mult), [(hs, 9), ogt], [acc, ht])
                        bs2 = tmp("bs2", [128, 6], F32)
                        I("dve", lambda e, bs2=bs2, ht=ht: e.bn_stats(out=bs2[:], in_=ht[:]), [ht], [bs2])
                        I("dve", lambda e, bs2=bs2, hs=hs: e.bn_aggr(out=hs[:, 0:2], in_=bs2[:]), [bs2], [(hs, 0), (hs, 1)])
                        rstd_chain(hs, 1, 1.0, 2)
                        I("dve", lambda e, hs=hs, ht=ht, htn=htn, h=h: e.tensor_scalar(
                            out=htn[:, h * 256:(h + 1) * 256], in0=ht[:], scalar1=hs[:, 0:1], scalar2=hs[:, 2:3],
                            op0=ALU.subtract, op1=ALU.mult), [ht, (hs, 0), (hs, 2)], [(htn, h)])
                    for g in range(8):
                        I("pe", lambda e, g=g, htn=htn: e.transpose(out=psB[:, g * 128:(g + 1) * 128], in_=htn[:, g * 128:(g + 1) * 128],
                                                                    identity=ident_b[:]), [(htn, g // 2), ident_b], [psB])
                    y1 = tmp("y1", [128, 8, 128], F32, 1)
                    y2 = tmp("y2", [128, 8, 128], F32, 1)
                    I("dve", lambda e, y1=y1: e.tensor_tensor(out=y1[:], in0=psB[:].rearrange("p (k t) -> p k t", k=8), in1=GN[:],
                                                              op=ALU.mult), GN_keys, [psB, y1])
                    I("pool", lambda e, y2=y2, xc=xc: e.tensor_tensor(out=y2[:], in0=xc[:], in1=SK[:], op=ALU.mult), SK_keys + [xc], [y2])
                    I("dve", lambda e, y1=y1, y2=y2: e.tensor_tensor(out=y1[:], in0=y1[:], in1=y2[:], op=ALU.add), [y1, y2], [y1])
                    ybt = ybp[b2]
                    I("dve", lambda e, y1=y1, ybt=ybt, zbt=zbt: e.tensor_tensor(out=ybt[:], in0=y1[:], in1=zbt[:], op=ALU.mult),
                      [y1, zbt], [ybt])
                    DMA("sp", ybt, lambda e, ybt=ybt, rows=rows: [e.dma_start(out=yT[8:16, :, rows].rearrange("k p t -> p k t"), in_=ybt[:])],
                        reads=[ybt])
                    if c < CPS - 1:
                        I("dve", lambda e, gs=gs: e.tensor_tensor(out=gs[:, 32:36], in0=gs[:, 24:28], in1=gs[:, 28:32], op=ALU.add),
                          [(gs, 5), (gs, 6)], [(gs, 7)])
                        I("act", lambda e, gs=gs: e.activation(out=gs[:, 32:36], in_=gs[:, 32:36], func=AF.Exp), [(gs, 7)], [(gs, 7)])
                        I("act", lambda e, gs=gs: e.activation(out=gs[:, 36:40], in_=gs[:, 28:32], func=AF.Exp), [(gs, 6)], [(gs, 8)])
                        kw = tmp("kw", [128, 4, 256], BF16)
                        for h in range(4):
                            I("dve", lambda e, h=h, kw=kw, gs=gs, ktm=ktm: e.tensor_scalar(
                                out=kw[:, h, :], in0=ktm[:, h, :], scalar1=gs[:, 32 + h:33 + h],
                                scalar2=None, op0=ALU.mult), [(gs, 7), (ktm, h // 2)], [(kw, h)])
                        for h in range(4):
                            for dc in range(2):
                                psu = ps_next()
                                I("pe", lambda e, h=h, dc=dc, psu=psu, kw=kw, va_=va_: e.matmul(
                                    psu[:, 0:257], lhsT=kw[:, h, dc * 128:(dc + 1) * 128], rhs=va_[:, h, 0:257], start=True, stop=True),
                                  [(kw, h), (va_, "o"), (va_, h // 2)], [psu])
                                if c == 0:
                                    I("dve", lambda e, h=h, dc=dc, psu=psu: e.tensor_copy(out=Cst[:, h, dc, 0:257], in_=psu[:, 0:257]),
                                      [], [psu, (Cst, h, dc)])
                                else:
                                    I("dve", lambda e, h=h, dc=dc, psu=psu, gs=gs: e.scalar_tensor_tensor(
                                        out=Cst[:, h, dc, 0:257], in0=Cst[:, h, dc, 0:257], scalar=gs[:, 36 + h:37 + h], in1=psu[:, 0:257],
                                        op0=ALU.mult, op1=ALU.add), [(gs, 8)], [psu, (Cst, h, dc)])
                                I("act", lambda e, h=h, dc=dc: e.activation(out=Cb[:, h, dc, 0:257], in_=Cst[:, h, dc, 0:257], func=AF.Copy),
                                  [(Cst, h, dc)], [(Cb, h, dc)])
                tile_loads(0)
                for tt in range(NT):
                    do_tile(tt)
                sc.barrier()

        import os as _os
        STOP = _os.environ.get("KSTOP", "")

        def phase_copyout():
            with ExitStack() as st:
                xp = pool(st, "cox", [128, D], F32, 2)
                for tt in range(NT):
                    xt = xp[tt % 2]
                    DMA("sp", xt, lambda e, xt=xt, tt=tt: [e.dma_start(out=xt[:], in_=xs[tt * 128:(tt + 1) * 128, :])], writes=[xt])
                    DMA("sp", xt, lambda e, xt=xt, tt=tt: [e.dma_start(out=out[tt * 128:(tt + 1) * 128, :], in_=xt[:])], reads=[xt])
                sc.barrier()

        def run_all():
            phase_norm0(layers[0][2])
            if STOP == "norm0":
                return phase_copyout()
            for li, (kind, pi, lno) in enumerate(layers):
                if kind == "odd":
                    phase_odd_inproj(pi)
                    if STOP == "inproj":
                        return phase_copyout()
                    phase_attn(pi, lno)
                    if STOP == "attn":
                        return phase_copyout()
                    phase_out(li, od_w_out[pi])
                else:
                    phase_even_inproj(pi)
                    if STOP == "inproj":
                        return phase_copyout()
                    phase_even_mix(pi)
                    if STOP == "attn":
                        return phase_copyout()
                    phase_out(li, ev_w_out[pi])

        run_all()
        sc.barrier(["sp"])
        sc.emit()
    return nc


def prep_inputs(inputs, S):
    f = lambda a: np.ascontiguousarray(np.asarray(a, dtype=np.float32))
    rep = lambda a: np.ascontiguousarray(np.broadcast_to(f(a)[None], (128,) + tuple(np.shape(a))))
    shared = {}
    shared["gvec"] = rep(np.concatenate([f(inputs["norm_g"]), f(inputs["final_g"])[None]], axis=0))
    shared["ident"] = np.eye(128, dtype=np.float32)
    shared["tri"] = np.triu(np.ones((128, 128), np.float32))
    cosT, sinT = rope_tables_T(S)
    shared["cosT"], shared["sinT"] = cosT, sinT
    shared["ev_w_in"] = f(inputs["ev_w_in"])
    shared["ev_w_out"] = f(inputs["ev_w_out"])
    w = f(inputs["od_w_in"])
    sw = np.concatenate([np.arange(32, 64), np.arange(0, 32)])
    cols = []
    for base in (0, 2048):
        for h in range(16):
            for cmp_ in range(2):
                cols.append(base + h * 128 + cmp_ * 64 + np.arange(64))
            for cmp_ in range(2):
                cols.append(base + h * 128 + cmp_ * 64 + sw)
    cols.append(6144 + np.arange(2048))
    cols.append(4096 + np.arange(2048))
    cols = np.concatenate(cols)
    shared["od_w_in"] = np.ascontiguousarray(w[:, :, cols])
    shared["od_w_out"] = f(inputs["od_w_out"])
    shared["a_ln_g"] = np.ascontiguousarray(np.broadcast_to(f(inputs["a_ln_g"])[:, None, :], (2, 128, D)))
    shared["a_wsT"] = np.ascontiguousarray(np.swapaxes(f(inputs["a_ws"]), 2, 3))
    shared["a_bsT"] = np.ascontiguousarray(np.swapaxes(f(inputs["a_bs"]), 1, 2))
    cw = f(inputs["b_conv_w"])
    shared["convw"] = np.ascontiguousarray(cw.reshape(2, 4, 8, 128).transpose(0, 3, 2, 1))
    colv = np.stack([f(inputs["b_conv_b"]), f(inputs["b_gn_g"]), f(inputs["b_skip"])], axis=1)
    shared["colv"] = np.ascontiguousarray(colv.reshape(2, 3, 8, 128).transpose(0, 3, 1, 2))
    shared["b_wqkv"] = np.ascontiguousarray(np.stack([f(inputs["b_wq"]), f(inputs["b_wk"]), f(inputs["b_wv"])], axis=1))
    gb = np.concatenate([f(inputs["b_ig_b"]), f(inputs["b_fg_b"])], axis=1)
    shared["gate_b"] = np.ascontiguousarray(np.broadcast_to(gb[:, None, :], (2, 128, 8)))
    lv = np.stack([f(inputs["c_lam_q1"]), f(inputs["c_lam_k1"]), f(inputs["c_lam_q2"]), f(inputs["c_lam_k2"])], axis=1)
    shared["lamv"] = np.ascontiguousarray(np.broadcast_to(lv[:, None], (2, 128, 4, 64)))
    shared["subln"] = np.ascontiguousarray(f(inputs["c_subln_g"]).reshape(2, 128, 1))
    return shared


_PROG_CACHE = {}


def kernel(**inputs):
    x = np.ascontiguousarray(inputs["x"], dtype=np.float32)
    B, S, _ = x.shape
    NS = B // NCORES
    key = (NS, S)
    if key not in _PROG_CACHE:
        _PROG_CACHE[key] = build_program(NS, S)
    nc = _PROG_CACHE[key]
    shared = prep_inputs(inputs, S)
    in_maps = []
    for c in range(NCORES):
        m = dict(shared)
        m["x"] = x[c * NS:(c + 1) * NS].reshape(NS * S, D)
        in_maps.append(m)
    res = run_bass_kernel_spmd(nc, in_maps, core_ids=list(range(NCORES)))
    outs = [r["out"].reshape(NS, S, D) for r in res.results]
    return np.concatenate(outs, axis=0)
```

```python
import math
from contextlib import ExitStack

import numpy as np
import concourse.bass as bass
import concourse.mybir as mybir
from concourse.bass_utils import run_bass_kernel_spmd

F32 = mybir.dt.float32
BF16 = mybir.dt.bfloat16
AF = mybir.ActivationFunctionType
ALU = mybir.AluOpType
AX = mybir.AxisListType

D = 1024
NCORES = 8
EPS = 1e-6
EVEN_IN = 6152
OD_EXT = 12288
N_DMA_SEMS = 84
FULL_LAYERS = (("even", 0, 0), ("odd", 0, 1), ("even", 1, 2), ("odd", 1, 3))


class Sched:
    ENGS = ("pe", "act", "dve", "pool", "sp")

    def __init__(self, nc, es):
        self.nc = nc
        self.es = es
        self.prog = {e: [] for e in self.ENGS}
        self.idx = {e: 0 for e in self.ENGS}
        self.seen = {e: {} for e in self.ENGS}
        self.cnt = {}
        self.last_w = {}
        self.readers = {}
        self.eng_sem = {}
        for e in ("pe", "act", "dve", "pool"):
            self.eng_sem[e] = self.new_sem("c_" + e)
        self.dma_sems = [self.new_sem(f"dq{i}") for i in range(N_DMA_SEMS)]
        self.dma_i = 0
        self.n_ops = 0

    def new_sem(self, name):
        s = self.es.enter_context(self.nc.semaphore(name))
        self.cnt[s] = 0
        return s

    def get_dma_sem(self):
        s = self.dma_sems[self.dma_i % N_DMA_SEMS]
        self.dma_i += 1
        return s

    def op(self, eng, fn, reads=(), writes=(), dma_sem=None, ndma=1):
        deps = []
        for k in reads:
            t = self.last_w.get(k)
            if t is not None:
                deps.append(t)
        for k in writes:
            t = self.last_w.get(k)
            if t is not None:
                deps.append(t)
            deps.extend(self.readers.get(k, ()))
        need = {}
        seen = self.seen[eng]
        cur = self.idx[eng]
        for (sem, val, teng, tidx, is_dma) in deps:
            if teng == eng and not is_dma:
                if eng == "pe":
                    continue
            if seen.get(sem, 0) >= val:
                continue
            if need.get(sem, 0) < val:
                need[sem] = val
        if dma_sem is not None:
            prev = self.cnt[dma_sem]
            if prev > 0 and seen.get(dma_sem, 0) < prev:
                need[dma_sem] = prev
        for sem, val in need.items():
            self.prog[eng].append(("w", sem, val))
            seen[sem] = val
        if dma_sem is None:
            sem = self.eng_sem[eng]
            self.cnt[sem] += 1
            is_dma = False
        else:
            sem = dma_sem
            self.cnt[sem] += 16 * ndma
            is_dma = True
        tok = (sem, self.cnt[sem], eng, cur, is_dma)
        self.prog[eng].append(("o", fn, sem))
        for k in writes:
            self.last_w[k] = tok
            self.readers[k] = []
        for k in reads:
            self.readers.setdefault(k, []).append(tok)
        self.idx[eng] = cur + 1
        self.n_ops += 1
        return tok

    def barrier(self, engs=None):
        for eng in (engs or self.ENGS):
            for sem, val in self.cnt.items():
                if val > 0 and self.seen[eng].get(sem, 0) < val:
                    self.prog[eng].append(("w", sem, val))
                    self.seen[eng][sem] = val
        if engs is None:
            self.last_w = {}
            self.readers = {}

    def emit(self):
        nc = self.nc
        engobj = {"pe": "tensor", "act": "scalar", "dve": "vector", "pool": "gpsimd", "sp": "sync"}
        with nc.Block() as block:
            for e in self.ENGS:
                prog = self.prog[e]

                def body(eng, prog=prog):
                    for it in prog:
                        if it[0] == "w":
                            eng.wait_ge(it[1], it[2])
                        else:
                            it[1](eng, it[2])

                getattr(block, engobj[e])(body)


class Tl:
    def __init__(self, t, sem=None):
        self.t = t
        self.sem = sem

    def __getitem__(self, k):
        return self.t[k]


def rope_tables_T(S):
    inv = (10000.0 ** (-np.arange(0, 64, 2, dtype=np.float32) / np.float32(64))).astype(np.float32)
    ang = (np.arange(S, dtype=np.float32)[:, None] * inv[None, :]).astype(np.float32)
    cos, sin = np.cos(ang).astype(np.float32), np.sin(ang).astype(np.float32)
    cosT = np.zeros((128, S), np.float32)
    sinT = np.zeros((128, S), np.float32)
    for p in range(128):
        d = p % 64
        cosT[p] = cos[:, d % 32]
        sinT[p] = -sin[:, d % 32] if d < 32 else sin[:, d % 32]
    return cosT, sinT


def lambda_init(layer):
    return 0.8 - 0.6 * math.exp(-0.3 * layer)


def build_program(NS, S, layers=FULL_LAYERS, dbg=False):
    T = NS * S
    NT = T // 128
    NC5 = T // 512
    CPS = S // 128
    n_layers = len(layers)
    nc = bass.Bass("TRN2", target_bir_lowering=False)
    es = ExitStack()

    def din(name, shape, dt=F32):
        return nc.dram_tensor(name, list(shape), dt, kind="ExternalInput").ap()

    def dscr(name, shape, dt):
        if dbg:
            return nc.dram_tensor(name, list(shape), dt, kind="ExternalOutput").ap()
        return nc.dram_tensor(name, list(shape), dt).ap()

    x_in = din("x", [T, D])
    gvec = din("gvec", [128, 5, D])
    ident_d = din("ident", [128, 128])
    tri_d = din("tri", [128, 128])
    cosT_d = din("cosT", [128, S])
    sinT_d = din("sinT", [128, S])
    ev_w_in = din("ev_w_in", [2, D, EVEN_IN])
    ev_w_out = din("ev_w_out", [2, 2048, D])
    od_w_in = din("od_w_in", [2, D, OD_EXT])
    od_w_out = din("od_w_out", [2, 2048, D])
    a_ln_g = din("a_ln_g", [2, 128, D])
    a_wsT = din("a_wsT", [2, 8, 128, 128])
    a_bsT = din("a_bsT", [2, 128, 8])
    convw = din("convw", [2, 128, 8, 4])
    colv = din("colv", [2, 128, 3, 8])
    b_wqkv = din("b_wqkv", [2, 3, 4, 256, 256])
    gate_b = din("gate_b", [2, 128, 8])
    lamv = din("lamv", [2, 128, 4, 64])
    subln = din("subln", [2, 128, 1])
    out = nc.dram_tensor("out", [T, D], F32, kind="ExternalOutput").ap()

    xs = dscr("xs", [T, D], F32)
    hnT = dscr("hnT", [8, 128, T], BF16)
    yT = dscr("yT", [16, 128, T], BF16)
    qT_s = dscr("qT_s", [16, 128, T], BF16)
    kT_s = dscr("kT_s", [16, 128, T], BF16)
    zT_s = dscr("zT_s", [16, 128, T], BF16)
    v_s = dscr("v_s", [T, 2048], BF16)
    u_s = dscr("u_s", [T, D], BF16)
    va_s = dscr("va_s", [T, D], BF16)
    og_s = dscr("og_s", [T, D], BF16)
    gt_s = dscr("gt_s", [T, 8], F32)
    zaT_s = dscr("zaT_s", [8, 128, T], BF16)
    xmT_s = dscr("xmT_s", [8, 128, T], BF16)
    zbT_s = dscr("zbT_s", [8, 128, T], BF16)

    with es:
        sc = Sched(nc, es)

        def I(eng, f, reads=(), writes=()):
            sc.op(eng, lambda e, s: f(e).then_inc(s, 1), reads, writes)

        def DMA(eng, tl, f, reads=(), writes=(), n=1):
            def g(e, s):
                r = f(e)
                assert len(r) == n
                for x in r:
                    x.then_inc(s, 16)
            sc.op(eng, g, reads, writes, dma_sem=tl.sem, ndma=n)

        uid = [0]

        def sbt(st, name, shape, dt, dma=True):
            uid[0] += 1
            t = st.enter_context(nc.sbuf_tensor(f"{name}_{uid[0]}", list(shape), dt))
            return Tl(t, sc.get_dma_sem() if dma else None)

        def pool(st, name, shape, dt, n, dma=True):
            return [sbt(st, f"{name}{i}", shape, dt, dma) for i in range(n)]

        ident_f = sbt(es, "ident_f", [128, 128], F32)
        ident_b = sbt(es, "ident_b", [128, 128], BF16)
        tri_f = sbt(es, "tri_f", [128, 128], F32)
        tri_b = sbt(es, "tri_b", [128, 128], BF16)
        ones_f = sbt(es, "ones_f", [128, 128], F32)
        psF = [Tl(es.enter_context(nc.psum_tensor(f"psF{i}", [128, 512], F32))) for i in range(7)]
        psB = Tl(es.enter_context(nc.psum_tensor("psB", [128, 1024], BF16)))
        ps_i = [0]

        def ps_next():
            p = psF[ps_i[0] % len(psF)]
            ps_i[0] += 1
            return p

        DMA("sp", ident_f, lambda e: [e.dma_start(out=ident_f[:], in_=ident_d[:, :])], writes=[ident_f])
        DMA("sp", tri_f, lambda e: [e.dma_start(out=tri_f[:], in_=tri_d[:, :])], writes=[tri_f])
        I("dve", lambda e: e.tensor_copy(out=ident_b[:], in_=ident_f[:]), [ident_f], [ident_b])
        I("dve", lambda e: e.tensor_copy(out=tri_b[:], in_=tri_f[:]), [tri_f], [tri_b])
        I("dve", lambda e: e.memset(ones_f[:], 1.0), [], [ones_f])

        def rstd_chain(st, ss_col, scale, out_col):
            I("dve", lambda e: e.tensor_scalar(out=st[:, 6:7], in0=st[:, ss_col:ss_col + 1], scalar1=scale,
                                               scalar2=EPS, op0=ALU.mult, op1=ALU.add),
              [(st, ss_col)], [(st, 6)])
            I("act", lambda e: e.activation(out=st[:, 7:8], in_=st[:, 6:7], func=AF.Ln), [(st, 6)], [(st, 7)])
            I("act", lambda e: e.activation(out=st[:, out_col:out_col + 1], in_=st[:, 7:8], func=AF.Exp, scale=-0.5),
              [(st, 7)], [(st, out_col)])

        def norm_to_hnT(st_pools, xt, tt, gt_, hT, final=False):
            junk, stat, hnp, otp = st_pools
            jt = junk[tt % len(junk)]
            st = stat[tt % len(stat)]
            I("act", lambda e: e.activation(out=jt[:], in_=xt[:], func=AF.Square, accum_out=st[:, 0:1]),
              [xt], [jt, (st, 0)])
            rstd_chain(st, 0, 1.0 / D, 1)
            if final:
                ot = otp[tt % len(otp)]
                I("dve", lambda e: e.scalar_tensor_tensor(out=ot[:], in0=xt[:], scalar=st[:, 1:2], in1=gt_[:],
                                                          op0=ALU.mult, op1=ALU.mult), [xt, (st, 1), gt_], [ot])
                DMA("sp", ot, lambda e: [e.dma_start(out=out[tt * 128:(tt + 1) * 128, :], in_=ot[:])], reads=[ot])
                return
            hn = hnp[tt % len(hnp)]
            I("dve", lambda e: e.scalar_tensor_tensor(out=hn[:], in0=xt[:], scalar=st[:, 1:2], in1=gt_[:],
                                                      op0=ALU.mult, op1=ALU.mult), [xt, (st, 1), gt_], [hn])
            for kc in range(8):
                I("pe", lambda e, kc=kc: e.transpose(out=psB[:, kc * 128:(kc + 1) * 128],
                                                     in_=hn[:, kc * 128:(kc + 1) * 128], identity=ident_b[:]),
                  [hn, ident_b], [psB])
            j = tt % 4
            I("act", lambda e: e.activation(out=hT[:, :, j * 128:(j + 1) * 128],
                                            in_=psB[:].rearrange("p (k t) -> p k t", k=8), func=AF.Copy),
              [], [psB, (hT, j)])
            if j == 3:
                c = tt // 4
                DMA("sp", hT, lambda e: [e.dma_start(out=hnT[:, :, c * 512:(c + 1) * 512].rearrange("k p t -> p k t"),
                                                     in_=hT[:])],
                    reads=[(hT, 0), (hT, 1), (hT, 2), (hT, 3)])

        def phase_norm0(gidx):
            with ExitStack() as st:
                xp = pool(st, "n0x", [128, D], F32, 3)
                pools = (pool(st, "n0j", [128, D], F32, 2, False), pool(st, "n0s", [128, 8], F32, 4, False),
                         pool(st, "n0h", [128, D], BF16, 2, False), None)
                hTp = pool(st, "n0T", [128, 8, 512], BF16, 2)
                gt_ = sbt(st, "gt0", [128, D], F32)
                DMA("sp", gt_, lambda e: [e.dma_start(out=gt_[:], in_=gvec[:, gidx, :])], writes=[gt_])
                for tt in range(NT):
                    xt = xp[tt % 3]
                    DMA("sp", xt, lambda e, xt=xt, tt=tt: [e.dma_start(out=xt[:], in_=x_in[tt * 128:(tt + 1) * 128, :])],
                        writes=[xt])
                    DMA("sp", xt, lambda e, xt=xt, tt=tt: [e.dma_start(out=xs[tt * 128:(tt + 1) * 128, :], in_=xt[:])],
                        reads=[xt])
                    norm_to_hnT(pools, xt, tt, gt_, hTp[(tt // 4) % 2])
                sc.barrier()

        def load_w_stage(w_dram_2d, c0, ncols, stage):
            DMA("sp", stage, lambda e: [e.dma_start(out=stage[:, :, 0:ncols],
                                                    in_=w_dram_2d[:, c0:c0 + ncols].rearrange("(kc p) c -> p kc c", p=128))],
                writes=[stage])

        def cast_w(wt, ncols, stage):
            I("act", lambda e: e.activation(out=wt[:, :, 0:ncols], in_=stage[:, :, 0:ncols], func=AF.Copy), [stage], [wt])

        def load_hT(hT, c):
            DMA("sp", hT, lambda e: [e.dma_start(out=hT[:], in_=hnT[:, :, c * 512:(c + 1) * 512].rearrange("k p t -> p k t"))],
                writes=[hT])

        def proj_fm(wt, units, evac, hTp, mid=None):
            load_hT(hTp[0], 0)
            for c in range(NC5):
                hT = hTp[c % len(hTp)]
                if c + 1 < NC5:
                    load_hT(hTp[(c + 1) % len(hTp)], c + 1)
                for ui, cols in enumerate(units):
                    pss = []
                    for co in cols:
                        ps = ps_next()
                        for kc in range(8):
                            I("pe", lambda e, ps=ps, kc=kc, co=co, hT=hT: e.matmul(
                                ps[:], lhsT=wt[:, kc, co:co + 128], rhs=hT[:, kc, :], start=(kc == 0), stop=(kc == 7)),
                              [wt, hT], [ps])
                        pss.append(ps)
                    evac(ui, c, pss)
                if mid is not None and c == min(4, NC5 - 1):
                    mid()

        def proj_tm(wt, ncols, evac, hTp, mid=None):
            load_hT(hTp[0], 0)
            for c in range(NC5):
                hT = hTp[c % len(hTp)]
                if c + 1 < NC5:
                    load_hT(hTp[(c + 1) % len(hTp)], c + 1)
                for j in range(4):
                    tt = c * 4 + j
                    for half in range((ncols + 511) // 512):
                        w = min(512, ncols - half * 512)
                        ps = ps_next()
                        for kc in range(8):
                            I("pe", lambda e, ps=ps, kc=kc, half=half, w=w, j=j, hT=hT: e.matmul(
                                ps[:, 0:w], lhsT=hT[:, kc, j * 128:(j + 1) * 128],
                                rhs=wt[:, kc, half * 512:half * 512 + w], start=(kc == 0), stop=(kc == 7)),
                              [wt, hT], [ps])
                        evac(tt, half, ps, w)
                if mid is not None and c == min(4, NC5 - 1):
                    mid()

        def phase_out(li, w_out_2d):
            final = (li == n_layers - 1)
            gidx = 4 if final else layers[li + 1][2]
            with ExitStack() as st:
                wo = sbt(st, "wo", [128, 16, D], BF16, False)
                wos = sbt(st, "wos", [128, 8, D], F32)
                for hf in range(2):
                    DMA("sp", wos, lambda e, hf=hf: [e.dma_start(
                        out=wos[:], in_=w_out_2d[hf * 1024:(hf + 1) * 1024, :].rearrange("(kc p) c -> p kc c", p=128))], writes=[wos])
                    I("pool", lambda e, hf=hf: e.tensor_copy(out=wo[:, hf * 8:(hf + 1) * 8, :], in_=wos[:]), [wos], [(wo, hf)])
                gt_ = sbt(st, "gt", [128, D], F32)
                DMA("sp", gt_, lambda e: [e.dma_start(out=gt_[:], in_=gvec[:, gidx, :])], writes=[gt_])
                yp = pool(st, "poy", [128, 16, 512], BF16, 2)
                xp = pool(st, "pox", [128, D], F32, 4)
                pools = (pool(st, "poj", [128, D], F32, 2, False), pool(st, "pos", [128, 8], F32, 4, False),
                         pool(st, "poh", [128, D], BF16, 2, False), pool(st, "poo", [128, D], F32, 2))
                hTp = pool(st, "poT", [128, 8, 512], BF16, 2)
                def load_y(c):
                    yt = yp[c % 2]
                    DMA("sp", yt, lambda e: [e.dma_start(
                        out=yt[:, hf * 8:(hf + 1) * 8, :], in_=yT[hf * 8:(hf + 1) * 8, :, c * 512:(c + 1) * 512].rearrange("k p t -> p k t"))
                        for hf in range(2)], writes=[yt], n=2)

                load_y(0)

                def stage_a(tt):
                    c, j = tt // 4, tt % 4
                    yt = yp[c % 2]
                    if j == 1 and c + 1 < NC5:
                        load_y(c + 1)
                    xt = xp[tt % 4]
                    DMA("sp", xt, lambda e: [e.dma_start(out=xt[:], in_=xs[tt * 128:(tt + 1) * 128, :])], writes=[xt])
                    for half in range(2):
                        ps = ps_next()
                        for kc in range(16):
                            I("pe", lambda e, ps=ps, kc=kc, half=half: e.matmul(
                                ps[:], lhsT=yt[:, kc, j * 128:(j + 1) * 128],
                                rhs=wo[:, kc, half * 512:(half + 1) * 512], start=(kc == 0), stop=(kc == 15)),
                              [(wo, 0), (wo, 1), yt], [ps])
                        I("dve", lambda e, ps=ps, half=half: e.tensor_tensor(
                            out=xt[:, half * 512:(half + 1) * 512], in0=ps[:], in1=xt[:, half * 512:(half + 1) * 512],
                            op=ALU.add), [], [ps, xt])
                    if not final:
                        DMA("sp", xt, lambda e: [e.dma_start(out=xs[tt * 128:(tt + 1) * 128, :], in_=xt[:])], reads=[xt])

                def stage_b(tt):
                    norm_to_hnT(pools, xp[tt % 4], tt, gt_, hTp[(tt // 4) % 2], final=final)

                stage_a(0)
                for tt in range(NT):
                    if tt + 1 < NT:
                        stage_a(tt + 1)
                    stage_b(tt)
                sc.barrier()

        def phase_odd_inproj(o):
            W = od_w_in[o]
            with ExitStack() as st:
                wtp = pool(st, "oiw", [128, 8, 1024], BF16, 2, False)
                wstage = sbt(st, "oiws", [128, 8, 1024], F32)
                hTp = pool(st, "oih", [128, 8, 512], BF16, 3)
                cosT = sbt(st, "cosT", [128, S], F32)
                sinT = sbt(st, "sinT", [128, S], F32)
                DMA("sp", cosT, lambda e: [e.dma_start(out=cosT[:], in_=cosT_d[:, :])], writes=[cosT])
                DMA("sp", sinT, lambda e: [e.dma_start(out=sinT[:], in_=sinT_d[:, :])], writes=[sinT])
                t1p = pool(st, "oit1", [128, 512], F32, 2, False)
                t2p = pool(st, "oit2", [128, 512], F32, 2, False)
                stg = pool(st, "oist", [128, 512], BF16, 4)
                cnt = [0]
                load_w_stage(W, 0, 1024, wstage)
                cast_w(wtp[0], 1024, wstage)
                for g in range(12):
                    wt = wtp[g % 2]
                    mid = None
                    if g + 1 < 12:
                        load_w_stage(W, (g + 1) * 1024, 1024, wstage)
                        mid = (lambda g=g: cast_w(wtp[(g + 1) % 2], 1024, wstage))
                    if g < 8:
                        dst = qT_s if g < 4 else kT_s
                        units = [(h * 256, h * 256 + 128) for h in range(4)]

                        def evac(ui, c, pss, g=g, dst=dst):
                            i = cnt[0]
                            cnt[0] += 1
                            t1, t2, sg = t1p[i % 2], t2p[i % 2], stg[i % 4]
                            p0 = (c * 512) % S
                            I("dve", lambda e: e.tensor_tensor(out=t1[:], in0=pss[0][:], in1=cosT[:, p0:p0 + 512], op=ALU.mult),
                              [cosT], [pss[0], t1])
                            I("dve", lambda e: e.tensor_tensor(out=t2[:], in0=pss[1][:], in1=sinT[:, p0:p0 + 512], op=ALU.mult),
                              [sinT], [pss[1], t2])
                            I("pool", lambda e: e.tensor_tensor(out=sg[:], in0=t1[:], in1=t2[:], op=ALU.add), [t1, t2], [sg])
                            h = (g % 4) * 4 + ui
                            DMA("sp", sg, lambda e: [e.dma_start(out=dst[h, :, c * 512:(c + 1) * 512], in_=sg[:])], reads=[sg])
                        proj_fm(wt, units, evac, hTp, mid)
                    elif g < 10:
                        units = [(j * 128,) for j in range(8)]

                        def evac(ui, c, pss, g=g):
                            i = cnt[0]
                            cnt[0] += 1
                            sg = stg[i % 4]
                            I("act", lambda e: e.activation(out=sg[:], in_=pss[0][:], func=AF.Silu), [], [pss[0], sg])
                            h = (g - 8) * 8 + ui
                            DMA("sp", sg, lambda e: [e.dma_start(out=zT_s[h, :, c * 512:(c + 1) * 512], in_=sg[:])], reads=[sg])
                        proj_fm(wt, units, evac, hTp, mid)
                    else:
                        def evac(tt, half, ps, w, g=g):
                            i = cnt[0]
                            cnt[0] += 1
                            sg = stg[i % 4]
                            I("act", lambda e: e.activation(out=sg[:], in_=ps[:], func=AF.Copy), [], [ps, sg])
                            c0 = (g - 10) * 1024 + half * 512
                            DMA("sp", sg, lambda e: [e.dma_start(out=v_s[tt * 128:(tt + 1) * 128, c0:c0 + 512], in_=sg[:])],
                                reads=[sg])
                        proj_tm(wt, 1024, evac, hTp, mid)
                sc.barrier()

        def phase_attn(o, layer_no):
            li = lambda_init(layer_no)
            with ExitStack() as st:
                lv = sbt(st, "lv", [128, 4, 64], F32)
                sl = sbt(st, "sl", [128, 8], F32, False)
                gcol = sbt(st, "gcol", [128, 1], F32)
                lj = sbt(st, "lj", [128, 64], F32, False)
                DMA("sp", lv, lambda e: [e.dma_start(out=lv[:], in_=lamv[o])], writes=[lv])
                DMA("sp", gcol, lambda e: [e.dma_start(out=gcol[:], in_=subln[o])], writes=[gcol])
                for i2 in range(2):
                    I("dve", lambda e, i2=i2: e.tensor_tensor(out=lj[:], in0=lv[:, 2 * i2, :], in1=lv[:, 2 * i2 + 1, :], op=ALU.mult),
                      [lv], [lj])
                    I("dve", lambda e, i2=i2: e.reduce_sum(out=sl[:, i2:i2 + 1], in_=lj[:], axis=AX.X), [lj], [(sl, i2)])
                    I("act", lambda e, i2=i2: e.activation(out=sl[:, 2 + i2:3 + i2], in_=sl[:, i2:i2 + 1], func=AF.Exp),
                      [(sl, i2)], [(sl, 2 + i2)])
                I("dve", lambda e: e.tensor_tensor(out=sl[:, 5:6], in0=sl[:, 3:4], in1=sl[:, 2:3], op=ALU.subtract),
                  [(sl, 2), (sl, 3)], [(sl, 5)])
                I("dve", lambda e: e.tensor_scalar(out=sl[:, 4:5], in0=sl[:, 5:6], scalar1=-li, scalar2=None, op0=ALU.add),
                  [(sl, 5)], [(sl, 4)])
                I("dve", lambda e: e.tensor_scalar(out=gcol[:], in0=gcol[:], scalar1=1.0 - li, scalar2=None, op0=ALU.mult),
                  [gcol], [gcol])

                NG = S // 256
                qp = pool(st, "aqq", [128, NG, 2, 256], BF16, 2)
                for b_ in range(2):
                    I("dve", lambda e, b_=b_: e.memset(qp[b_][64:128, :, 0, :], 0.0), [], [(qp[b_], "z0")])
                    I("dve", lambda e, b_=b_: e.memset(qp[b_][0:64, :, 1, :], 0.0), [], [(qp[b_], "z1")])
                kp = pool(st, "ak", [128, S], BF16, 2)
                zp = pool(st, "az", [128, S], BF16, 2)
                vp = pool(st, "av", [128, CPS, 132], BF16, 2)
                yp = pool(st, "ay", [128, S], BF16, 2)
                pp = pool(st, "ap", [128, 512], BF16, 6, False)
                stp = pool(st, "ast", [128, 8], F32, 8, False)
                o1p = pool(st, "ao1", [128, 128], F32, 8, False)
                o2p = pool(st, "ao2", [128, 128], F32, 8, False)
                jkp = pool(st, "ajk", [128, 128], F32, 8, False)
                onp = pool(st, "aon", [128, 128], BF16, 8, False)
                for v_ in vp:
                    I("dve", lambda e, v_=v_: e.memset(v_[:, :, 128:129], 1.0), [], [(v_, "ones")])
                it = 0
                qb = 0
                epi_i = [0]
                gstep = [0]
                pending = []

                def defer(delay, fn):
                    pending.append([gstep[0] + delay, fn, it])

                def run_due(force=False, owner_lt=None):
                    keep = []
                    for item in list(pending):
                        if force or item[0] <= gstep[0] or (owner_lt is not None and item[2] < owner_lt):
                            item[1]()
                        else:
                            keep.append(item)
                    pending[:] = keep

                def issue_loads(idx):
                    s_, h = idx // 16, idx % 16
                    qq, k, z, v = qp[idx % 2], kp[idx % 2], zp[idx % 2], vp[idx % 2]
                    t0 = s_ * S
                    DMA("sp", qq, lambda e: [
                        e.dma_start(out=qq[0:64, :, 0, :], in_=qT_s[h, 0:64, t0:t0 + S].rearrange("p (g q) -> p g q", q=256)),
                        e.dma_start(out=qq[64:128, :, 1, :], in_=qT_s[h, 64:128, t0:t0 + S].rearrange("p (g q) -> p g q", q=256))],
                        writes=[qq], n=2)
                    DMA("sp", k, lambda e: [e.dma_start(out=k[:], in_=kT_s[h, :, t0:t0 + S])], writes=[k])
                    DMA("sp", z, lambda e: [e.dma_start(out=z[:], in_=zT_s[h, :, t0:t0 + S])], writes=[z])
                    nvp = (CPS + 7) // 8
                    DMA("sp", v, lambda e: [e.dma_start(
                        out=v[:, j8 * 8:min(CPS, j8 * 8 + 8), 0:128],
                        in_=v_s[t0 + j8 * 1024:min(t0 + S, t0 + j8 * 1024 + 1024), h * 128:(h + 1) * 128].rearrange(
                            "(c p) e -> p c e", p=128)) for j8 in range(nvp)], writes=[v], n=nvp)

                issue_loads(0)
                for s in range(NS):
                    for h in range(16):
                        qq, k, z, v, y = qp[it % 2], kp[it % 2], zp[it % 2], vp[it % 2], yp[it % 2]
                        it += 1
                        t0 = s * S
                        steps = [(G, i) for G in range(CPS // 2) for i in range(2 * G + 2)]
                        info = {}

                        def emit_qk(n, qq=qq, k=k):
                            nonlocal qb
                            G, i = steps[n]
                            r = 1 if i == 2 * G + 1 else 0
                            sps = psF[4 + qb % 3]
                            pt = pp[qb % 4]
                            qb += 1
                            info[n] = (r, sps, pt)
                            qdeps = [k, qq, (qq, "z0"), (qq, "z1")]
                            if r == 0:
                                I("pe", lambda e, sps=sps, i=i, G=G: e.matmul(
                                    sps[:, 0:512], lhsT=k[:, i * 128:(i + 1) * 128],
                                    rhs=qq[:, G, :, :].rearrange("p c q -> p (c q)"), start=True, stop=True), qdeps, [sps])
                            else:
                                for cmp_ in range(2):
                                    I("pe", lambda e, sps=sps, cmp_=cmp_, i=i, G=G: e.matmul(
                                        sps[:, cmp_ * 256:cmp_ * 256 + 128], lhsT=k[:, i * 128:(i + 1) * 128],
                                        rhs=qq[:, G, cmp_, 128:256], start=True, stop=True), qdeps, [sps])

                        def emit_rest(n, v=v):
                            G, i = steps[n]
                            r, sps, pt = info.pop(n)
                            if r == 0:
                                I("act", lambda e, sps=sps, pt=pt: e.activation(out=pt[:], in_=sps[:], func=AF.Exp, scale=0.125),
                                  [], [sps, pt])
                            else:
                                I("act", lambda e, sps=sps, pt=pt: e.activation(
                                    out=pt[:].rearrange("p (c q) -> p c q", c=2)[:, :, 0:128],
                                    in_=sps[:].rearrange("p (c q) -> p c q", c=2)[:, :, 0:128], func=AF.Exp, scale=0.125),
                                  [], [sps, pt])
                            if i >= 2 * G:
                                for cmp_ in range(2):
                                    ds_ = slice(cmp_ * 256, cmp_ * 256 + 128)
                                    I("dve", lambda e, pt=pt, ds_=ds_: e.tensor_tensor(
                                        out=pt[:, ds_], in0=pt[:, ds_], in1=tri_b[:], op=ALU.mult), [tri_b], [pt])
                            for ql in range(r, 2):
                                for cmp_ in range(2):
                                    acc = psF[(G % 2) * 2 + ql]
                                    col = cmp_ * 256 + (ql - r) * 128
                                    first = (i == 0 and cmp_ == 0)
                                    last = (i == 2 * G + ql)
                                    I("pe", lambda e, acc=acc, pt=pt, col=col, i=i, v=v, first=first, last=last, cmp_=cmp_: e.matmul(
                                        acc[:, cmp_ * 256:cmp_ * 256 + 129], lhsT=pt[:, col:col + 128], rhs=v[:, i, 0:129],
                                        start=first, stop=last, skip_group_check=True),
                                      [pt, v, (v, "ones")], [acc])

                        def emit_epilogue(G, y=y, z=z):
                            par = G % 2
                            bufs = []
                            for ql in range(2):
                                ab = psF[par * 2 + ql]
                                bi = epi_i[0] % 8
                                epi_i[0] += 1
                                stt, o1, o2, jk, on = stp[bi], o1p[bi], o2p[bi], jkp[bi], onp[bi]
                                bufs.append((stt, o2, on))
                                I("dve", lambda e, ab=ab, stt=stt: e.reciprocal(out=stt[:, 0:1], in_=ab[:, 128:129]), [], [ab, (stt, 0)])
                                I("dve", lambda e, ab=ab, stt=stt: e.reciprocal(out=stt[:, 2:3], in_=ab[:, 384:385]), [], [ab, (stt, 2)])
                                I("dve", lambda e, stt=stt: e.tensor_tensor(out=stt[:, 3:4], in0=stt[:, 2:3], in1=sl[:, 4:5], op=ALU.mult),
                                  [(stt, 2), (sl, 4)], [(stt, 3)])
                                I("dve", lambda e, ab=ab, stt=stt, o1=o1: e.tensor_scalar(
                                    out=o1[:], in0=ab[:, 0:128], scalar1=stt[:, 0:1], scalar2=None, op0=ALU.mult),
                                  [(stt, 0)], [ab, o1])
                                I("dve", lambda e, ab=ab, stt=stt, o1=o1, o2=o2: e.scalar_tensor_tensor(
                                    out=o2[:], in0=ab[:, 256:384], scalar=stt[:, 3:4], in1=o1[:], op0=ALU.mult, op1=ALU.add),
                                  [(stt, 3), o1], [ab, o2])
                                I("dve", lambda e, o2=o2, jk=jk, stt=stt: e.scalar_tensor_tensor(
                                    out=jk[:], in0=o2[:], scalar=1.0, in1=o2[:], op0=ALU.mult, op1=ALU.mult, accum_out=stt[:, 4:5]),
                                  [o2], [jk, (stt, 4)])
                                I("dve", lambda e, stt=stt: e.tensor_scalar(out=stt[:, 6:7], in0=stt[:, 4:5], scalar1=1.0 / 128,
                                                                            scalar2=EPS, op0=ALU.mult, op1=ALU.add), [(stt, 4)], [(stt, 6)])

                            def s2():
                                for (stt, o2, on) in bufs:
                                    I("act", lambda e, stt=stt: e.activation(out=stt[:, 7:8], in_=stt[:, 6:7], func=AF.Ln), [(stt, 6)], [(stt, 7)])
                                    I("act", lambda e, stt=stt: e.activation(out=stt[:, 5:6], in_=stt[:, 7:8], func=AF.Exp, scale=-0.5),
                                      [(stt, 7)], [(stt, 5)])

                            def s3():
                                for (stt, o2, on) in bufs:
                                    I("dve", lambda e, o2=o2, on=on, stt=stt: e.tensor_scalar(
                                        out=on[:], in0=o2[:], scalar1=stt[:, 5:6], scalar2=None, op0=ALU.mult), [o2, (stt, 5)], [on])

                            def s4():
                                for ql, (stt, o2, on) in enumerate(bufs):
                                    I("pe", lambda e, on=on, ql=ql: e.transpose(out=psB[:, ql * 128:(ql + 1) * 128], in_=on[:],
                                                                                identity=ident_b[:]), [on, ident_b], [psB])

                            def s5():
                                qcol = 2 * G * 128
                                I("dve", lambda e: e.scalar_tensor_tensor(
                                    out=y[:, qcol:qcol + 256], in0=psB[:, 0:256], scalar=gcol[:, 0:1], in1=z[:, qcol:qcol + 256],
                                    op0=ALU.mult, op1=ALU.mult), [gcol, z], [psB, y])

                            defer(4, s2)
                            defer(6, s3)
                            defer(8, s4)
                            defer(10, s5)

                        LA = 2
                        for n0 in range(min(LA, len(steps))):
                            emit_qk(n0)
                        for n in range(len(steps)):
                            if n + LA < len(steps):
                                emit_qk(n + LA)
                            emit_rest(n)
                            gstep[0] += 1
                            run_due()
                            if n == min(14, len(steps) - 1) and it < NS * 16:
                                run_due(owner_lt=it)
                                issue_loads(it)
                            G, i = steps[n]
                            if i == 2 * G + 1:
                                emit_epilogue(G)
                        defer(12, lambda y=y, h=h, t0=t0: DMA(
                            "sp", y, lambda e: [e.dma_start(out=yT[h, :, t0:t0 + S], in_=y[:])], reads=[y]))
                run_due(force=True)
                sc.barrier()


        def gelu_evac(ps, w, dstt, tmps):
            t1, t2 = tmps
            I("act", lambda e: e.activation(out=t1[:, 0:w], in_=ps[:, 0:w], func=AF.Square), [], [ps, t1])
            I("dve", lambda e: e.tensor_scalar(out=t1[:, 0:w], in0=t1[:, 0:w], scalar1=0.044715, scalar2=1.0,
                                               op0=ALU.mult, op1=ALU.add), [t1], [t1])
            I("dve", lambda e: e.tensor_tensor(out=t2[:, 0:w], in0=ps[:, 0:w], in1=t1[:, 0:w], op=ALU.mult), [t1], [ps, t2])
            I("act", lambda e: e.activation(out=t1[:, 0:w], in_=t2[:, 0:w], func=AF.Sigmoid, scale=1.5957691216057308),
              [t2], [t1])
            I("dve", lambda e: e.tensor_tensor(out=dstt[:, 0:w], in0=ps[:, 0:w], in1=t1[:, 0:w], op=ALU.mult), [t1], [ps, dstt])

        def phase_even_inproj(ei):
            W = ev_w_in[ei]
            with ExitStack() as st:
                wtp = pool(st, "eiw", [128, 8, 1024], BF16, 2, False)
                wstage = sbt(st, "eiws", [128, 8, 1024], F32)
                hTp = pool(st, "eih", [128, 8, 512], BF16, 3)
                t1p = pool(st, "eit1", [128, 512], F32, 2, False)
                t2p = pool(st, "eit2", [128, 512], F32, 2, False)
                stg = pool(st, "eist", [128, 512], BF16, 4)
                stgf = pool(st, "eisf", [128, 8], F32, 3)
                cnt = [0]
                groups = [(0, 1024, "tm", "gelu", u_s), (1024, 1024, "tm", "gelu", va_s), (2048, 1024, "fm", "silu", zaT_s),
                          (3072, 1024, "fm", "copy", xmT_s), (4096, 1024, "tm", "sigm", og_s), (5128, 1024, "fm", "silu", zbT_s),
                          (5120, 8, "tm", "gate", gt_s)]
                load_w_stage(W, groups[0][0], groups[0][1], wstage)
                cast_w(wtp[0], groups[0][1], wstage)
                for gi, (c0, ncols, mode, kind, dst) in enumerate(groups):
                    wt = wtp[gi % 2]
                    mid = None
                    if gi + 1 < len(groups):
                        nc0, nnc = groups[gi + 1][0], groups[gi + 1][1]
                        load_w_stage(W, nc0, nnc, wstage)
                        mid = (lambda gi=gi, nnc=nnc: cast_w(wtp[(gi + 1) % 2], nnc, wstage))
                    if mode == "fm":
                        units = [(j * 128,) for j in range(8)]

                        def evac(ui, c, pss, kind=kind, dst=dst):
                            i = cnt[0]
                            cnt[0] += 1
                            sg = stg[i % 4]
                            fn = AF.Silu if kind == "silu" else AF.Copy
                            I("act", lambda e: e.activation(out=sg[:], in_=pss[0][:], func=fn), [], [pss[0], sg])
                            DMA("sp", sg, lambda e: [e.dma_start(out=dst[ui, :, c * 512:(c + 1) * 512], in_=sg[:])], reads=[sg])
                        proj_fm(wt, units, evac, hTp, mid)
                    else:
                        def evac(tt, half, ps, w, kind=kind, dst=dst):
                            i = cnt[0]
                            cnt[0] += 1
                            if kind == "gate":
                                sf = stgf[i % 3]
                                I("act", lambda e: e.activation(out=sf[:], in_=ps[:, 0:8], func=AF.Copy), [], [ps, sf])
                                DMA("sp", sf, lambda e: [e.dma_start(out=dst[tt * 128:(tt + 1) * 128, :], in_=sf[:])], reads=[sf])
                                return
                            sg = stg[i % 4]
                            if kind == "gelu":
                                gelu_evac(ps, w, sg, (t1p[i % 2], t2p[i % 2]))
                            else:
                                I("act", lambda e: e.activation(out=sg[:], in_=ps[:], func=AF.Sigmoid), [], [ps, sg])
                            DMA("sp", sg, lambda e: [e.dma_start(out=dst[tt * 128:(tt + 1) * 128, half * 512:(half + 1) * 512],
                                                                 in_=sg[:])], reads=[sg])
                        proj_tm(wt, ncols, evac, hTp, mid)
                sc.barrier()

        def phase_even_mix(ei):
            with ExitStack() as st:
                alg = sbt(st, "alg", [128, D], F32)
                wsf = sbt(st, "wsf", [128, 8, 128], F32)
                wsb = sbt(st, "wsb", [128, 8, 128], BF16, False)
                bsT = sbt(st, "bsT", [128, 8], F32)
                cw = sbt(st, "cw", [128, 8, 4], F32)
                cv = sbt(st, "cv", [128, 3, 8], F32)
                gb = sbt(st, "gb", [128, 8], F32)
                Wb = sbt(st, "Wb", [128, 3, 8, 256], BF16, False)
                GN = sbt(st, "GN", [128, 8, 128], F32, False)
                SK = sbt(st, "SK", [128, 8, 128], F32, False)
                tri4 = sbt(st, "tri4", [128, 4, 128], F32, False)
                DMA("sp", alg, lambda e: [e.dma_start(out=alg[:], in_=a_ln_g[ei])], writes=[alg])
                DMA("sp", wsf, lambda e: [e.dma_start(out=wsf[:], in_=a_wsT[ei].rearrange("g s t -> s g t"))], writes=[wsf])
                DMA("sp", bsT, lambda e: [e.dma_start(out=bsT[:], in_=a_bsT[ei])], writes=[bsT])
                DMA("sp", cw, lambda e: [e.dma_start(out=cw[:], in_=convw[ei])], writes=[cw])
                DMA("sp", cv, lambda e: [e.dma_start(out=cv[:], in_=colv[ei])], writes=[cv])
                DMA("sp", gb, lambda e: [e.dma_start(out=gb[:], in_=gate_b[ei])], writes=[gb])
                Wbs = sbt(st, "Wbs", [128, 8, 256], F32)
                for j in range(3):
                    DMA("sp", Wbs, lambda e, j=j: [e.dma_start(out=Wbs[:], in_=b_wqkv[ei, j].rearrange("h (dc p) e -> p (h dc) e", p=128))],
                        writes=[Wbs])
                    I("pool", lambda e, j=j: e.tensor_copy(out=Wb[:, j, :, :], in_=Wbs[:]), [Wbs], [(Wb, j)])
                Wb_keys = [(Wb, j) for j in range(3)]
                for g in range(8):
                    I("dve", lambda e, g=g: e.tensor_tensor(out=wsb[:, g, :], in0=wsf[:, g, :], in1=tri_f[:], op=ALU.mult),
                      [wsf, tri_f], [(wsb, g)])
                    I("dve", lambda e, g=g: e.tensor_scalar(out=GN[:, g, :], in0=ones_f[:], scalar1=cv[:, 1, g:g + 1], scalar2=None,
                                                            op0=ALU.mult), [ones_f, cv], [(GN, g)])
                    I("dve", lambda e, g=g: e.tensor_scalar(out=SK[:, g, :], in0=ones_f[:], scalar1=cv[:, 2, g:g + 1], scalar2=None,
                                                            op0=ALU.mult), [ones_f, cv], [(SK, g)])
                for h in range(4):
                    I("dve", lambda e, h=h: e.tensor_copy(out=tri4[:, h, :], in_=tri_f[:]), [tri_f], [(tri4, h)])
                wsb_keys = [(wsb, g) for g in range(8)]
                GN_keys = [(GN, g) for g in range(8)]
                SK_keys = [(SK, g) for g in range(8)]
                tri4_keys = [(tri4, h) for h in range(4)]

                tmps = {}

                def tmp(name, shape, dt, n=2):
                    if name not in tmps:
                        tmps[name] = [pool(st, "t" + name, shape, dt, n, False), 0]
                    p = tmps[name]
                    t = p[0][p[1] % n]
                    p[1] += 1
                    return t

                Cst = sbt(st, "Cst", [128, 4, 2, 260], F32, False)
                Cb = sbt(st, "Cb", [128, 4, 2, 260], BF16, False)

                up = pool(st, "mu", [128, D], BF16, 2)
                vap = pool(st, "mva", [128, D], BF16, 2)
                ogp = pool(st, "mog", [128, D], BF16, 2)
                gtp = pool(st, "mgt", [128, 8], F32, 2)
                zap = pool(st, "mza", [128, 8, 128], BF16, 2)
                zbp = pool(st, "mzb", [128, 8, 128], BF16, 2)
                xmp = pool(st, "mxm", [128, 8, 131], BF16, 2)
                yap = pool(st, "mya", [128, 8, 128], BF16, 2)
                ybp = pool(st, "myb", [128, 8, 128], BF16, 2)
                def tile_loads(tt):
                    c = tt % CPS
                    tok0 = tt * 128
                    b2 = tt % 2
                    ut, vat, ogt, gtt, zat, zbt, xmt = up[b2], vap[b2], ogp[b2], gtp[b2], zap[b2], zbp[b2], xmp[b2]
                    rows = slice(tok0, tok0 + 128)
                    DMA("sp", ut, lambda e, ut=ut, rows=rows: [e.dma_start(out=ut[:], in_=u_s[rows, :])], writes=[ut])
                    DMA("sp", vat, lambda e, vat=vat, rows=rows: [e.dma_start(out=vat[:], in_=va_s[rows, :])], writes=[vat])
                    DMA("sp", ogt, lambda e, ogt=ogt, rows=rows: [e.dma_start(out=ogt[:], in_=og_s[rows, :])], writes=[ogt])
                    DMA("sp", gtt, lambda e, gtt=gtt, rows=rows: [e.dma_start(out=gtt[:], in_=gt_s[rows, :])], writes=[gtt])
                    DMA("sp", zat, lambda e, zat=zat, rows=rows: [e.dma_start(
                        out=zat[:], in_=zaT_s[:, :, rows].rearrange("k p t -> p k t"))], writes=[zat])
                    DMA("sp", zbt, lambda e, zbt=zbt, rows=rows: [e.dma_start(
                        out=zbt[:], in_=zbT_s[:, :, rows].rearrange("k p t -> p k t"))], writes=[zbt])
                    if c == 0:
                        I("dve", lambda e, xmt=xmt: e.memset(xmt[:, :, 0:3], 0.0), [], [xmt])
                        DMA("sp", xmt, lambda e, xmt=xmt, rows=rows: [e.dma_start(
                            out=xmt[:, :, 3:131], in_=xmT_s[:, :, rows].rearrange("k p t -> p k t"))], writes=[xmt])
                    else:
                        DMA("sp", xmt, lambda e, xmt=xmt, tok0=tok0: [e.dma_start(
                            out=xmt[:], in_=xmT_s[:, :, tok0 - 3:tok0 + 128].rearrange("k p t -> p k t"))], writes=[xmt])


                def do_tile(tt):
                    c = tt % CPS
                    tok0 = tt * 128
                    b2 = tt % 2
                    ut, vat, ogt, gtt, zat, zbt, xmt = up[b2], vap[b2], ogp[b2], gtp[b2], zap[b2], zbp[b2], xmp[b2]
                    rows = slice(tok0, tok0 + 128)
                    if tt + 1 < NT:
                        tile_loads(tt + 1)
                    bst = tmp("bst", [128, 2, 6], F32)
                    mv = tmp("mv", [128, 8], F32)
                    for hf in range(2):
                        I("dve", lambda e, hf=hf, bst=bst, vat=vat: e.bn_stats(out=bst[:, hf, :], in_=vat[:, hf * 512:(hf + 1) * 512]),
                          [vat], [(bst, hf)])
                    I("dve", lambda e, bst=bst, mv=mv: e.bn_aggr(out=mv[:, 0:2], in_=bst[:].rearrange("p a b -> p (a b)")),
                      [(bst, 0), (bst, 1)], [(mv, 0), (mv, 1)])
                    rstd_chain(mv, 1, 1.0, 2)
                    vn = tmp("vn", [128, D], F32, 1)
                    vnb = tmp("vnb", [128, D], BF16)
                    I("dve", lambda e, vn=vn, vat=vat, mv=mv: e.tensor_scalar(out=vn[:], in0=vat[:], scalar1=mv[:, 0:1], scalar2=mv[:, 2:3],
                                                                              op0=ALU.subtract, op1=ALU.mult), [vat, (mv, 0), (mv, 2)], [vn])
                    I("pool", lambda e, vn=vn, vnb=vnb: e.tensor_tensor(out=vnb[:], in0=vn[:], in1=alg[:], op=ALU.mult), [vn, alg], [vnb])
                    psA = [ps_next(), ps_next()]
                    for g in range(8):
                        pg = psA[g // 4]
                        I("pe", lambda e, g=g, pg=pg, vnb=vnb: e.matmul(pg[:, (g % 4) * 128:(g % 4 + 1) * 128], lhsT=wsb[:, g, :],
                                                                        rhs=vnb[:, g * 128:(g + 1) * 128], start=True, stop=True),
                          [(wsb, g), vnb], [pg])
                    ya = tmp("ya", [128, D], BF16)
                    for g in range(8):
                        pg = psA[g // 4]
                        I("dve", lambda e, g=g, pg=pg, ya=ya, ut=ut: e.scalar_tensor_tensor(
                            out=ya[:, g * 128:(g + 1) * 128], in0=pg[:, (g % 4) * 128:(g % 4 + 1) * 128], scalar=bsT[:, g:g + 1],
                            in1=ut[:, g * 128:(g + 1) * 128], op0=ALU.add, op1=ALU.mult), [bsT, ut], [pg, (ya, g)])
                    for g in range(8):
                        I("pe", lambda e, g=g, ya=ya: e.transpose(out=psB[:, g * 128:(g + 1) * 128], in_=ya[:, g * 128:(g + 1) * 128],
                                                                  identity=ident_b[:]), [(ya, g), ident_b], [psB])
                    yat = yap[b2]
                    I("dve", lambda e, yat=yat, zat=zat: e.tensor_tensor(out=yat[:], in0=psB[:].rearrange("p (k t) -> p k t", k=8),
                                                                         in1=zat[:], op=ALU.mult), [zat], [psB, yat])
                    DMA("sp", yat, lambda e, yat=yat, rows=rows: [e.dma_start(out=yT[0:8, :, rows].rearrange("k p t -> p k t"), in_=yat[:])],
                        reads=[yat])

                    xa = tmp("xa", [128, 8, 128], F32, 1)
                    for j in range(8):
                        I("dve", lambda e, j=j, xa=xa, xmt=xmt: e.tensor_scalar(
                            out=xa[:, j, :], in0=xmt[:, j, 0:128], scalar1=cw[:, j, 0:1], scalar2=cv[:, 0, j:j + 1],
                            op0=ALU.mult, op1=ALU.add), [xmt, cw, cv], [(xa, j)])
                    for k in range(1, 4):
                        for j in range(8):
                            I("dve", lambda e, j=j, k=k, xa=xa, xmt=xmt: e.scalar_tensor_tensor(
                                out=xa[:, j, :], in0=xmt[:, j, k:k + 128], scalar=cw[:, j, k:k + 1], in1=xa[:, j, :],
                                op0=ALU.mult, op1=ALU.add), [xmt, cw, (xa, j)], [(xa, j)])
                    xc = tmp("xc", [128, 8, 128], BF16)
                    I("act", lambda e, xa=xa, xc=xc: e.activation(out=xc[:], in_=xa[:], func=AF.Silu), [(xa, j) for j in range(8)], [xc])
                    qb_ = tmp("qb", [128, 4, 2, 128], BF16)
                    kb_ = tmp("kb", [128, 4, 2, 128], BF16)
                    qs_ = tmp("qs", [128, 4, 2, 128], BF16)
                    psq = [ps_next(), ps_next()]
                    for (wi, pss, dstb) in ((0, psq, qb_), (1, [ps_next(), ps_next()], kb_)):
                        for h in range(4):
                            pg = pss[h // 2]
                            for ec in range(2):
                                o0 = (h % 2) * 256 + ec * 128
                                for dc in range(2):
                                    I("pe", lambda e, wi=wi, pg=pg, h=h, ec=ec, dc=dc, o0=o0, xc=xc: e.matmul(
                                        pg[:, o0:o0 + 128], lhsT=Wb[:, wi, h * 2 + dc, ec * 128:(ec + 1) * 128], rhs=xc[:, h * 2 + dc, :],
                                        start=(dc == 0), stop=(dc == 1)), Wb_keys + [xc], [pg])
                        for bk in range(2):
                            I("act", lambda e, pss=pss, bk=bk, dstb=dstb, wi=wi: e.activation(
                                out=dstb[:, bk * 2:bk * 2 + 2, :, :], in_=pss[bk][:].rearrange("p (h c t) -> p h c t", h=2, c=2),
                                func=AF.Copy, scale=(1.0 if wi == 0 else 0.0625)), [], [pss[bk], (dstb, bk)])
                    psk = [ps_next(), ps_next()]
                    for h in range(4):
                        for dc in range(2):
                            I("pe", lambda e, h=h, dc=dc, xc=xc: e.matmul(
                                psk[h // 2][:, (h % 2) * 256:(h % 2 + 1) * 256], lhsT=xc[:, h * 2 + dc, :], rhs=Wb[:, 1, h * 2 + dc, :],
                                start=(dc == 0), stop=(dc == 1)), Wb_keys + [xc], [psk[h // 2]])
                    ktm = tmp("ktm", [128, 4, 256], BF16)
                    for bk in range(2):
                        I("act", lambda e, bk=bk, ktm=ktm: e.activation(
                            out=ktm[:, bk * 2:bk * 2 + 2, :], in_=psk[bk][:].rearrange("p (h e) -> p h e", h=2), func=AF.Copy,
                            scale=0.0625), [], [psk[bk], (ktm, bk)])
                    va_ = tmp("vaug", [128, 4, 260], BF16)
                    I("pool", lambda e, va_=va_: e.memset(va_[:, :, 256:257], 1.0), [], [(va_, "o")])
                    for bk in range(2):
                        psv = ps_next()
                        for hh in range(2):
                            h = bk * 2 + hh
                            for dc in range(2):
                                I("pe", lambda e, h=h, hh=hh, dc=dc, psv=psv, xmt=xmt: e.matmul(
                                    psv[:, hh * 256:(hh + 1) * 256], lhsT=xmt[:, h * 2 + dc, 3:131], rhs=Wb[:, 2, h * 2 + dc, :],
                                    start=(dc == 0), stop=(dc == 1)), Wb_keys + [xmt], [psv])
                        I("act", lambda e, psv=psv, bk=bk, va_=va_: e.activation(
                            out=va_[:, bk * 2:bk * 2 + 2, 0:256], in_=psv[:].rearrange("p (h e) -> p h e", h=2), func=AF.Copy),
                          [], [psv, (va_, bk)])
                    gs = tmp("gs", [128, 40], F32)
                    I("dve", lambda e, gs=gs, gtt=gtt: e.tensor_tensor(out=gs[:, 0:8], in0=gtt[:], in1=gb[:], op=ALU.add), [gtt, gb], [(gs, 0)])
                    I("act", lambda e, gs=gs: e.activation(out=gs[:, 8:12], in_=gs[:, 4:8], func=AF.Exp, scale=-1.0), [(gs, 0)], [(gs, 1)])
                    I("dve", lambda e, gs=gs: e.tensor_scalar(out=gs[:, 12:16], in0=gs[:, 8:12], scalar1=1.0, scalar2=None, op0=ALU.add),
                      [(gs, 1)], [(gs, 2)])
                    I("act", lambda e, gs=gs: e.activation(out=gs[:, 16:20], in_=gs[:, 12:16], func=AF.Ln), [(gs, 2)], [(gs, 3)])
                    psg = ps_next()
                    I("pe", lambda e, gs=gs, psg=psg: e.matmul(psg[:, 0:4], lhsT=tri_f[:], rhs=gs[:, 16:20], start=True, stop=True),
                      [tri_f, (gs, 3)], [psg])
                    I("dve", lambda e, gs=gs, psg=psg: e.tensor_copy(out=gs[:, 20:24], in_=psg[:, 0:4]), [], [psg, (gs, 4)])
                    I("dve", lambda e, gs=gs: e.tensor_tensor(out=gs[:, 24:28], in0=gs[:, 0:4], in1=gs[:, 20:24], op=ALU.add),
                      [(gs, 0), (gs, 4)], [(gs, 5)])
                    dg = tmp("dg", [128, 4, 128], F32, 1)
                    for h in range(4):
                        I("dve", lambda e, h=h, dg=dg, gs=gs: e.tensor_scalar(out=dg[:, h, :], in0=ident_f[:], scalar1=gs[:, 20 + h:21 + h],
                                                                              scalar2=-1.0, op0=ALU.mult, op1=ALU.mult),
                          [ident_f, (gs, 4)], [(dg, h)])
                    psr = ps_next()
                    I("pe", lambda e, psr=psr, dg=dg: e.matmul(psr[:], lhsT=ones_f[:], rhs=dg[:].rearrange("p h t -> p (h t)"),
                                                               start=True, stop=True), [ones_f] + [(dg, h) for h in range(4)], [psr])
                    PT = tmp("PT", [128, 4, 128], F32, 1)
                    for h in range(4):
                        I("act", lambda e, h=h, PT=PT, psr=psr, gs=gs: e.activation(
                            out=PT[:, h, :], in_=psr[:, h * 128:(h + 1) * 128], func=AF.Exp, bias=gs[:, 24 + h:25 + h]),
                          [(gs, 5)], [psr, (PT, h)])
                    EB = tmp("EB", [128, 4, 128], F32, 1)
                    I("act", lambda e, EB=EB, psr=psr: e.activation(out=EB[:].rearrange("p h t -> p (h t)"), in_=psr[:], func=AF.Exp),
                      [], [psr, EB])
                    I("dve", lambda e, gs=gs, psr=psr: e.tensor_copy(
                        out=gs[:, 28:32], in_=psr[:].rearrange("p (h t) -> p h t", h=4)[:, :, 127]), [], [psr, (gs, 6)])
                    I("pool", lambda e, PT=PT: e.tensor_tensor(out=PT[:], in0=PT[:], in1=tri4[:], op=ALU.mult),
                      tri4_keys + [(PT, h) for h in range(4)], [(PT, h) for h in range(4)])
                    if c > 0:
                        for bk in range(2):
                            for ec in range(2):
                                I("dve", lambda e, bk=bk, ec=ec, qs_=qs_, EB=EB, qb_=qb_: e.tensor_tensor(
                                    out=qs_[:, bk * 2:bk * 2 + 2, ec, :], in0=qb_[:, bk * 2:bk * 2 + 2, ec, :],
                                    in1=EB[:, bk * 2:bk * 2 + 2, :], op=ALU.mult), [EB, (qb_, bk)], [(qs_, bk, ec)])
                    psqk = ps_next()
                    for h in range(4):
                        for ec in range(2):
                            I("pe", lambda e, h=h, ec=ec, psqk=psqk, kb_=kb_, qb_=qb_: e.matmul(
                                psqk[:, h * 128:(h + 1) * 128], lhsT=kb_[:, h, ec, :], rhs=qb_[:, h, ec, :],
                                start=(ec == 0), stop=(ec == 1)), [(kb_, 0), (kb_, 1), (qb_, 0), (qb_, 1)], [psqk])
                    scT = tmp("scT", [128, 4, 128], BF16)
                    I("dve", lambda e, scT=scT, psqk=psqk, PT=PT: e.tensor_tensor(
                        out=scT[:].rearrange("p h t -> p (h t)"), in0=psqk[:], in1=PT[:].rearrange("p h t -> p (h t)"), op=ALU.mult),
                      [(PT, h) for h in range(4)], [psqk, scT])
                    htn = tmp("htn", [128, D], BF16)
                    for h in range(4):
                        acc = ps_next()
                        I("pe", lambda e, h=h, acc=acc, scT=scT, va_=va_: e.matmul(
                            acc[:, 0:257], lhsT=scT[:, h, :], rhs=va_[:, h, 0:257], start=True, stop=(c == 0)),
                          [scT, (va_, "o"), (va_, h // 2)], [acc])
                        if c > 0:
                            for dc in range(2):
                                I("pe", lambda e, h=h, dc=dc, acc=acc, qs_=qs_: e.matmul(
                                    acc[:, 0:257], lhsT=qs_[:, h, dc, :], rhs=Cb[:, h, dc, 0:257], start=False, stop=(dc == 1)),
                                  [(qs_, h // 2, dc), (Cb, h, dc)], [acc])
                        hs = tmp("hs", [128, 16], F32)
                        ht = tmp("ht", [128, 256], F32)
                        I("act", lambda e, hs=hs, acc=acc: e.activation(out=hs[:, 8:9], in_=acc[:, 256:257], func=AF.Abs),
                          [], [acc, (hs, 8)])
                        I("dve", lambda e, hs=hs: e.tensor_scalar(out=hs[:, 10:11], in0=hs[:, 8:9], scalar1=1.0, scalar2=None,
                                                                  op0=ALU.max), [(hs, 8)], [(hs, 10)])
                        I("dve", lambda e, hs=hs: e.reciprocal(out=hs[:, 9:10], in_=hs[:, 10:11]), [(hs, 10)], [(hs, 9)])
                        I("dve", lambda e, hs=hs, acc=acc, ht=ht, h=h, ogt=ogt: e.scalar_tensor_tensor(
                            out=ht[:], in0=acc[:, 0:256], scalar=hs[:, 9:10], in1=ogt[:, h * 256:(h + 1) * 256],
                            op0=ALU.mult, op1=ALU.mult), [(hs, 9), ogt], [acc, ht])
                        bs2 = tmp("bs2", [128, 6], F32)
                        I("dve", lambda e, bs2=bs2, ht=ht: e.bn_stats(out=bs2[:], in_=ht[:]), [ht], [bs2])
                        I("dve", lambda e, bs2=bs2, hs=hs: e.bn_aggr(out=hs[:, 0:2], in_=bs2[:]), [bs2], [(hs, 0), (hs, 1)])
                        rstd_chain(hs, 1, 1.0, 2)
                        I("dve", lambda e, hs=hs, ht=ht, htn=htn, h=h: e.tensor_scalar(
                            out=htn[:, h * 256:(h + 1) * 256], in0=ht[:], scalar1=hs[:, 0:1], scalar2=hs[:, 2:3],
                            op0=ALU.subtract, op1=ALU.mult), [ht, (hs, 0), (hs, 2)], [(htn, h)])
                    for g in range(8):
                        I("pe", lambda e, g=g, htn=htn: e.transpose(out=psB[:, g * 128:(g + 1) * 128], in_=htn[:, g * 128:(g + 1) * 128],
                                                                    identity=ident_b[:]), [(htn, g // 2), ident_b], [psB])
                    y1 = tmp("y1", [128, 8, 128], F32, 1)
                    y2 = tmp("y2", [128, 8, 128], F32, 1)
                    I("dve", lambda e, y1=y1: e.tensor_tensor(out=y1[:], in0=psB[:].rearrange("p (k t) -> p k t", k=8), in1=GN[:],
                                                              op=ALU.mult), GN_keys, [psB, y1])
                    I("pool", lambda e, y2=y2, xc=xc: e.tensor_tensor(out=y2[:], in0=xc[:], in1=SK[:], op=ALU.mult), SK_keys + [xc], [y2])
                    I("dve", lambda e, y1=y1, y2=y2: e.tensor_tensor(out=y1[:], in0=y1[:], in1=y2[:], op=ALU.add), [y1, y2], [y1])
                    ybt = ybp[b2]
                    I("dve", lambda e, y1=y1, ybt=ybt, zbt=zbt: e.tensor_tensor(out=ybt[:], in0=y1[:], in1=zbt[:], op=ALU.mult),
                      [y1, zbt], [ybt])
                    DMA("sp", ybt, lambda e, ybt=ybt, rows=rows: [e.dma_start(out=yT[8:16, :, rows].rearrange("k p t -> p k t"), in_=ybt[:])],
                        reads=[ybt])
                    if c < CPS - 1:
                        I("dve", lambda e, gs=gs: e.tensor_tensor(out=gs[:, 32:36], in0=gs[:, 24:28], in1=gs[:, 28:32], op=ALU.add),
                          [(gs, 5), (gs, 6)], [(gs, 7)])
                        I("act", lambda e, gs=gs: e.activation(out=gs[:, 32:36], in_=gs[:, 32:36], func=AF.Exp), [(gs, 7)], [(gs, 7)])
                        I("act", lambda e, gs=gs: e.activation(out=gs[:, 36:40], in_=gs[:, 28:32], func=AF.Exp), [(gs, 6)], [(gs, 8)])
                        kw = tmp("kw", [128, 4, 256], BF16)
                        for h in range(4):
                            I("dve", lambda e, h=h, kw=kw, gs=gs, ktm=ktm: e.tensor_scalar(
                                out=kw[:, h, :], in0=ktm[:, h, :], scalar1=gs[:, 32 + h:33 + h],
                                scalar2=None, op0=ALU.mult), [(gs, 7), (ktm, h // 2)], [(kw, h)])
                        for h in range(4):
                            for dc in range(2):
                                psu = ps_next()
                                I("pe", lambda e, h=h, dc=dc, psu=psu, kw=kw, va_=va_: e.matmul(
                                    psu[:, 0:257], lhsT=kw[:, h, dc * 128:(dc + 1) * 128], rhs=va_[:, h, 0:257], start=True, stop=True),
                                  [(kw, h), (va_, "o"), (va_, h // 2)], [psu])
                                if c == 0:
                                    I("dve", lambda e, h=h, dc=dc, psu=psu: e.tensor_copy(out=Cst[:, h, dc, 0:257], in_=psu[:, 0:257]),
                                      [], [psu, (Cst, h, dc)])
                                else:
                                    I("dve", lambda e, h=h, dc=dc, psu=psu, gs=gs: e.scalar_tensor_tensor(
                                        out=Cst[:, h, dc, 0:257], in0=Cst[:, h, dc, 0:257], scalar=gs[:, 36 + h:37 + h], in1=psu[:, 0:257],
                                        op0=ALU.mult, op1=ALU.add), [(gs, 8)], [psu, (Cst, h, dc)])
                                I("act", lambda e, h=h, dc=dc: e.activation(out=Cb[:, h, dc, 0:257], in_=Cst[:, h, dc, 0:257], func=AF.Copy),
                                  [(Cst, h, dc)], [(Cb, h, dc)])
                tile_loads(0)
                for tt in range(NT):
                    do_tile(tt)
                sc.barrier()

        import os as _os
        STOP = _os.environ.get("KSTOP", "")

        def phase_copyout():
            with ExitStack() as st:
                xp = pool(st, "cox", [128, D], F32, 2)
                for tt in range(NT):
                    xt = xp[tt % 2]
                    DMA("sp", xt, lambda e, xt=xt, tt=tt: [e.dma_start(out=xt[:], in_=xs[tt * 128:(tt + 1) * 128, :])], writes=[xt])
                    DMA("sp", xt, lambda e, xt=xt, tt=tt: [e.dma_start(out=out[tt * 128:(tt + 1) * 128, :], in_=xt[:])], reads=[xt])
                sc.barrier()

        def run_all():
            phase_norm0(layers[0][2])
            if STOP == "norm0":
                return phase_copyout()
            for li, (kind, pi, lno) in enumerate(layers):
                if kind == "odd":
                    phase_odd_inproj(pi)
                    if STOP == "inproj":
                        return phase_copyout()
                    phase_attn(pi, lno)
                    if STOP == "attn":
                        return phase_copyout()
                    phase_out(li, od_w_out[pi])
                else:
                    phase_even_inproj(pi)
                    if STOP == "inproj":
                        return phase_copyout()
                    phase_even_mix(pi)
                    if STOP == "attn":
                        return phase_copyout()
                    phase_out(li, ev_w_out[pi])

        run_all()
        sc.barrier(["sp"])
        sc.emit()
    return nc


def prep_inputs(inputs, S):
    f = lambda a: np.ascontiguousarray(np.asarray(a, dtype=np.float32))
    rep = lambda a: np.ascontiguousarray(np.broadcast_to(f(a)[None], (128,) + tuple(np.shape(a))))
    shared = {}
    shared["gvec"] = rep(np.concatenate([f(inputs["norm_g"]), f(inputs["final_g"])[None]], axis=0))
    shared["ident"] = np.eye(128, dtype=np.float32)
    shared["tri"] = np.triu(np.ones((128, 128), np.float32))
    cosT, sinT = rope_tables_T(S)
    shared["cosT"], shared["sinT"] = cosT, sinT
    shared["ev_w_in"] = f(inputs["ev_w_in"])
    shared["ev_w_out"] = f(inputs["ev_w_out"])
    w = f(inputs["od_w_in"])
    sw = np.concatenate([np.arange(32, 64), np.arange(0, 32)])
    cols = []
    for base in (0, 2048):
        for h in range(16):
            for cmp_ in range(2):
                cols.append(base + h * 128 + cmp_ * 64 + np.arange(64))
            for cmp_ in range(2):
                cols.append(base + h * 128 + cmp_ * 64 + sw)
    cols.append(6144 + np.arange(2048))
    cols.append(4096 + np.arange(2048))
    cols = np.concatenate(cols)
    shared["od_w_in"] = np.ascontiguousarray(w[:, :, cols])
    shared["od_w_out"] = f(inputs["od_w_out"])
    shared["a_ln_g"] = np.ascontiguousarray(np.broadcast_to(f(inputs["a_ln_g"])[:, None, :], (2, 128, D)))
    shared["a_wsT"] = np.ascontiguousarray(np.swapaxes(f(inputs["a_ws"]), 2, 3))
    shared["a_bsT"] = np.ascontiguousarray(np.swapaxes(f(inputs["a_bs"]), 1, 2))
    cw = f(inputs["b_conv_w"])
    shared["convw"] = np.ascontiguousarray(cw.reshape(2, 4, 8, 128).transpose(0, 3, 2, 1))
    colv = np.stack([f(inputs["b_conv_b"]), f(inputs["b_gn_g"]), f(inputs["b_skip"])], axis=1)
    shared["colv"] = np.ascontiguousarray(colv.reshape(2, 3, 8, 128).transpose(0, 3, 1, 2))
    shared["b_wqkv"] = np.ascontiguousarray(np.stack([f(inputs["b_wq"]), f(inputs["b_wk"]), f(inputs["b_wv"])], axis=1))
    gb = np.concatenate([f(inputs["b_ig_b"]), f(inputs["b_fg_b"])], axis=1)
    shared["gate_b"] = np.ascontiguousarray(np.broadcast_to(gb[:, None, :], (2, 128, 8)))
    lv = np.stack([f(inputs["c_lam_q1"]), f(inputs["c_lam_k1"]), f(inputs["c_lam_q2"]), f(inputs["c_lam_k2"])], axis=1)
    shared["lamv"] = np.ascontiguousarray(np.broadcast_to(lv[:, None], (2, 128, 4, 64)))
    shared["subln"] = np.ascontiguousarray(f(inputs["c_subln_g"]).reshape(2, 128, 1))
    return shared


_PROG_CACHE = {}


def kernel(**inputs):
    x = np.ascontiguousarray(inputs["x"], dtype=np.float32)
    B, S, _ = x.shape
    NS = B // NCORES
    key = (NS, S)
    if key not in _PROG_CACHE:
        _PROG_CACHE[key] = build_program(NS, S)
    nc = _PROG_CACHE[key]
    shared = prep_inputs(inputs, S)
    in_maps = []
    for c in range(NCORES):
        m = dict(shared)
        m["x"] = x[c * NS:(c + 1) * NS].reshape(NS * S, D)
        in_maps.append(m)
    res = run_bass_kernel_spmd(nc, in_maps, core_ids=list(range(NCORES)))
    outs = [r["out"].reshape(NS, S, D) for r in res.results]
    return np.concatenate(outs, axis=0)
```

```python
import math
from contextlib import ExitStack

import numpy as np
import concourse.bass as bass
import concourse.mybir as mybir
from concourse.bass_utils import run_bass_kernel_spmd

F32 = mybir.dt.float32
BF16 = mybir.dt.bfloat16
AF = mybir.ActivationFunctionType
ALU = mybir.AluOpType
AX = mybir.AxisListType

D = 1024
NCORES = 8
EPS = 1e-6
EVEN_IN = 6152
OD_EXT = 12288
N_DMA_SEMS = 84
FULL_LAYERS = (("even", 0, 0), ("odd", 0, 1), ("even", 1, 2), ("odd", 1, 3))


class Sched:
    ENGS = ("pe", "act", "dve", "pool", "sp")

    def __init__(self, nc, es):
        self.nc = nc
        self.es = es
        self.prog = {e: [] for e in self.ENGS}
        self.idx = {e: 0 for e in self.ENGS}
        self.seen = {e: {} for e in self.ENGS}
        self.cnt = {}
        self.last_w = {}
        self.readers = {}
        self.eng_sem = {}
        for e in ("pe", "act", "dve", "pool"):
            self.eng_sem[e] = self.new_sem("c_" + e)
        self.dma_sems = [self.new_sem(f"dq{i}") for i in range(N_DMA_SEMS)]
        self.dma_i = 0
        self.n_ops = 0

    def new_sem(self, name):
        s = self.es.enter_context(self.nc.semaphore(name))
        self.cnt[s] = 0
        return s

    def get_dma_sem(self):
        s = self.dma_sems[self.dma_i % N_DMA_SEMS]
        self.dma_i += 1
        return s

    def op(self, eng, fn, reads=(), writes=(), dma_sem=None, ndma=1):
        deps = []
        for k in reads:
            t = self.last_w.get(k)
            if t is not None:
                deps.append(t)
        for k in writes:
            t = self.last_w.get(k)
            if t is not None:
                deps.append(t)
            deps.extend(self.readers.get(k, ()))
        need = {}
        seen = self.seen[eng]
        cur = self.idx[eng]
        for (sem, val, teng, tidx, is_dma) in deps:
            if teng == eng and not is_dma:
                if eng == "pe":
                    continue
            if seen.get(sem, 0) >= val:
                continue
            if need.get(sem, 0) < val:
                need[sem] = val
        if dma_sem is not None:
            prev = self.cnt[dma_sem]
            if prev > 0 and seen.get(dma_sem, 0) < prev:
                need[dma_sem] = prev
        for sem, val in need.items():
            self.prog[eng].append(("w", sem, val))
            seen[sem] = val
        if dma_sem is None:
            sem = self.eng_sem[eng]
            self.cnt[sem] += 1
            is_dma = False
        else:
            sem = dma_sem
            self.cnt[sem] += 16 * ndma
            is_dma = True
        tok = (sem, self.cnt[sem], eng, cur, is_dma)
        self.prog[eng].append(("o", fn, sem))
        for k in writes:
            self.last_w[k] = tok
            self.readers[k] = []
        for k in reads:
            self.readers.setdefault(k, []).append(tok)
        self.idx[eng] = cur + 1
        self.n_ops += 1
        return tok

    def barrier(self, engs=None):
        for eng in (engs or self.ENGS):
            for sem, val in self.cnt.items():
                if val > 0 and self.seen[eng].get(sem, 0) < val:
                    self.prog[eng].append(("w", sem, val))
                    self.seen[eng][sem] = val
        if engs is None:
            self.last_w = {}
            self.readers = {}

    def emit(self):
        nc = self.nc
        engobj = {"pe": "tensor", "act": "scalar", "dve": "vector", "pool": "gpsimd", "sp": "sync"}
        with nc.Block() as block:
            for e in self.ENGS:
                prog = self.prog[e]

                def body(eng, prog=prog):
                    for it in prog:
                        if it[0] == "w":
                            eng.wait_ge(it[1], it[2])
                        else:
                            it[1](eng, it[2])

                getattr(block, engobj[e])(body)


class Tl:
    def __init__(self, t, sem=None):
        self.t = t
        self.sem = sem

    def __getitem__(self, k):
        return self.t[k]


def rope_tables_T(S):
    inv = (10000.0 ** (-np.arange(0, 64, 2, dtype=np.float32) / np.float32(64))).astype(np.float32)
    ang = (np.arange(S, dtype=np.float32)[:, None] * inv[None, :]).astype(np.float32)
    cos, sin = np.cos(ang).astype(np.float32), np.sin(ang).astype(np.float32)
    cosT = np.zeros((128, S), np.float32)
    sinT = np.zeros((128, S), np.float32)
    for p in range(128):
        d = p % 64
        cosT[p] = cos[:, d % 32]
        sinT[p] = -sin[:, d % 32] if d < 32 else sin[:, d % 32]
    return cosT, sinT


def lambda_init(layer):
    return 0.8 - 0.6 * math.exp(-0.3 * layer)


def build_program(NS, S, layers=FULL_LAYERS, dbg=False):
    T = NS * S
    NT = T // 128
    NC5 = T // 512
    CPS = S // 128
    n_layers = len(layers)
    nc = bass.Bass("TRN2", target_bir_lowering=False)
    es = ExitStack()

    def din(name, shape, dt=F32):
        return nc.dram_tensor(name, list(shape), dt, kind="ExternalInput").ap()

    def dscr(name, shape, dt):
        if dbg:
            return nc.dram_tensor(name, list(shape), dt, kind="ExternalOutput").ap()
        return nc.dram_tensor(name, list(shape), dt).ap()

    x_in = din("x", [T, D])
    gvec = din("gvec", [128, 5, D])
    ident_d = din("ident", [128, 128])
    tri_d = din("tri", [128, 128])
    cosT_d = din("cosT", [128, S])
    sinT_d = din("sinT", [128, S])
    ev_w_in = din("ev_w_in", [2, D, EVEN_IN])
    ev_w_out = din("ev_w_out", [2, 2048, D])
    od_w_in = din("od_w_in", [2, D, OD_EXT])
    od_w_out = din("od_w_out", [2, 2048, D])
    a_ln_g = din("a_ln_g", [2, 128, D])
    a_wsT = din("a_wsT", [2, 8, 128, 128])
    a_bsT = din("a_bsT", [2, 128, 8])
    convw = din("convw", [2, 128, 8, 4])
    colv = din("colv", [2, 128, 3, 8])
    b_wqkv = din("b_wqkv", [2, 3, 4, 256, 256])
    gate_b = din("gate_b", [2, 128, 8])
    lamv = din("lamv", [2, 128, 4, 64])
    subln = din("subln", [2, 128, 1])
    out = nc.dram_tensor("out", [T, D], F32, kind="ExternalOutput").ap()

    xs = dscr("xs", [T, D], F32)
    hnT = dscr("hnT", [8, 128, T], BF16)
    yT = dscr("yT", [16, 128, T], BF16)
    qT_s = dscr("qT_s", [16, 128, T], BF16)
    kT_s = dscr("kT_s", [16, 128, T], BF16)
    zT_s = dscr("zT_s", [16, 128, T], BF16)
    v_s = dscr("v_s", [T, 2048], BF16)
    u_s = dscr("u_s", [T, D], BF16)
    va_s = dscr("va_s", [T, D], BF16)
    og_s = dscr("og_s", [T, D], BF16)
    gt_s = dscr("gt_s", [T, 8], F32)
    zaT_s = dscr("zaT_s", [8, 128, T], BF16)
    xmT_s = dscr("xmT_s", [8, 128, T], BF16)
    zbT_s = dscr("zbT_s", [8, 128, T], BF16)

    with es:
        sc = Sched(nc, es)

        def I(eng, f, reads=(), writes=()):
            sc.op(eng, lambda e, s: f(e).then_inc(s, 1), reads, writes)

        def DMA(eng, tl, f, reads=(), writes=(), n=1):
            def g(e, s):
                r = f(e)
                assert len(r) == n
                for x in r:
                    x.then_inc(s, 16)
            sc.op(eng, g, reads, writes, dma_sem=tl.sem, ndma=n)

        uid = [0]

        def sbt(st, name, shape, dt, dma=True):
            uid[0] += 1
            t = st.enter_context(nc.sbuf_tensor(f"{name}_{uid[0]}", list(shape), dt))
            return Tl(t, sc.get_dma_sem() if dma else None)

        def pool(st, name, shape, dt, n, dma=True):
            return [sbt(st, f"{name}{i}", shape, dt, dma) for i in range(n)]

        ident_f = sbt(es, "ident_f", [128, 128], F32)
        ident_b = sbt(es, "ident_b", [128, 128], BF16)
        tri_f = sbt(es, "tri_f", [128, 128], F32)
        tri_b = sbt(es, "tri_b", [128, 128], BF16)
        ones_f = sbt(es, "ones_f", [128, 128], F32)
        psF = [Tl(es.enter_context(nc.psum_tensor(f"psF{i}", [128, 512], F32))) for i in range(7)]
        psB = Tl(es.enter_context(nc.psum_tensor("psB", [128, 1024], BF16)))
        ps_i = [0]

        def ps_next():
            p = psF[ps_i[0] % len(psF)]
            ps_i[0] += 1
            return p

        DMA("sp", ident_f, lambda e: [e.dma_start(out=ident_f[:], in_=ident_d[:, :])], writes=[ident_f])
        DMA("sp", tri_f, lambda e: [e.dma_start(out=tri_f[:], in_=tri_d[:, :])], writes=[tri_f])
        I("dve", lambda e: e.tensor_copy(out=ident_b[:], in_=ident_f[:]), [ident_f], [ident_b])
        I("dve", lambda e: e.tensor_copy(out=tri_b[:], in_=tri_f[:]), [tri_f], [tri_b])
        I("dve", lambda e: e.memset(ones_f[:], 1.0), [], [ones_f])

        def rstd_chain(st, ss_col, scale, out_col):
            I("dve", lambda e: e.tensor_scalar(out=st[:, 6:7], in0=st[:, ss_col:ss_col + 1], scalar1=scale,
                                               scalar2=EPS, op0=ALU.mult, op1=ALU.add),
              [(st, ss_col)], [(st, 6)])
            I("act", lambda e: e.activation(out=st[:, 7:8], in_=st[:, 6:7], func=AF.Ln), [(st, 6)], [(st, 7)])
            I("act", lambda e: e.activation(out=st[:, out_col:out_col + 1], in_=st[:, 7:8], func=AF.Exp, scale=-0.5),
              [(st, 7)], [(st, out_col)])

        def norm_to_hnT(st_pools, xt, tt, gt_, hT, final=False):
            junk, stat, hnp, otp = st_pools
            jt = junk[tt % len(junk)]
            st = stat[tt % len(stat)]
            I("act", lambda e: e.activation(out=jt[:], in_=xt[:], func=AF.Square, accum_out=st[:, 0:1]),
              [xt], [jt, (st, 0)])
            rstd_chain(st, 0, 1.0 / D, 1)
            if final:
                ot = otp[tt % len(otp)]
                I("dve", lambda e: e.scalar_tensor_tensor(out=ot[:], in0=xt[:], scalar=st[:, 1:2], in1=gt_[:],
                                                          op0=ALU.mult, op1=ALU.mult), [xt, (st, 1), gt_], [ot])
                DMA("sp", ot, lambda e: [e.dma_start(out=out[tt * 128:(tt + 1) * 128, :], in_=ot[:])], reads=[ot])
                return
            hn = hnp[tt % len(hnp)]
            I("dve", lambda e: e.scalar_tensor_tensor(out=hn[:], in0=xt[:], scalar=st[:, 1:2], in1=gt_[:],
                                                      op0=ALU.mult, op1=ALU.mult), [xt, (st, 1), gt_], [hn])
            for kc in range(8):
                I("pe", lambda e, kc=kc: e.transpose(out=psB[:, kc * 128:(kc + 1) * 128],
                                                     in_=hn[:, kc * 128:(kc + 1) * 128], identity=ident_b[:]),
                  [hn, ident_b], [psB])
            j = tt % 4
            I("act", lambda e: e.activation(out=hT[:, :, j * 128:(j + 1) * 128],
                                            in_=psB[:].rearrange("p (k t) -> p k t", k=8), func=AF.Copy),
              [], [psB, (hT, j)])
            if j == 3:
                c = tt // 4
                DMA("sp", hT, lambda e: [e.dma_start(out=hnT[:, :, c * 512:(c + 1) * 512].rearrange("k p t -> p k t"),
                                                     in_=hT[:])],
                    reads=[(hT, 0), (hT, 1), (hT, 2), (hT, 3)])

        def phase_norm0(gidx):
            with ExitStack() as st:
                xp = pool(st, "n0x", [128, D], F32, 3)
                pools = (pool(st, "n0j", [128, D], F32, 2, False), pool(st, "n0s", [128, 8], F32, 4, False),
                         pool(st, "n0h", [128, D], BF16, 2, False), None)
                hTp = pool(st, "n0T", [128, 8, 512], BF16, 2)
                gt_ = sbt(st, "gt0", [128, D], F32)
                DMA("sp", gt_, lambda e: [e.dma_start(out=gt_[:], in_=gvec[:, gidx, :])], writes=[gt_])
                for tt in range(NT):
                    xt = xp[tt % 3]
                    DMA("sp", xt, lambda e, xt=xt, tt=tt: [e.dma_start(out=xt[:], in_=x_in[tt * 128:(tt + 1) * 128, :])],
                        writes=[xt])
                    DMA("sp", xt, lambda e, xt=xt, tt=tt: [e.dma_start(out=xs[tt * 128:(tt + 1) * 128, :], in_=xt[:])],
                        reads=[xt])
                    norm_to_hnT(pools, xt, tt, gt_, hTp[(tt // 4) % 2])
                sc.barrier()

        def load_w_stage(w_dram_2d, c0, ncols, stage):
            DMA("sp", stage, lambda e: [e.dma_start(out=stage[:, :, 0:ncols],
                                                    in_=w_dram_2d[:, c0:c0 + ncols].rearrange("(kc p) c -> p kc c", p=128))],
                writes=[stage])

        def cast_w(wt, ncols, stage):
            I("act", lambda e: e.activation(out=wt[:, :, 0:ncols], in_=stage[:, :, 0:ncols], func=AF.Copy), [stage], [wt])

        def load_hT(hT, c):
            DMA("sp", hT, lambda e: [e.dma_start(out=hT[:], in_=hnT[:, :, c * 512:(c + 1) * 512].rearrange("k p t -> p k t"))],
                writes=[hT])

        def proj_fm(wt, units, evac, hTp, mid=None):
            load_hT(hTp[0], 0)
            for c in range(NC5):
                hT = hTp[c % len(hTp)]
                if c + 1 < NC5:
                    load_hT(hTp[(c + 1) % len(hTp)], c + 1)
                for ui, cols in enumerate(units):
                    pss = []
                    for co in cols:
                        ps = ps_next()
                        for kc in range(8):
                            I("pe", lambda e, ps=ps, kc=kc, co=co, hT=hT: e.matmul(
                                ps[:], lhsT=wt[:, kc, co:co + 128], rhs=hT[:, kc, :], start=(kc == 0), stop=(kc == 7)),
                              [wt, hT], [ps])
                        pss.append(ps)
                    evac(ui, c, pss)
                if mid is not None and c == min(4, NC5 - 1):
                    mid()

        def proj_tm(wt, ncols, evac, hTp, mid=None):
            load_hT(hTp[0], 0)
            for c in range(NC5):
                hT = hTp[c % len(hTp)]
                if c + 1 < NC5:
                    load_hT(hTp[(c + 1) % len(hTp)], c + 1)
                for j in range(4):
                    tt = c * 4 + j
                    for half in range((ncols + 511) // 512):
                        w = min(512, ncols - half * 512)
                        ps = ps_next()
                        for kc in range(8):
                            I("pe", lambda e, ps=ps, kc=kc, half=half, w=w, j=j, hT=hT: e.matmul(
                                ps[:, 0:w], lhsT=hT[:, kc, j * 128:(j + 1) * 128],
                                rhs=wt[:, kc, half * 512:half * 512 + w], start=(kc == 0), stop=(kc == 7)),
                              [wt, hT], [ps])
                        evac(tt, half, ps, w)
                if mid is not None and c == min(4, NC5 - 1):
                    mid()

        def phase_out(li, w_out_2d):
            final = (li == n_layers - 1)
            gidx = 4 if final else layers[li + 1][2]
            with ExitStack() as st:
                wo = sbt(st, "wo", [128, 16, D], BF16, False)
                wos = sbt(st, "wos", [128, 8, D], F32)
                for hf in range(2):
                    DMA("sp", wos, lambda e, hf=hf: [e.dma_start(
                        out=wos[:], in_=w_out_2d[hf * 1024:(hf + 1) * 1024, :].rearrange("(kc p) c -> p kc c", p=128))], writes=[wos])
                    I("act", lambda e, hf=hf: e.activation(out=wo[:, hf * 8:(hf + 1) * 8, :], in_=wos[:], func=AF.Copy), [wos], [(wo, hf)])
                gt_ = sbt(st, "gt", [128, D], F32)
                DMA("sp", gt_, lambda e: [e.dma_start(out=gt_[:], in_=gvec[:, gidx, :])], writes=[gt_])
                yp = pool(st, "poy", [128, 16, 512], BF16, 2)
                xp = pool(st, "pox", [128, D], F32, 4)
                pools = (pool(st, "poj", [128, D], F32, 2, False), pool(st, "pos", [128, 8], F32, 4, False),
                         pool(st, "poh", [128, D], BF16, 2, False), pool(st, "poo", [128, D], F32, 2))
                hTp = pool(st, "poT", [128, 8, 512], BF16, 2)
                def load_y(c):
                    yt = yp[c % 2]
                    DMA("sp", yt, lambda e: [e.dma_start(
                        out=yt[:, hf * 8:(hf + 1) * 8, :], in_=yT[hf * 8:(hf + 1) * 8, :, c * 512:(c + 1) * 512].rearrange("k p t -> p k t"))
                        for hf in range(2)], writes=[yt], n=2)

                load_y(0)

                def stage_a(tt):
                    c, j = tt // 4, tt % 4
                    yt = yp[c % 2]
                    if j == 1 and c + 1 < NC5:
                        load_y(c + 1)
                    xt = xp[tt % 4]
                    DMA("sp", xt, lambda e: [e.dma_start(out=xt[:], in_=xs[tt * 128:(tt + 1) * 128, :])], writes=[xt])
                    for half in range(2):
                        ps = ps_next()
                        for kc in range(16):
                            I("pe", lambda e, ps=ps, kc=kc, half=half: e.matmul(
                                ps[:], lhsT=yt[:, kc, j * 128:(j + 1) * 128],
                                rhs=wo[:, kc, half * 512:(half + 1) * 512], start=(kc == 0), stop=(kc == 15)),
                              [(wo, 0), (wo, 1), yt], [ps])
                        I("dve", lambda e, ps=ps, half=half: e.tensor_tensor(
                            out=xt[:, half * 512:(half + 1) * 512], in0=ps[:], in1=xt[:, half * 512:(half + 1) * 512],
                            op=ALU.add), [], [ps, xt])
                    if not final:
                        DMA("sp", xt, lambda e: [e.dma_start(out=xs[tt * 128:(tt + 1) * 128, :], in_=xt[:])], reads=[xt])

                def stage_b(tt):
                    norm_to_hnT(pools, xp[tt % 4], tt, gt_, hTp[(tt // 4) % 2], final=final)

                stage_a(0)
                for tt in range(NT):
                    if tt + 1 < NT:
                        stage_a(tt + 1)
                    stage_b(tt)
                sc.barrier()

        def phase_odd_inproj(o):
            W = od_w_in[o]
            with ExitStack() as st:
                wtp = pool(st, "oiw", [128, 8, 1024], BF16, 2, False)
                wstage = sbt(st, "oiws", [128, 8, 1024], F32)
                hTp = pool(st, "oih", [128, 8, 512], BF16, 3)
                cosT = sbt(st, "cosT", [128, S], F32)
                sinT = sbt(st, "sinT", [128, S], F32)
                DMA("sp", cosT, lambda e: [e.dma_start(out=cosT[:], in_=cosT_d[:, :])], writes=[cosT])
                DMA("sp", sinT, lambda e: [e.dma_start(out=sinT[:], in_=sinT_d[:, :])], writes=[sinT])
                t1p = pool(st, "oit1", [128, 512], F32, 2, False)
                t2p = pool(st, "oit2", [128, 512], F32, 2, False)
                stg = pool(st, "oist", [128, 512], BF16, 4)
                cnt = [0]
                load_w_stage(W, 0, 1024, wstage)
                cast_w(wtp[0], 1024, wstage)
                for g in range(12):
                    wt = wtp[g % 2]
                    mid = None
                    if g + 1 < 12:
                        load_w_stage(W, (g + 1) * 1024, 1024, wstage)
                        mid = (lambda g=g: cast_w(wtp[(g + 1) % 2], 1024, wstage))
                    if g < 8:
                        dst = qT_s if g < 4 else kT_s
                        units = [(h * 256, h * 256 + 128) for h in range(4)]

                        def evac(ui, c, pss, g=g, dst=dst):
                            i = cnt[0]
                            cnt[0] += 1
                            t1, t2, sg = t1p[i % 2], t2p[i % 2], stg[i % 4]
                            p0 = (c * 512) % S
                            I("dve", lambda e: e.tensor_tensor(out=t1[:], in0=pss[0][:], in1=cosT[:, p0:p0 + 512], op=ALU.mult),
                              [cosT], [pss[0], t1])
                            I("dve", lambda e: e.tensor_tensor(out=t2[:], in0=pss[1][:], in1=sinT[:, p0:p0 + 512], op=ALU.mult),
                              [sinT], [pss[1], t2])
                            I("pool", lambda e: e.tensor_tensor(out=sg[:], in0=t1[:], in1=t2[:], op=ALU.add), [t1, t2], [sg])
                            h = (g % 4) * 4 + ui
                            DMA("sp", sg, lambda e: [e.dma_start(out=dst[h, :, c * 512:(c + 1) * 512], in_=sg[:])], reads=[sg])
                        proj_fm(wt, units, evac, hTp, mid)
                    elif g < 10:
                        units = [(j * 128,) for j in range(8)]

                        def evac(ui, c, pss, g=g):
                            i = cnt[0]
                            cnt[0] += 1
                            sg = stg[i % 4]
                            I("act", lambda e: e.activation(out=sg[:], in_=pss[0][:], func=AF.Silu), [], [pss[0], sg])
                            h = (g - 8) * 8 + ui
                            DMA("sp", sg, lambda e: [e.dma_start(out=zT_s[h, :, c * 512:(c + 1) * 512], in_=sg[:])], reads=[sg])
                        proj_fm(wt, units, evac, hTp, mid)
                    else:
                        def evac(tt, half, ps, w, g=g):
                            i = cnt[0]
                            cnt[0] += 1
                            sg = stg[i % 4]
                            I("act", lambda e: e.activation(out=sg[:], in_=ps[:], func=AF.Copy), [], [ps, sg])
                            c0 = (g - 10) * 1024 + half * 512
                            DMA("sp", sg, lambda e: [e.dma_start(out=v_s[tt * 128:(tt + 1) * 128, c0:c0 + 512], in_=sg[:])],
                                reads=[sg])
                        proj_tm(wt, 1024, evac, hTp, mid)
                sc.barrier()

        def phase_attn(o, layer_no):
            li = lambda_init(layer_no)
            with ExitStack() as st:
                lv = sbt(st, "lv", [128, 4, 64], F32)
                sl = sbt(st, "sl", [128, 8], F32, False)
                gcol = sbt(st, "gcol", [128, 1], F32)
                lj = sbt(st, "lj", [128, 64], F32, False)
                DMA("sp", lv, lambda e: [e.dma_start(out=lv[:], in_=lamv[o])], writes=[lv])
                DMA("sp", gcol, lambda e: [e.dma_start(out=gcol[:], in_=subln[o])], writes=[gcol])
                for i2 in range(2):
                    I("dve", lambda e, i2=i2: e.tensor_tensor(out=lj[:], in0=lv[:, 2 * i2, :], in1=lv[:, 2 * i2 + 1, :], op=ALU.mult),
                      [lv], [lj])
                    I("dve", lambda e, i2=i2: e.reduce_sum(out=sl[:, i2:i2 + 1], in_=lj[:], axis=AX.X), [lj], [(sl, i2)])
                    I("act", lambda e, i2=i2: e.activation(out=sl[:, 2 + i2:3 + i2], in_=sl[:, i2:i2 + 1], func=AF.Exp),
                      [(sl, i2)], [(sl, 2 + i2)])
                I("dve", lambda e: e.tensor_tensor(out=sl[:, 5:6], in0=sl[:, 3:4], in1=sl[:, 2:3], op=ALU.subtract),
                  [(sl, 2), (sl, 3)], [(sl, 5)])
                I("dve", lambda e: e.tensor_scalar(out=sl[:, 4:5], in0=sl[:, 5:6], scalar1=-li, scalar2=None, op0=ALU.add),
                  [(sl, 5)], [(sl, 4)])
                I("dve", lambda e: e.tensor_scalar(out=gcol[:], in0=gcol[:], scalar1=1.0 - li, scalar2=None, op0=ALU.mult),
                  [gcol], [gcol])

                NG = S // 256
                qp = pool(st, "aqq", [128, NG, 2, 256], BF16, 2)
                for b_ in range(2):
                    I("dve", lambda e, b_=b_: e.memset(qp[b_][64:128, :, 0, :], 0.0), [], [(qp[b_], "z0")])
                    I("dve", lambda e, b_=b_: e.memset(qp[b_][0:64, :, 1, :], 0.0), [], [(qp[b_], "z1")])
                kp = pool(st, "ak", [128, S], BF16, 2)
                zp = pool(st, "az", [128, S], BF16, 2)
                vp = pool(st, "av", [128, CPS, 132], BF16, 2)
                yp = pool(st, "ay", [128, S], BF16, 2)
                pp = pool(st, "ap", [128, 512], BF16, 6, False)
                stp = pool(st, "ast", [128, 8], F32, 8, False)
                o1p = pool(st, "ao1", [128, 128], F32, 8, False)
                o2p = pool(st, "ao2", [128, 128], F32, 8, False)
                jkp = pool(st, "ajk", [128, 128], F32, 8, False)
                onp = pool(st, "aon", [128, 128], BF16, 8, False)
                for v_ in vp:
                    I("dve", lambda e, v_=v_: e.memset(v_[:, :, 128:129], 1.0), [], [(v_, "ones")])
                it = 0
                qb = 0
                epi_i = [0]
                gstep = [0]
                pending = []

                def defer(delay, fn):
                    pending.append([gstep[0] + delay, fn, it])

                def run_due(force=False, owner_lt=None):
                    keep = []
                    for item in list(pending):
                        if force or item[0] <= gstep[0] or (owner_lt is not None and item[2] < owner_lt):
                            item[1]()
                        else:
                            keep.append(item)
                    pending[:] = keep

                def issue_loads(idx):
                    s_, h = idx // 16, idx % 16
                    qq, k, z, v = qp[idx % 2], kp[idx % 2], zp[idx % 2], vp[idx % 2]
                    t0 = s_ * S
                    DMA("sp", qq, lambda e: [
                        e.dma_start(out=qq[0:64, :, 0, :], in_=qT_s[h, 0:64, t0:t0 + S].rearrange("p (g q) -> p g q", q=256)),
                        e.dma_start(out=qq[64:128, :, 1, :], in_=qT_s[h, 64:128, t0:t0 + S].rearrange("p (g q) -> p g q", q=256))],
                        writes=[qq], n=2)
                    DMA("sp", k, lambda e: [e.dma_start(out=k[:], in_=kT_s[h, :, t0:t0 + S])], writes=[k])
                    DMA("sp", z, lambda e: [e.dma_start(out=z[:], in_=zT_s[h, :, t0:t0 + S])], writes=[z])
                    nvp = (CPS + 7) // 8
                    DMA("sp", v, lambda e: [e.dma_start(
                        out=v[:, j8 * 8:min(CPS, j8 * 8 + 8), 0:128],
                        in_=v_s[t0 + j8 * 1024:min(t0 + S, t0 + j8 * 1024 + 1024), h * 128:(h + 1) * 128].rearrange(
                            "(c p) e -> p c e", p=128)) for j8 in range(nvp)], writes=[v], n=nvp)

                issue_loads(0)
                for s in range(NS):
                    for h in range(16):
                        qq, k, z, v, y = qp[it % 2], kp[it % 2], zp[it % 2], vp[it % 2], yp[it % 2]
                        it += 1
                        t0 = s * S
                        steps = [(G, i) for G in range(CPS // 2) for i in range(2 * G + 2)]
                        info = {}

                        def emit_qk(n, qq=qq, k=k):
                            nonlocal qb
                            G, i = steps[n]
                            r = 1 if i == 2 * G + 1 else 0
                            sps = psF[4 + qb % 3]
                            pt = pp[qb % 4]
                            qb += 1
                            info[n] = (r, sps, pt)
                            qdeps = [k, qq, (qq, "z0"), (qq, "z1")]
                            if r == 0:
                                I("pe", lambda e, sps=sps, i=i, G=G: e.matmul(
                                    sps[:, 0:512], lhsT=k[:, i * 128:(i + 1) * 128],
                                    rhs=qq[:, G, :, :].rearrange("p c q -> p (c q)"), start=True, stop=True), qdeps, [sps])
                            else:
                                for cmp_ in range(2):
                                    I("pe", lambda e, sps=sps, cmp_=cmp_, i=i, G=G: e.matmul(
                                        sps[:, cmp_ * 256:cmp_ * 256 + 128], lhsT=k[:, i * 128:(i + 1) * 128],
                                        rhs=qq[:, G, cmp_, 128:256], start=True, stop=True), qdeps, [sps])

                        def emit_rest(n, v=v):
                            G, i = steps[n]
                            r, sps, pt = info.pop(n)
                            if r == 0:
                                I("act", lambda e, sps=sps, pt=pt: e.activation(out=pt[:], in_=sps[:], func=AF.Exp, scale=0.125),
                                  [], [sps, pt])
                            else:
                                I("act", lambda e, sps=sps, pt=pt: e.activation(
                                    out=pt[:].rearrange("p (c q) -> p c q", c=2)[:, :, 0:128],
                                    in_=sps[:].rearrange("p (c q) -> p c q", c=2)[:, :, 0:128], func=AF.Exp, scale=0.125),
                                  [], [sps, pt])
                            if i >= 2 * G:
                                for cmp_ in range(2):
                                    ds_ = slice(cmp_ * 256, cmp_ * 256 + 128)
                                    I("dve", lambda e, pt=pt, ds_=ds_: e.tensor_tensor(
                                        out=pt[:, ds_], in0=pt[:, ds_], in1=tri_b[:], op=ALU.mult), [tri_b], [pt])
                            for ql in range(r, 2):
                                for cmp_ in range(2):
                                    acc = psF[(G % 2) * 2 + ql]
                                    col = cmp_ * 256 + (ql - r) * 128
                                    first = (i == 0 and cmp_ == 0)
                                    last = (i == 2 * G + ql)
                                    I("pe", lambda e, acc=acc, pt=pt, col=col, i=i, v=v, first=first, last=last, cmp_=cmp_: e.matmul(
                                        acc[:, cmp_ * 256:cmp_ * 256 + 129], lhsT=pt[:, col:col + 128], rhs=v[:, i, 0:129],
                                        start=first, stop=last, skip_group_check=True),
                                      [pt, v, (v, "ones")], [acc])

                        def emit_epilogue(G, y=y, z=z):
                            par = G % 2
                            bufs = []
                            for ql in range(2):
                                ab = psF[par * 2 + ql]
                                bi = epi_i[0] % 8
                                epi_i[0] += 1
                                stt, o1, o2, jk, on = stp[bi], o1p[bi], o2p[bi], jkp[bi], onp[bi]
                                bufs.append((stt, o2, on))
                                I("dve", lambda e, ab=ab, stt=stt: e.reciprocal(out=stt[:, 0:1], in_=ab[:, 128:129]), [], [ab, (stt, 0)])
                                I("dve", lambda e, ab=ab, stt=stt: e.reciprocal(out=stt[:, 2:3], in_=ab[:, 384:385]), [], [ab, (stt, 2)])
                                I("dve", lambda e, stt=stt: e.tensor_tensor(out=stt[:, 3:4], in0=stt[:, 2:3], in1=sl[:, 4:5], op=ALU.mult),
                                  [(stt, 2), (sl, 4)], [(stt, 3)])
                                I("dve", lambda e, ab=ab, stt=stt, o1=o1: e.tensor_scalar(
                                    out=o1[:], in0=ab[:, 0:128], scalar1=stt[:, 0:1], scalar2=None, op0=ALU.mult),
                                  [(stt, 0)], [ab, o1])
                                I("dve", lambda e, ab=ab, stt=stt, o1=o1, o2=o2: e.scalar_tensor_tensor(
                                    out=o2[:], in0=ab[:, 256:384], scalar=stt[:, 3:4], in1=o1[:], op0=ALU.mult, op1=ALU.add),
                                  [(stt, 3), o1], [ab, o2])
                                I("dve", lambda e, o2=o2, jk=jk, stt=stt: e.scalar_tensor_tensor(
                                    out=jk[:], in0=o2[:], scalar=1.0, in1=o2[:], op0=ALU.mult, op1=ALU.mult, accum_out=stt[:, 4:5]),
                                  [o2], [jk, (stt, 4)])
                                I("dve", lambda e, stt=stt: e.tensor_scalar(out=stt[:, 6:7], in0=stt[:, 4:5], scalar1=1.0 / 128,
                                                                            scalar2=EPS, op0=ALU.mult, op1=ALU.add), [(stt, 4)], [(stt, 6)])

                            def s2():
                                for (stt, o2, on) in bufs:
                                    I("act", lambda e, stt=stt: e.activation(out=stt[:, 7:8], in_=stt[:, 6:7], func=AF.Ln), [(stt, 6)], [(stt, 7)])
                                    I("act", lambda e, stt=stt: e.activation(out=stt[:, 5:6], in_=stt[:, 7:8], func=AF.Exp, scale=-0.5),
                                      [(stt, 7)], [(stt, 5)])

                            def s3():
                                for (stt, o2, on) in bufs:
                                    I("dve", lambda e, o2=o2, on=on, stt=stt: e.tensor_scalar(
                                        out=on[:], in0=o2[:], scalar1=stt[:, 5:6], scalar2=None, op0=ALU.mult), [o2, (stt, 5)], [on])

                            def s4():
                                for ql, (stt, o2, on) in enumerate(bufs):
                                    I("pe", lambda e, on=on, ql=ql: e.transpose(out=psB[:, ql * 128:(ql + 1) * 128], in_=on[:],
                                                                                identity=ident_b[:]), [on, ident_b], [psB])

                            def s5():
                                qcol = 2 * G * 128
                                I("dve", lambda e: e.scalar_tensor_tensor(
                                    out=y[:, qcol:qcol + 256], in0=psB[:, 0:256], scalar=gcol[:, 0:1], in1=z[:, qcol:qcol + 256],
                                    op0=ALU.mult, op1=ALU.mult), [gcol, z], [psB, y])

                            defer(4, s2)
                            defer(6, s3)
                            defer(8, s4)
                            defer(10, s5)

                        LA = 2
                        for n0 in range(min(LA, len(steps))):
                            emit_qk(n0)
                        for n in range(len(steps)):
                            if n + LA < len(steps):
                                emit_qk(n + LA)
                            emit_rest(n)
                            gstep[0] += 1
                            run_due()
                            if n == min(14, len(steps) - 1) and it < NS * 16:
                                run_due(owner_lt=it)
                                issue_loads(it)
                            G, i = steps[n]
                            if i == 2 * G + 1:
                                emit_epilogue(G)
                        defer(12, lambda y=y, h=h, t0=t0: DMA(
                            "sp", y, lambda e: [e.dma_start(out=yT[h, :, t0:t0 + S], in_=y[:])], reads=[y]))
                run_due(force=True)
                sc.barrier()


        def gelu_evac(ps, w, dstt, tmps):
            t1, t2 = tmps
            I("act", lambda e: e.activation(out=t1[:, 0:w], in_=ps[:, 0:w], func=AF.Square), [], [ps, t1])
            I("dve", lambda e: e.tensor_scalar(out=t1[:, 0:w], in0=t1[:, 0:w], scalar1=0.044715, scalar2=1.0,
                                               op0=ALU.mult, op1=ALU.add), [t1], [t1])
            I("dve", lambda e: e.tensor_tensor(out=t2[:, 0:w], in0=ps[:, 0:w], in1=t1[:, 0:w], op=ALU.mult), [t1], [ps, t2])
            I("act", lambda e: e.activation(out=t1[:, 0:w], in_=t2[:, 0:w], func=AF.Sigmoid, scale=1.5957691216057308),
              [t2], [t1])
            I("dve", lambda e: e.tensor_tensor(out=dstt[:, 0:w], in0=ps[:, 0:w], in1=t1[:, 0:w], op=ALU.mult), [t1], [ps, dstt])

        def phase_even_inproj(ei):
            W = ev_w_in[ei]
            with ExitStack() as st:
                wtp = pool(st, "eiw", [128, 8, 1024], BF16, 2, False)
                wstage = sbt(st, "eiws", [128, 8, 1024], F32)
                hTp = pool(st, "eih", [128, 8, 512], BF16, 3)
                t1p = pool(st, "eit1", [128, 512], F32, 2, False)
                t2p = pool(st, "eit2", [128, 512], F32, 2, False)
                stg = pool(st, "eist", [128, 512], BF16, 4)
                stgf = pool(st, "eisf", [128, 8], F32, 3)
                cnt = [0]
                groups = [(0, 1024, "tm", "gelu", u_s), (1024, 1024, "tm", "gelu", va_s), (2048, 1024, "fm", "silu", zaT_s),
                          (3072, 1024, "fm", "copy", xmT_s), (4096, 1024, "tm", "sigm", og_s), (5128, 1024, "fm", "silu", zbT_s),
                          (5120, 8, "tm", "gate", gt_s)]
                load_w_stage(W, groups[0][0], groups[0][1], wstage)
                cast_w(wtp[0], groups[0][1], wstage)
                for gi, (c0, ncols, mode, kind, dst) in enumerate(groups):
                    wt = wtp[gi % 2]
                    mid = None
                    if gi + 1 < len(groups):
                        nc0, nnc = groups[gi + 1][0], groups[gi + 1][1]
                        load_w_stage(W, nc0, nnc, wstage)
                        mid = (lambda gi=gi, nnc=nnc: cast_w(wtp[(gi + 1) % 2], nnc, wstage))
                    if mode == "fm":
                        units = [(j * 128,) for j in range(8)]

                        def evac(ui, c, pss, kind=kind, dst=dst):
                            i = cnt[0]
                            cnt[0] += 1
                            sg = stg[i % 4]
                            fn = AF.Silu if kind == "silu" else AF.Copy
                            I("act", lambda e: e.activation(out=sg[:], in_=pss[0][:], func=fn), [], [pss[0], sg])
                            DMA("sp", sg, lambda e: [e.dma_start(out=dst[ui, :, c * 512:(c + 1) * 512], in_=sg[:])], reads=[sg])
                        proj_fm(wt, units, evac, hTp, mid)
                    else:
                        def evac(tt, half, ps, w, kind=kind, dst=dst):
                            i = cnt[0]
                            cnt[0] += 1
                            if kind == "gate":
                                sf = stgf[i % 3]
                                I("act", lambda e: e.activation(out=sf[:], in_=ps[:, 0:8], func=AF.Copy), [], [ps, sf])
                                DMA("sp", sf, lambda e: [e.dma_start(out=dst[tt * 128:(tt + 1) * 128, :], in_=sf[:])], reads=[sf])
                                return
                            sg = stg[i % 4]
                            if kind == "gelu":
                                gelu_evac(ps, w, sg, (t1p[i % 2], t2p[i % 2]))
                            else:
                                I("act", lambda e: e.activation(out=sg[:], in_=ps[:], func=AF.Sigmoid), [], [ps, sg])
                            DMA("sp", sg, lambda e: [e.dma_start(out=dst[tt * 128:(tt + 1) * 128, half * 512:(half + 1) * 512],
                                                                 in_=sg[:])], reads=[sg])
                        proj_tm(wt, ncols, evac, hTp, mid)
                sc.barrier()

        def phase_even_mix(ei):
            with ExitStack() as st:
                alg = sbt(st, "alg", [128, D], F32)
                wsf = sbt(st, "wsf", [128, 8, 128], F32)
                wsb = sbt(st, "wsb", [128, 8, 128], BF16, False)
                bsT = sbt(st, "bsT", [128, 8], F32)
                cw = sbt(st, "cw", [128, 8, 4], F32)
                cv = sbt(st, "cv", [128, 3, 8], F32)
                gb = sbt(st, "gb", [128, 8], F32)
                Wb = sbt(st, "Wb", [128, 3, 8, 256], BF16, False)
                GN = sbt(st, "GN", [128, 8, 128], F32, False)
                SK = sbt(st, "SK", [128, 8, 128], F32, False)
                tri4 = sbt(st, "tri4", [128, 4, 128], F32, False)
                DMA("sp", alg, lambda e: [e.dma_start(out=alg[:], in_=a_ln_g[ei])], writes=[alg])
                DMA("sp", wsf, lambda e: [e.dma_start(out=wsf[:], in_=a_wsT[ei].rearrange("g s t -> s g t"))], writes=[wsf])
                DMA("sp", bsT, lambda e: [e.dma_start(out=bsT[:], in_=a_bsT[ei])], writes=[bsT])
                DMA("sp", cw, lambda e: [e.dma_start(out=cw[:], in_=convw[ei])], writes=[cw])
                DMA("sp", cv, lambda e: [e.dma_start(out=cv[:], in_=colv[ei])], writes=[cv])
                DMA("sp", gb, lambda e: [e.dma_start(out=gb[:], in_=gate_b[ei])], writes=[gb])
                Wbs = sbt(st, "Wbs", [128, 8, 256], F32)
                for j in range(3):
                    DMA("sp", Wbs, lambda e, j=j: [e.dma_start(out=Wbs[:], in_=b_wqkv[ei, j].rearrange("h (dc p) e -> p (h dc) e", p=128))],
                        writes=[Wbs])
                    I("act", lambda e, j=j: e.activation(out=Wb[:, j, :, :], in_=Wbs[:], func=AF.Copy), [Wbs], [(Wb, j)])
                Wb_keys = [(Wb, j) for j in range(3)]
                for g in range(8):
                    I("dve", lambda e, g=g: e.tensor_tensor(out=wsb[:, g, :], in0=wsf[:, g, :], in1=tri_f[:], op=ALU.mult),
                      [wsf, tri_f], [(wsb, g)])
                    I("dve", lambda e, g=g: e.tensor_scalar(out=GN[:, g, :], in0=ones_f[:], scalar1=cv[:, 1, g:g + 1], scalar2=None,
                                                            op0=ALU.mult), [ones_f, cv], [(GN, g)])
                    I("dve", lambda e, g=g: e.tensor_scalar(out=SK[:, g, :], in0=ones_f[:], scalar1=cv[:, 2, g:g + 1], scalar2=None,
                                                            op0=ALU.mult), [ones_f, cv], [(SK, g)])
                for h in range(4):
                    I("dve", lambda e, h=h: e.tensor_copy(out=tri4[:, h, :], in_=tri_f[:]), [tri_f], [(tri4, h)])
                dgw = sbt(st, "dgw", [128, 8, 4, 128], BF16, False)
                for j in range(8):
                    for k in range(4):
                        I("dve", lambda e, j=j, k=k: e.tensor_scalar(out=dgw[:, j, k, :], in0=ident_f[:], scalar1=cw[:, j, k:k + 1],
                                                                      scalar2=None, op0=ALU.mult), [ident_f, cw], [(dgw, j)])
                wsb_keys = [(wsb, g) for g in range(8)]
                GN_keys = [(GN, g) for g in range(8)]
                SK_keys = [(SK, g) for g in range(8)]
                tri4_keys = [(tri4, h) for h in range(4)]

                tmps = {}

                def xc_keys(xc):
                    return [(xc, j) for j in range(8)]

                def tmp(name, shape, dt, n=2):
                    if name not in tmps:
                        tmps[name] = [pool(st, "t" + name, shape, dt, n, False), 0]
                    p = tmps[name]
                    t = p[0][p[1] % n]
                    p[1] += 1
                    return t

                Cst = sbt(st, "Cst", [128, 4, 2, 260], F32, False)
                Cb = sbt(st, "Cb", [128, 4, 2, 260], BF16, False)

                up = pool(st, "mu", [128, D], BF16, 2)
                vap = pool(st, "mva", [128, D], BF16, 2)
                ogp = pool(st, "mog", [128, D], BF16, 2)
                gtp = pool(st, "mgt", [128, 8], F32, 2)
                zap = pool(st, "mza", [128, 8, 128], BF16, 2)
                zbp = pool(st, "mzb", [128, 8, 128], BF16, 2)
                xmp = pool(st, "mxm", [128, 8, 131], BF16, 2)
                yap = pool(st, "mya", [128, 8, 128], BF16, 2)
                ybp = pool(st, "myb", [128, 8, 128], BF16, 2)
                def tile_loads(tt):
                    c = tt % CPS
                    tok0 = tt * 128
                    b2 = tt % 2
                    ut, vat, ogt, gtt, zat, zbt, xmt = up[b2], vap[b2], ogp[b2], gtp[b2], zap[b2], zbp[b2], xmp[b2]
                    rows = slice(tok0, tok0 + 128)
                    DMA("sp", ut, lambda e, ut=ut, rows=rows: [e.dma_start(out=ut[:], in_=u_s[rows, :])], writes=[ut])
                    DMA("sp", vat, lambda e, vat=vat, rows=rows: [e.dma_start(out=vat[:], in_=va_s[rows, :])], writes=[vat])
                    DMA("sp", ogt, lambda e, ogt=ogt, rows=rows: [e.dma_start(out=ogt[:], in_=og_s[rows, :])], writes=[ogt])
                    DMA("sp", gtt, lambda e, gtt=gtt, rows=rows: [e.dma_start(out=gtt[:], in_=gt_s[rows, :])], writes=[gtt])
                    DMA("sp", zat, lambda e, zat=zat, rows=rows: [e.dma_start(
                        out=zat[:], in_=zaT_s[:, :, rows].rearrange("k p t -> p k t"))], writes=[zat])
                    DMA("sp", zbt, lambda e, zbt=zbt, rows=rows: [e.dma_start(
                        out=zbt[:], in_=zbT_s[:, :, rows].rearrange("k p t -> p k t"))], writes=[zbt])
                    if c == 0:
                        I("dve", lambda e, xmt=xmt: e.memset(xmt[:, :, 0:3], 0.0), [], [xmt])
                        DMA("sp", xmt, lambda e, xmt=xmt, rows=rows: [e.dma_start(
                            out=xmt[:, :, 3:131], in_=xmT_s[:, :, rows].rearrange("k p t -> p k t"))], writes=[xmt])
                    else:
                        DMA("sp", xmt, lambda e, xmt=xmt, tok0=tok0: [e.dma_start(
                            out=xmt[:], in_=xmT_s[:, :, tok0 - 3:tok0 + 128].rearrange("k p t -> p k t"))], writes=[xmt])


                def do_tile(tt):
                    c = tt % CPS
                    tok0 = tt * 128
                    b2 = tt % 2
                    ut, vat, ogt, gtt, zat, zbt, xmt = up[b2], vap[b2], ogp[b2], gtp[b2], zap[b2], zbp[b2], xmp[b2]
                    rows = slice(tok0, tok0 + 128)
                    if tt + 1 < NT:
                        tile_loads(tt + 1)
                    bst = tmp("bst", [128, 2, 6], F32)
                    mv = tmp("mv", [128, 8], F32)
                    for hf in range(2):
                        I("dve", lambda e, hf=hf, bst=bst, vat=vat: e.bn_stats(out=bst[:, hf, :], in_=vat[:, hf * 512:(hf + 1) * 512]),
                          [vat], [(bst, hf)])
                    I("dve", lambda e, bst=bst, mv=mv: e.bn_aggr(out=mv[:, 0:2], in_=bst[:].rearrange("p a b -> p (a b)")),
                      [(bst, 0), (bst, 1)], [(mv, 0), (mv, 1)])
                    rstd_chain(mv, 1, 1.0, 2)
                    vn = tmp("vn", [128, D], F32, 1)
                    vnb = tmp("vnb", [128, D], BF16)
                    I("dve", lambda e, vn=vn, vat=vat, mv=mv: e.tensor_scalar(out=vn[:], in0=vat[:], scalar1=mv[:, 0:1], scalar2=mv[:, 2:3],
                                                                              op0=ALU.subtract, op1=ALU.mult), [vat, (mv, 0), (mv, 2)], [vn])
                    I("pool", lambda e, vn=vn, vnb=vnb: e.tensor_tensor(out=vnb[:], in0=vn[:], in1=alg[:], op=ALU.mult), [vn, alg], [vnb])
                    psA = [ps_next(), ps_next()]
                    for g in range(8):
                        pg = psA[g // 4]
                        I("pe", lambda e, g=g, pg=pg, vnb=vnb: e.matmul(pg[:, (g % 4) * 128:(g % 4 + 1) * 128], lhsT=wsb[:, g, :],
                                                                        rhs=vnb[:, g * 128:(g + 1) * 128], start=True, stop=True),
                          [(wsb, g), vnb], [pg])
                    ya = tmp("ya", [128, D], BF16)
                    for g in range(8):
                        pg = psA[g // 4]
                        I("dve", lambda e, g=g, pg=pg, ya=ya, ut=ut: e.scalar_tensor_tensor(
                            out=ya[:, g * 128:(g + 1) * 128], in0=pg[:, (g % 4) * 128:(g % 4 + 1) * 128], scalar=bsT[:, g:g + 1],
                            in1=ut[:, g * 128:(g + 1) * 128], op0=ALU.add, op1=ALU.mult), [bsT, ut], [pg, (ya, g)])
                    for g in range(8):
                        I("pe", lambda e, g=g, ya=ya: e.transpose(out=psB[:, g * 128:(g + 1) * 128], in_=ya[:, g * 128:(g + 1) * 128],
                                                                  identity=ident_b[:]), [(ya, g), ident_b], [psB])
                    yat = yap[b2]
                    I("dve", lambda e, yat=yat, zat=zat: e.tensor_tensor(out=yat[:], in0=psB[:].rearrange("p (k t) -> p k t", k=8),
                                                                         in1=zat[:], op=ALU.mult), [zat], [psB, yat])
                    DMA("sp", yat, lambda e, yat=yat, rows=rows: [e.dma_start(out=yT[0:8, :, rows].rearrange("k p t -> p k t"), in_=yat[:])],
                        reads=[yat])

                    xc = tmp("xc", [128, 8, 128], BF16)
                    psc = [ps_next(), ps_next()]
                    for j in range(8):
                        pg = psc[j // 4]
                        for k in range(4):
                            I("pe", lambda e, j=j, k=k, pg=pg: e.matmul(
                                pg[:, (j % 4) * 128:(j % 4 + 1) * 128], lhsT=dgw[:, j, k, :], rhs=xmt[:, j, k:k + 128],
                                start=(k == 0), stop=(k == 3)), [(dgw, j), xmt], [pg])
                    for j in range(8):
                        pg = psc[j // 4]
                        I("act", lambda e, j=j, pg=pg: e.activation(
                            out=xc[:, j, :], in_=pg[:, (j % 4) * 128:(j % 4 + 1) * 128], func=AF.Silu, bias=cv[:, 0, j:j + 1]),
                          [cv], [pg, (xc, j)])
                    qb_ = tmp("qb", [128, 4, 2, 128], BF16)
                    kb_ = tmp("kb", [128, 4, 2, 128], BF16)
                    qs_ = tmp("qs", [128, 4, 2, 128], BF16)
                    psq = [ps_next(), ps_next()]
                    for (wi, pss, dstb) in ((0, psq, qb_), (1, [ps_next(), ps_next()], kb_)):
                        for h in range(4):
                            pg = pss[h // 2]
                            for ec in range(2):
                                o0 = (h % 2) * 256 + ec * 128
                                for dc in range(2):
                                    I("pe", lambda e, wi=wi, pg=pg, h=h, ec=ec, dc=dc, o0=o0, xc=xc: e.matmul(
                                        pg[:, o0:o0 + 128], lhsT=Wb[:, wi, h * 2 + dc, ec * 128:(ec + 1) * 128], rhs=xc[:, h * 2 + dc, :],
                                        start=(dc == 0), stop=(dc == 1)), Wb_keys + xc_keys(xc), [pg])
                        for bk in range(2):
                            I("act", lambda e, pss=pss, bk=bk, dstb=dstb, wi=wi: e.activation(
                                out=dstb[:, bk * 2:bk * 2 + 2, :, :], in_=pss[bk][:].rearrange("p (h c t) -> p h c t", h=2, c=2),
                                func=AF.Copy, scale=(1.0 if wi == 0 else 0.0625)), [], [pss[bk], (dstb, bk)])
                    psk = [ps_next(), ps_next()]
                    for h in range(4):
                        for dc in range(2):
                            I("pe", lambda e, h=h, dc=dc, xc=xc: e.matmul(
                                psk[h // 2][:, (h % 2) * 256:(h % 2 + 1) * 256], lhsT=xc[:, h * 2 + dc, :], rhs=Wb[:, 1, h * 2 + dc, :],
                                start=(dc == 0), stop=(dc == 1)), Wb_keys + xc_keys(xc), [psk[h // 2]])
                    ktm = tmp("ktm", [128, 4, 256], BF16)
                    for bk in range(2):
                        I("act", lambda e, bk=bk, ktm=ktm: e.activation(
                            out=ktm[:, bk * 2:bk * 2 + 2, :], in_=psk[bk][:].rearrange("p (h e) -> p h e", h=2), func=AF.Copy,
                            scale=0.0625), [], [psk[bk], (ktm, bk)])
                    va_ = tmp("vaug", [128, 4, 260], BF16)
                    I("pool", lambda e, va_=va_: e.memset(va_[:, :, 256:257], 1.0), [], [(va_, "o")])
                    for bk in range(2):
                        psv = ps_next()
                        for hh in range(2):
                            h = bk * 2 + hh
                            for dc in range(2):
                                I("pe", lambda e, h=h, hh=hh, dc=dc, psv=psv, xmt=xmt: e.matmul(
                                    psv[:, hh * 256:(hh + 1) * 256], lhsT=xmt[:, h * 2 + dc, 3:131], rhs=Wb[:, 2, h * 2 + dc, :],
                                    start=(dc == 0), stop=(dc == 1)), Wb_keys + [xmt], [psv])
                        I("act", lambda e, psv=psv, bk=bk, va_=va_: e.activation(
                            out=va_[:, bk * 2:bk * 2 + 2, 0:256], in_=psv[:].rearrange("p (h e) -> p h e", h=2), func=AF.Copy),
                          [], [psv, (va_, bk)])
                    gs = tmp("gs", [128, 40], F32)
                    I("dve", lambda e, gs=gs, gtt=gtt: e.tensor_tensor(out=gs[:, 0:8], in0=gtt[:], in1=gb[:], op=ALU.add), [gtt, gb], [(gs, 0)])
                    I("act", lambda e, gs=gs: e.activation(out=gs[:, 8:12], in_=gs[:, 4:8], func=AF.Exp, scale=-1.0), [(gs, 0)], [(gs, 1)])
                    I("dve", lambda e, gs=gs: e.tensor_scalar(out=gs[:, 12:16], in0=gs[:, 8:12], scalar1=1.0, scalar2=None, op0=ALU.add),
                      [(gs, 1)], [(gs, 2)])
                    I("act", lambda e, gs=gs: e.activation(out=gs[:, 16:20], in_=gs[:, 12:16], func=AF.Ln), [(gs, 2)], [(gs, 3)])
                    psg = ps_next()
                    I("pe", lambda e, gs=gs, psg=psg: e.matmul(psg[:, 0:4], lhsT=tri_f[:], rhs=gs[:, 16:20], start=True, stop=True),
                      [tri_f, (gs, 3)], [psg])
                    I("dve", lambda e, gs=gs, psg=psg: e.tensor_copy(out=gs[:, 20:24], in_=psg[:, 0:4]), [], [psg, (gs, 4)])
                    I("dve", lambda e, gs=gs: e.tensor_tensor(out=gs[:, 24:28], in0=gs[:, 0:4], in1=gs[:, 20:24], op=ALU.add),
                      [(gs, 0), (gs, 4)], [(gs, 5)])
                    dg = tmp("dg", [128, 4, 128], F32, 1)
                    for h in range(4):
                        I("dve", lambda e, h=h, dg=dg, gs=gs: e.tensor_scalar(out=dg[:, h, :], in0=ident_f[:], scalar1=gs[:, 20 + h:21 + h],
                                                                              scalar2=-1.0, op0=ALU.mult, op1=ALU.mult),
                          [ident_f, (gs, 4)], [(dg, h)])
                    psr = ps_next()
                    I("pe", lambda e, psr=psr, dg=dg: e.matmul(psr[:], lhsT=ones_f[:], rhs=dg[:].rearrange("p h t -> p (h t)"),
                                                               start=True, stop=True), [ones_f] + [(dg, h) for h in range(4)], [psr])
                    PT = tmp("PT", [128, 4, 128], F32, 1)
                    for h in range(4):
                        I("act", lambda e, h=h, PT=PT, psr=psr, gs=gs: e.activation(
                            out=PT[:, h, :], in_=psr[:, h * 128:(h + 1) * 128], func=AF.Exp, bias=gs[:, 24 + h:25 + h]),
                          [(gs, 5)], [psr, (PT, h)])
                    EB = tmp("EB", [128, 4, 128], F32, 1)
                    I("act", lambda e, EB=EB, psr=psr: e.activation(out=EB[:].rearrange("p h t -> p (h t)"), in_=psr[:], func=AF.Exp),
                      [], [psr, EB])
                    I("dve", lambda e, gs=gs, psr=psr: e.tensor_copy(
                        out=gs[:, 28:32], in_=psr[:].rearrange("p (h t) -> p h t", h=4)[:, :, 127]), [], [psr, (gs, 6)])
                    I("pool", lambda e, PT=PT: e.tensor_tensor(out=PT[:], in0=PT[:], in1=tri4[:], op=ALU.mult),
                      tri4_keys + [(PT, h) for h in range(4)], [(PT, h) for h in range(4)])
                    if c > 0:
                        for bk in range(2):
                            for ec in range(2):
                                I("dve", lambda e, bk=bk, ec=ec, qs_=qs_, EB=EB, qb_=qb_: e.tensor_tensor(
                                    out=qs_[:, bk * 2:bk * 2 + 2, ec, :], in0=qb_[:, bk * 2:bk * 2 + 2, ec, :],
                                    in1=EB[:, bk * 2:bk * 2 + 2, :], op=ALU.mult), [EB, (qb_, bk)], [(qs_, bk, ec)])
                    psqk = ps_next()
                    for h in range(4):
                        for ec in range(2):
                            I("pe", lambda e, h=h, ec=ec, psqk=psqk, kb_=kb_, qb_=qb_: e.matmul(
                                psqk[:, h * 128:(h + 1) * 128], lhsT=kb_[:, h, ec, :], rhs=qb_[:, h, ec, :],
                                start=(ec == 0), stop=(ec == 1)), [(kb_, 0), (kb_, 1), (qb_, 0), (qb_, 1)], [psqk])
                    scT = tmp("scT", [128, 4, 128], BF16)
                    I("dve", lambda e, scT=scT, psqk=psqk, PT=PT: e.tensor_tensor(
                        out=scT[:].rearrange("p h t -> p (h t)"), in0=psqk[:], in1=PT[:].rearrange("p h t -> p (h t)"), op=ALU.mult),
                      [(PT, h) for h in range(4)], [psqk, scT])
                    htn = tmp("htn", [128, D], BF16)
                    for h in range(4):
                        acc = ps_next()
                        I("pe", lambda e, h=h, acc=acc, scT=scT, va_=va_: e.matmul(
                            acc[:, 0:257], lhsT=scT[:, h, :], rhs=va_[:, h, 0:257], start=True, stop=(c == 0)),
                          [scT, (va_, "o"), (va_, h // 2)], [acc])
                        if c > 0:
                            for dc in range(2):
                                I("pe", lambda e, h=h, dc=dc, acc=acc, qs_=qs_: e.matmul(
                                    acc[:, 0:257], lhsT=qs_[:, h, dc, :], rhs=Cb[:, h, dc, 0:257], start=False, stop=(dc == 1)),
                                  [(qs_, h // 2, dc), (Cb, h, dc)], [acc])
                        hs = tmp("hs", [128, 16], F32)
                        ht = tmp("ht", [128, 256], F32)
                        I("act", lambda e, hs=hs, acc=acc: e.activation(out=hs[:, 8:9], in_=acc[:, 256:257], func=AF.Abs),
                          [], [acc, (hs, 8)])
                        I("dve", lambda e, hs=hs: e.tensor_scalar(out=hs[:, 10:11], in0=hs[:, 8:9], scalar1=1.0, scalar2=None,
                                                                  op0=ALU.max), [(hs, 8)], [(hs, 10)])
                        I("dve", lambda e, hs=hs: e.reciprocal(out=hs[:, 9:10], in_=hs[:, 10:11]), [(hs, 10)], [(hs, 9)])
                        I("dve", lambda e, hs=hs, acc=acc, ht=ht, h=h, ogt=ogt: e.scalar_tensor_tensor(
                            out=ht[:], in0=acc[:, 0:256], scalar=hs[:, 9:10], in1=ogt[:, h * 256:(h + 1) * 256],
                            op0=ALU.mult, op1=ALU.mult), [(hs, 9), ogt], [acc, ht])
                        bs2 = tmp("bs2", [128, 6], F32)
                        I("dve", lambda e, bs2=bs2, ht=ht: e.bn_stats(out=bs2[:], in_=ht[:]), [ht], [bs2])
                        I("dve", lambda e, bs2=bs2, hs=hs: e.bn_aggr(out=hs[:, 0:2], in_=bs2[:]), [bs2], [(hs, 0), (hs, 1)])
                        rstd_chain(hs, 1, 1.0, 2)
                        I("dve", lambda e, hs=hs, ht=ht, htn=htn, h=h: e.tensor_scalar(
                            out=htn[:, h * 256:(h + 1) * 256], in0=ht[:], scalar1=hs[:, 0:1], scalar2=hs[:, 2:3],
                            op0=ALU.subtract, op1=ALU.mult), [ht, (hs, 0), (hs, 2)], [(htn, h)])
                    for g in range(8):
                        I("pe", lambda e, g=g, htn=htn: e.transpose(out=psB[:, g * 128:(g + 1) * 128], in_=htn[:, g * 128:(g + 1) * 128],
                                                                    identity=ident_b[:]), [(htn, g // 2), ident_b], [psB])
                    y1 = tmp("y1", [128, 8, 128], F32, 1)
                    y2 = tmp("y2", [128, 8, 128], F32, 1)
                    I("dve", lambda e, y1=y1: e.tensor_tensor(out=y1[:], in0=psB[:].rearrange("p (k t) -> p k t", k=8), in1=GN[:],
                                                              op=ALU.mult), GN_keys, [psB, y1])
                    I("pool", lambda e, y2=y2, xc=xc: e.tensor_tensor(out=y2[:], in0=xc[:], in1=SK[:], op=ALU.mult), SK_keys + xc_keys(xc), [y2])
                    I("dve", lambda e, y1=y1, y2=y2: e.tensor_tensor(out=y1[:], in0=y1[:], in1=y2[:], op=ALU.add), [y1, y2], [y1])
                    ybt = ybp[b2]
                    I("dve", lambda e, y1=y1, ybt=ybt, zbt=zbt: e.tensor_tensor(out=ybt[:], in0=y1[:], in1=zbt[:], op=ALU.mult),
                      [y1, zbt], [ybt])
                    DMA("sp", ybt, lambda e, ybt=ybt, rows=rows: [e.dma_start(out=yT[8:16, :, rows].rearrange("k p t -> p k t"), in_=ybt[:])],
                        reads=[ybt])
                    if c < CPS - 1:
                        I("dve", lambda e, gs=gs: e.tensor_tensor(out=gs[:, 32:36], in0=gs[:, 24:28], in1=gs[:, 28:32], op=ALU.add),
                          [(gs, 5), (gs, 6)], [(gs, 7)])
                        I("act", lambda e, gs=gs: e.activation(out=gs[:, 32:36], in_=gs[:, 32:36], func=AF.Exp), [(gs, 7)], [(gs, 7)])
                        I("act", lambda e, gs=gs: e.activation(out=gs[:, 36:40], in_=gs[:, 28:32], func=AF.Exp), [(gs, 6)], [(gs, 8)])
                        kw = tmp("kw", [128, 4, 256], BF16)
                        for h in range(4):
                            I("dve", lambda e, h=h, kw=kw, gs=gs, ktm=ktm: e.tensor_scalar(
                                out=kw[:, h, :], in0=ktm[:, h, :], scalar1=gs[:, 32 + h:33 + h],
                                scalar2=None, op0=ALU.mult), [(gs, 7), (ktm, h // 2)], [(kw, h)])
                        for h in range(4):
                            for dc in range(2):
                                psu = ps_next()
                                I("pe", lambda e, h=h, dc=dc, psu=psu, kw=kw, va_=va_: e.matmul(
                                    psu[:, 0:257], lhsT=kw[:, h, dc * 128:(dc + 1) * 128], rhs=va_[:, h, 0:257], start=True, stop=True),
                                  [(kw, h), (va_, "o"), (va_, h // 2)], [psu])
                                if c == 0:
                                    I("dve", lambda e, h=h, dc=dc, psu=psu: e.tensor_copy(out=Cst[:, h, dc, 0:257], in_=psu[:, 0:257]),
                                      [], [psu, (Cst, h, dc)])
                                else:
                                    I("dve", lambda e, h=h, dc=dc, psu=psu, gs=gs: e.scalar_tensor_tensor(
                                        out=Cst[:, h, dc, 0:257], in0=Cst[:, h, dc, 0:257], scalar=gs[:, 36 + h:37 + h], in1=psu[:, 0:257],
                                        op0=ALU.mult, op1=ALU.add), [(gs, 8)], [psu, (Cst, h, dc)])
                                I("act", lambda e, h=h, dc=dc: e.activation(out=Cb[:, h, dc, 0:257], in_=Cst[:, h, dc, 0:257], func=AF.Copy),
                                  [(Cst, h, dc)], [(Cb, h, dc)])
                tile_loads(0)
                for tt in range(NT):
                    do_tile(tt)
                sc.barrier()

        import os as _os
        STOP = _os.environ.get("KSTOP", "")

        def phase_copyout():
            with ExitStack() as st:
                xp = pool(st, "cox", [128, D], F32, 2)
                for tt in range(NT):
                    xt = xp[tt % 2]
                    DMA("sp", xt, lambda e, xt=xt, tt=tt: [e.dma_start(out=xt[:], in_=xs[tt * 128:(tt + 1) * 128, :])], writes=[xt])
                    DMA("sp", xt, lambda e, xt=xt, tt=tt: [e.dma_start(out=out[tt * 128:(tt + 1) * 128, :], in_=xt[:])], reads=[xt])
                sc.barrier()

        def run_all():
            phase_norm0(layers[0][2])
            if STOP == "norm0":
                return phase_copyout()
            for li, (kind, pi, lno) in enumerate(layers):
                if kind == "odd":
                    phase_odd_inproj(pi)
                    if STOP == "inproj":
                        return phase_copyout()
                    phase_attn(pi, lno)
                    if STOP == "attn":
                        return phase_copyout()
                    phase_out(li, od_w_out[pi])
                else:
                    phase_even_inproj(pi)
                    if STOP == "inproj":
                        return phase_copyout()
                    phase_even_mix(pi)
                    if STOP == "attn":
                        return phase_copyout()
                    phase_out(li, ev_w_out[pi])

        run_all()
        sc.barrier(["sp"])
        sc.emit()
    return nc


def prep_inputs(inputs, S):
    f = lambda a: np.ascontiguousarray(np.asarray(a, dtype=np.float32))
    rep = lambda a: np.ascontiguousarray(np.broadcast_to(f(a)[None], (128,) + tuple(np.shape(a))))
    shared = {}
    shared["gvec"] = rep(np.concatenate([f(inputs["norm_g"]), f(inputs["final_g"])[None]], axis=0))
    shared["ident"] = np.eye(128, dtype=np.float32)
    shared["tri"] = np.triu(np.ones((128, 128), np.float32))
    cosT, sinT = rope_tables_T(S)
    shared["cosT"], shared["sinT"] = cosT, sinT
    shared["ev_w_in"] = f(inputs["ev_w_in"])
    shared["ev_w_out"] = f(inputs["ev_w_out"])
    w = f(inputs["od_w_in"])
    sw = np.concatenate([np.arange(32, 64), np.arange(0, 32)])
    cols = []
    for base in (0, 2048):
        for h in range(16):
            for cmp_ in range(2):
                cols.append(base + h * 128 + cmp_ * 64 + np.arange(64))
            for cmp_ in range(2):
                cols.append(base + h * 128 + cmp_ * 64 + sw)
    cols.append(6144 + np.arange(2048))
    cols.append(4096 + np.arange(2048))
    cols = np.concatenate(cols)
    shared["od_w_in"] = np.ascontiguousarray(w[:, :, cols])
    shared["od_w_out"] = f(inputs["od_w_out"])
    shared["a_ln_g"] = np.ascontiguousarray(np.broadcast_to(f(inputs["a_ln_g"])[:, None, :], (2, 128, D)))
    shared["a_wsT"] = np.ascontiguousarray(np.swapaxes(f(inputs["a_ws"]), 2, 3))
    shared["a_bsT"] = np.ascontiguousarray(np.swapaxes(f(inputs["a_bs"]), 1, 2))
    cw = f(inputs["b_conv_w"])
    shared["convw"] = np.ascontiguousarray(cw.reshape(2, 4, 8, 128).transpose(0, 3, 2, 1))
    colv = np.stack([f(inputs["b_conv_b"]), f(inputs["b_gn_g"]), f(inputs["b_skip"])], axis=1)
    shared["colv"] = np.ascontiguousarray(colv.reshape(2, 3, 8, 128).transpose(0, 3, 1, 2))
    shared["b_wqkv"] = np.ascontiguousarray(np.stack([f(inputs["b_wq"]), f(inputs["b_wk"]), f(inputs["b_wv"])], axis=1))
    gb = np.concatenate([f(inputs["b_ig_b"]), f(inputs["b_fg_b"])], axis=1)
    shared["gate_b"] = np.ascontiguousarray(np.broadcast_to(gb[:, None, :], (2, 128, 8)))
    lv = np.stack([f(inputs["c_lam_q1"]), f(inputs["c_lam_k1"]), f(inputs["c_lam_q2"]), f(inputs["c_lam_k2"])], axis=1)
    shared["lamv"] = np.ascontiguousarray(np.broadcast_to(lv[:, None], (2, 128, 4, 64)))
    shared["subln"] = np.ascontiguousarray(f(inputs["c_subln_g"]).reshape(2, 128, 1))
    return shared


_PROG_CACHE = {}


def kernel(**inputs):
    x = np.ascontiguousarray(inputs["x"], dtype=np.float32)
    B, S, _ = x.shape
    NS = B // NCORES
    key = (NS, S)
    if key not in _PROG_CACHE:
        _PROG_CACHE[key] = build_program(NS, S)
    nc = _PROG_CACHE[key]
    shared = prep_inputs(inputs, S)
    in_maps = []
    for c in range(NCORES):
        m = dict(shared)
        m["x"] = x[c * NS:(c + 1) * NS].reshape(NS * S, D)
        in_maps.append(m)
    res = run_bass_kernel_spmd(nc, in_maps, core_ids=list(range(NCORES)))
    outs = [r["out"].reshape(NS, S, D) for r in res.results]
    return np.concatenate(outs, axis=0)
```

```python
import math
from contextlib import ExitStack

import numpy as np
import concourse.bass as bass
import concourse.mybir as mybir
from concourse.bass_utils import run_bass_kernel_spmd

F32 = mybir.dt.float32
BF16 = mybir.dt.bfloat16
AF = mybir.ActivationFunctionType
ALU = mybir.AluOpType
AX = mybir.AxisListType

D = 1024
NCORES = 8
EPS = 1e-6
EVEN_IN = 6152
OD_EXT = 8192
N_DMA_SEMS = 84
FULL_LAYERS = (("even", 0, 0), ("odd", 0, 1), ("even", 1, 2), ("odd", 1, 3))


class Sched:
    ENGS = ("pe", "act", "dve", "pool", "sp")

    def __init__(self, nc, es):
        self.nc = nc
        self.es = es
        self.prog = {e: [] for e in self.ENGS}
        self.idx = {e: 0 for e in self.ENGS}
        self.seen = {e: {} for e in self.ENGS}
        self.cnt = {}
        self.last_w = {}
        self.readers = {}
        self.eng_sem = {}
        for e in ("pe", "act", "dve", "pool"):
            self.eng_sem[e] = self.new_sem("c_" + e)
        self.dma_sems = [self.new_sem(f"dq{i}") for i in range(N_DMA_SEMS)]
        self.dma_i = 0
        self.n_ops = 0

    def new_sem(self, name):
        s = self.es.enter_context(self.nc.semaphore(name))
        self.cnt[s] = 0
        return s

    def get_dma_sem(self):
        s = self.dma_sems[self.dma_i % N_DMA_SEMS]
        self.dma_i += 1
        return s

    def op(self, eng, fn, reads=(), writes=(), dma_sem=None, ndma=1):
        deps = []
        for k in reads:
            t = self.last_w.get(k)
            if t is not None:
                deps.append(t)
        for k in writes:
            t = self.last_w.get(k)
            if t is not None:
                deps.append(t)
            deps.extend(self.readers.get(k, ()))
        need = {}
        seen = self.seen[eng]
        cur = self.idx[eng]
        for (sem, val, teng, tidx, is_dma) in deps:
            if teng == eng and not is_dma:
                if eng == "pe":
                    continue
            if seen.get(sem, 0) >= val:
                continue
            if need.get(sem, 0) < val:
                need[sem] = val
        if dma_sem is not None:
            prev = self.cnt[dma_sem]
            if prev > 0 and seen.get(dma_sem, 0) < prev:
                need[dma_sem] = prev
        for sem, val in need.items():
            self.prog[eng].append(("w", sem, val))
            seen[sem] = val
        if dma_sem is None:
            sem = self.eng_sem[eng]
            self.cnt[sem] += 1
            is_dma = False
        else:
            sem = dma_sem
            self.cnt[sem] += 16 * ndma
            is_dma = True
        tok = (sem, self.cnt[sem], eng, cur, is_dma)
        self.prog[eng].append(("o", fn, sem))
        for k in writes:
            self.last_w[k] = tok
            self.readers[k] = []
        for k in reads:
            self.readers.setdefault(k, []).append(tok)
        self.idx[eng] = cur + 1
        self.n_ops += 1
        return tok

    def barrier(self, engs=None):
        for eng in (engs or self.ENGS):
            for sem, val in self.cnt.items():
                if val > 0 and self.seen[eng].get(sem, 0) < val:
                    self.prog[eng].append(("w", sem, val))
                    self.seen[eng][sem] = val
        if engs is None:
            self.last_w = {}
            self.readers = {}

    def emit(self):
        nc = self.nc
        engobj = {"pe": "tensor", "act": "scalar", "dve": "vector", "pool": "gpsimd", "sp": "sync"}
        with nc.Block() as block:
            for e in self.ENGS:
                prog = self.prog[e]

                def body(eng, prog=prog):
                    for it in prog:
                        if it[0] == "w":
                            eng.wait_ge(it[1], it[2])
                        else:
                            it[1](eng, it[2])

                getattr(block, engobj[e])(body)


class Tl:
    def __init__(self, t, sem=None):
        self.t = t
        self.sem = sem

    def __getitem__(self, k):
        return self.t[k]


def rope_tables_T(S):
    inv = (10000.0 ** (-np.arange(0, 64, 2, dtype=np.float32) / np.float32(64))).astype(np.float32)
    ang = (np.arange(S, dtype=np.float32)[:, None] * inv[None, :]).astype(np.float32)
    cos, sin = np.cos(ang).astype(np.float32), np.sin(ang).astype(np.float32)
    cosT = np.zeros((128, S), np.float32)
    sinT = np.zeros((128, S), np.float32)
    for p in range(128):
        d = p % 64
        cosT[p] = cos[:, d % 32]
        sinT[p] = -sin[:, d % 32] if d < 32 else sin[:, d % 32]
    return cosT, sinT


def lambda_init(layer):
    return 0.8 - 0.6 * math.exp(-0.3 * layer)


def build_program(NS, S, layers=FULL_LAYERS, dbg=False):
    T = NS * S
    NT = T // 128
    NC5 = T // 512
    CPS = S // 128
    n_layers = len(layers)
    nc = bass.Bass("TRN2", target_bir_lowering=False)
    es = ExitStack()

    def din(name, shape, dt=F32):
        return nc.dram_tensor(name, list(shape), dt, kind="ExternalInput").ap()

    def dscr(name, shape, dt):
        if dbg:
            return nc.dram_tensor(name, list(shape), dt, kind="ExternalOutput").ap()
        return nc.dram_tensor(name, list(shape), dt).ap()

    x_in = din("x", [T, D])
    gvec = din("gvec", [128, 5, D])
    ident_d = din("ident", [128, 128])
    tri_d = din("tri", [128, 128])
    perm_d = din("perm", [128, 128])
    cosT_d = din("cosT", [128, S])
    sinT_d = din("sinT", [128, S])
    ev_w_in = din("ev_w_in", [2, D, EVEN_IN])
    ev_w_out = din("ev_w_out", [2, 2048, D])
    od_w_in = din("od_w_in", [2, D, OD_EXT])
    od_w_out = din("od_w_out", [2, 2048, D])
    a_ln_g = din("a_ln_g", [2, 128, D])
    a_wsT = din("a_wsT", [2, 8, 128, 128])
    a_bsT = din("a_bsT", [2, 128, 8])
    convw = din("convw", [2, 128, 8, 4])
    colv = din("colv", [2, 128, 3, 8])
    b_wqkv = din("b_wqkv", [2, 3, 4, 256, 256])
    gate_b = din("gate_b", [2, 128, 8])
    lamv = din("lamv", [2, 128, 4, 64])
    subln = din("subln", [2, 128, 1])
    out = nc.dram_tensor("out", [T, D], F32, kind="ExternalOutput").ap()

    xs = dscr("xs", [T, D], F32)
    hnT = dscr("hnT", [8, 128, T], BF16)
    yT = dscr("yT", [16, 128, T], BF16)
    qT_s = dscr("qT_s", [16, 128, T], BF16)
    kT_s = dscr("kT_s", [16, 128, T], BF16)
    zT_s = dscr("zT_s", [16, 128, T], BF16)
    v_s = dscr("v_s", [T, 2048], BF16)
    u_s = dscr("u_s", [T, D], BF16)
    va_s = dscr("va_s", [T, D], BF16)
    og_s = dscr("og_s", [T, D], BF16)
    gt_s = dscr("gt_s", [T, 8], F32)
    zaT_s = dscr("zaT_s", [8, 128, T], BF16)
    xmT_s = dscr("xmT_s", [8, 128, T], BF16)
    zbT_s = dscr("zbT_s", [8, 128, T], BF16)

    with es:
        sc = Sched(nc, es)

        def I(eng, f, reads=(), writes=()):
            sc.op(eng, lambda e, s: f(e).then_inc(s, 1), reads, writes)

        def DMA(eng, tl, f, reads=(), writes=(), n=1):
            def g(e, s):
                r = f(e)
                assert len(r) == n
                for x in r:
                    x.then_inc(s, 16)
            sc.op(eng, g, reads, writes, dma_sem=tl.sem, ndma=n)

        uid = [0]

        def sbt(st, name, shape, dt, dma=True):
            uid[0] += 1
            t = st.enter_context(nc.sbuf_tensor(f"{name}_{uid[0]}", list(shape), dt))
            return Tl(t, sc.get_dma_sem() if dma else None)

        def pool(st, name, shape, dt, n, dma=True):
            return [sbt(st, f"{name}{i}", shape, dt, dma) for i in range(n)]

        ident_f = sbt(es, "ident_f", [128, 128], F32)
        ident_b = sbt(es, "ident_b", [128, 128], BF16)
        tri_f = sbt(es, "tri_f", [128, 128], F32)
        tri_b = sbt(es, "tri_b", [128, 128], BF16)
        ones_f = sbt(es, "ones_f", [128, 128], F32)
        perm_f = sbt(es, "perm_f", [128, 128], F32)
        perm_b = sbt(es, "perm_b", [128, 128], BF16)
        psF = [Tl(es.enter_context(nc.psum_tensor(f"psF{i}", [128, 512], F32))) for i in range(7)]
        psB = Tl(es.enter_context(nc.psum_tensor("psB", [128, 1024], BF16)))
        ps_i = [0]

        def ps_next():
            p = psF[ps_i[0] % len(psF)]
            ps_i[0] += 1
            return p

        DMA("sp", ident_f, lambda e: [e.dma_start(out=ident_f[:], in_=ident_d[:, :])], writes=[ident_f])
        DMA("sp", tri_f, lambda e: [e.dma_start(out=tri_f[:], in_=tri_d[:, :])], writes=[tri_f])
        I("dve", lambda e: e.tensor_copy(out=ident_b[:], in_=ident_f[:]), [ident_f], [ident_b])
        I("dve", lambda e: e.tensor_copy(out=tri_b[:], in_=tri_f[:]), [tri_f], [tri_b])
        I("dve", lambda e: e.memset(ones_f[:], 1.0), [], [ones_f])
        DMA("sp", perm_f, lambda e: [e.dma_start(out=perm_f[:], in_=perm_d[:, :])], writes=[perm_f])
        I("dve", lambda e: e.tensor_copy(out=perm_b[:], in_=perm_f[:]), [perm_f], [perm_b])

        def rstd_chain(st, ss_col, scale, out_col):
            I("dve", lambda e: e.tensor_scalar(out=st[:, 6:7], in0=st[:, ss_col:ss_col + 1], scalar1=scale,
                                               scalar2=EPS, op0=ALU.mult, op1=ALU.add),
              [(st, ss_col)], [(st, 6)])
            I("act", lambda e: e.activation(out=st[:, 7:8], in_=st[:, 6:7], func=AF.Ln), [(st, 6)], [(st, 7)])
            I("act", lambda e: e.activation(out=st[:, out_col:out_col + 1], in_=st[:, 7:8], func=AF.Exp, scale=-0.5),
              [(st, 7)], [(st, out_col)])

        def norm_to_hnT(st_pools, xt, tt, gt_, hT, final=False):
            junk, stat, hnp, otp = st_pools
            jt = junk[tt % len(junk)]
            st = stat[tt % len(stat)]
            I("act", lambda e: e.activation(out=jt[:], in_=xt[:], func=AF.Square, accum_out=st[:, 0:1]),
              [xt], [jt, (st, 0)])
            rstd_chain(st, 0, 1.0 / D, 1)
            if final:
                ot = otp[tt % len(otp)]
                I("dve", lambda e: e.scalar_tensor_tensor(out=ot[:], in0=xt[:], scalar=st[:, 1:2], in1=gt_[:],
                                                          op0=ALU.mult, op1=ALU.mult), [xt, (st, 1), gt_], [ot])
                DMA("sp", ot, lambda e: [e.dma_start(out=out[tt * 128:(tt + 1) * 128, :], in_=ot[:])], reads=[ot])
                return
            hn = hnp[tt % len(hnp)]
            I("dve", lambda e: e.scalar_tensor_tensor(out=hn[:], in0=xt[:], scalar=st[:, 1:2], in1=gt_[:],
                                                      op0=ALU.mult, op1=ALU.mult), [xt, (st, 1), gt_], [hn])
            for kc in range(8):
                I("pe", lambda e, kc=kc: e.transpose(out=psB[:, kc * 128:(kc + 1) * 128],
                                                     in_=hn[:, kc * 128:(kc + 1) * 128], identity=ident_b[:]),
                  [hn, ident_b], [psB])
            j = tt % 4
            I("act", lambda e: e.activation(out=hT[:, :, j * 128:(j + 1) * 128],
                                            in_=psB[:].rearrange("p (k t) -> p k t", k=8), func=AF.Copy),
              [], [psB, (hT, j)])
            if j == 3:
                c = tt // 4
                DMA("sp", hT, lambda e: [e.dma_start(out=hnT[:, :, c * 512:(c + 1) * 512].rearrange("k p t -> p k t"),
                                                     in_=hT[:])],
                    reads=[(hT, 0), (hT, 1), (hT, 2), (hT, 3)])

        def phase_norm0(gidx):
            with ExitStack() as st:
                xp = pool(st, "n0x", [128, D], F32, 3)
                pools = (pool(st, "n0j", [128, D], F32, 2, False), pool(st, "n0s", [128, 8], F32, 4, False),
                         pool(st, "n0h", [128, D], BF16, 2, False), None)
                hTp = pool(st, "n0T", [128, 8, 512], BF16, 2)
                gt_ = sbt(st, "gt0", [128, D], F32)
                DMA("sp", gt_, lambda e: [e.dma_start(out=gt_[:], in_=gvec[:, gidx, :])], writes=[gt_])
                for tt in range(NT):
                    xt = xp[tt % 3]
                    DMA("sp", xt, lambda e, xt=xt, tt=tt: [e.dma_start(out=xt[:], in_=x_in[tt * 128:(tt + 1) * 128, :])],
                        writes=[xt])
                    DMA("sp", xt, lambda e, xt=xt, tt=tt: [e.dma_start(out=xs[tt * 128:(tt + 1) * 128, :], in_=xt[:])],
                        reads=[xt])
                    norm_to_hnT(pools, xt, tt, gt_, hTp[(tt // 4) % 2])
                sc.barrier()

        def load_w_stage(w_dram_2d, c0, ncols, stage):
            DMA("sp", stage, lambda e: [e.dma_start(out=stage[:, :, 0:ncols],
                                                    in_=w_dram_2d[:, c0:c0 + ncols].rearrange("(kc p) c -> p kc c", p=128))],
                writes=[stage])

        def cast_w(wt, ncols, stage):
            I("act", lambda e: e.activation(out=wt[:, :, 0:ncols], in_=stage[:, :, 0:ncols], func=AF.Copy), [stage], [wt])

        def load_hT(hT, c):
            DMA("sp", hT, lambda e: [e.dma_start(out=hT[:], in_=hnT[:, :, c * 512:(c + 1) * 512].rearrange("k p t -> p k t"))],
                writes=[hT])

        def proj_fm(wt, units, evac, hTp, mid=None):
            pend_ = [None]
            load_hT(hTp[0], 0)
            for c in range(NC5):
                hT = hTp[c % len(hTp)]
                if c + 1 < NC5:
                    load_hT(hTp[(c + 1) % len(hTp)], c + 1)
                for ui, cols in enumerate(units):
                    pss = []
                    for co in cols:
                        ps = ps_next()
                        for kc in range(8):
                            I("pe", lambda e, ps=ps, kc=kc, co=co, hT=hT: e.matmul(
                                ps[:], lhsT=wt[:, kc, co:co + 128], rhs=hT[:, kc, :], start=(kc == 0), stop=(kc == 7)),
                              [wt, hT], [ps])
                        pss.append(ps)
                    r_ = evac(ui, c, pss)
                    if pend_[0] is not None:
                        pend_[0]()
                    pend_[0] = r_
                if mid is not None and c == min(4, NC5 - 1):
                    mid()
            if pend_[0] is not None:
                pend_[0]()

        def proj_tm(wt, ncols, evac, hTp, mid=None):
            load_hT(hTp[0], 0)
            for c in range(NC5):
                hT = hTp[c % len(hTp)]
                if c + 1 < NC5:
                    load_hT(hTp[(c + 1) % len(hTp)], c + 1)
                for j in range(4):
                    tt = c * 4 + j
                    for half in range((ncols + 511) // 512):
                        w = min(512, ncols - half * 512)
                        ps = ps_next()
                        for kc in range(8):
                            I("pe", lambda e, ps=ps, kc=kc, half=half, w=w, j=j, hT=hT: e.matmul(
                                ps[:, 0:w], lhsT=hT[:, kc, j * 128:(j + 1) * 128],
                                rhs=wt[:, kc, half * 512:half * 512 + w], start=(kc == 0), stop=(kc == 7)),
                              [wt, hT], [ps])
                        evac(tt, half, ps, w)
                if mid is not None and c == min(4, NC5 - 1):
                    mid()

        def phase_out(li, w_out_2d):
            final = (li == n_layers - 1)
            gidx = 4 if final else layers[li + 1][2]
            with ExitStack() as st:
                wo = sbt(st, "wo", [128, 16, D], BF16, False)
                wos = sbt(st, "wos", [128, 8, D], F32)
                for hf in range(2):
                    DMA("sp", wos, lambda e, hf=hf: [e.dma_start(
                        out=wos[:], in_=w_out_2d[hf * 1024:(hf + 1) * 1024, :].rearrange("(kc p) c -> p kc c", p=128))], writes=[wos])
                    I("act", lambda e, hf=hf: e.activation(out=wo[:, hf * 8:(hf + 1) * 8, :], in_=wos[:], func=AF.Copy), [wos], [(wo, hf)])
                gt_ = sbt(st, "gt", [128, D], F32)
                DMA("sp", gt_, lambda e: [e.dma_start(out=gt_[:], in_=gvec[:, gidx, :])], writes=[gt_])
                yp = pool(st, "poy", [128, 16, 512], BF16, 2)
                xp = pool(st, "pox", [128, D], F32, 4)
                pools = (pool(st, "poj", [128, D], F32, 2, False), pool(st, "pos", [128, 8], F32, 4, False),
                         pool(st, "poh", [128, D], BF16, 2, False), pool(st, "poo", [128, D], F32, 2))
                hTp = pool(st, "poT", [128, 8, 512], BF16, 2)
                def load_y(c):
                    yt = yp[c % 2]
                    DMA("sp", yt, lambda e: [e.dma_start(
                        out=yt[:, hf * 8:(hf + 1) * 8, :], in_=yT[hf * 8:(hf + 1) * 8, :, c * 512:(c + 1) * 512].rearrange("k p t -> p k t"))
                        for hf in range(2)], writes=[yt], n=2)

                load_y(0)

                def stage_a(tt):
                    c, j = tt // 4, tt % 4
                    yt = yp[c % 2]
                    if j == 1 and c + 1 < NC5:
                        load_y(c + 1)
                    xt = xp[tt % 4]
                    DMA("sp", xt, lambda e: [e.dma_start(out=xt[:], in_=xs[tt * 128:(tt + 1) * 128, :])], writes=[xt])
                    for half in range(2):
                        ps = ps_next()
                        for kc in range(16):
                            I("pe", lambda e, ps=ps, kc=kc, half=half: e.matmul(
                                ps[:], lhsT=yt[:, kc, j * 128:(j + 1) * 128],
                                rhs=wo[:, kc, half * 512:(half + 1) * 512], start=(kc == 0), stop=(kc == 15)),
                              [(wo, 0), (wo, 1), yt], [ps])
                        I("dve", lambda e, ps=ps, half=half: e.tensor_tensor(
                            out=xt[:, half * 512:(half + 1) * 512], in0=ps[:], in1=xt[:, half * 512:(half + 1) * 512],
                            op=ALU.add), [], [ps, xt])
                    if not final:
                        DMA("sp", xt, lambda e: [e.dma_start(out=xs[tt * 128:(tt + 1) * 128, :], in_=xt[:])], reads=[xt])

                def stage_b(tt):
                    norm_to_hnT(pools, xp[tt % 4], tt, gt_, hTp[(tt // 4) % 2], final=final)

                stage_a(0)
                for tt in range(NT):
                    if tt + 1 < NT:
                        stage_a(tt + 1)
                    stage_b(tt)
                sc.barrier()

        def phase_odd_inproj(o):
            W = od_w_in[o]
            with ExitStack() as st:
                wtp = pool(st, "oiw", [128, 8, 1024], BF16, 2, False)
                wstage = sbt(st, "oiws", [128, 8, 1024], F32)
                hTp = pool(st, "oih", [128, 8, 512], BF16, 3)
                cosT = sbt(st, "cosT", [128, S], F32)
                sinT = sbt(st, "sinT", [128, S], F32)
                DMA("sp", cosT, lambda e: [e.dma_start(out=cosT[:], in_=cosT_d[:, :])], writes=[cosT])
                DMA("sp", sinT, lambda e: [e.dma_start(out=sinT[:], in_=sinT_d[:, :])], writes=[sinT])
                t1p = pool(st, "oit1", [128, 512], F32, 2, False)
                t2p = pool(st, "oit2", [128, 512], F32, 2, False)
                stg = pool(st, "oist", [128, 512], BF16, 4)
                cnt = [0]
                qsp = pool(st, "oiq", [128, 512], BF16, 3, False)
                load_w_stage(W, 0, 1024, wstage)
                cast_w(wtp[0], 1024, wstage)
                NGRP = 8
                for g in range(NGRP):
                    wt = wtp[g % 2]
                    mid = None
                    if g + 1 < NGRP:
                        load_w_stage(W, (g + 1) * 1024, 1024, wstage)
                        mid = (lambda g=g: cast_w(wtp[(g + 1) % 2], 1024, wstage))
                    if g < 4:
                        dst = qT_s if g < 2 else kT_s
                        units = [(j * 128,) for j in range(8)]

                        def evac(ui, c, pss, g=g, dst=dst):
                            i = cnt[0]
                            cnt[0] += 1
                            t1, t2, sg, qs = t1p[i % 2], t2p[i % 2], stg[i % 4], qsp[i % 3]
                            ps = pss[0]
                            p0 = (c * 512) % S
                            I("act", lambda e: e.activation(out=qs[:], in_=ps[:], func=AF.Copy), [], [ps, qs])
                            I("dve", lambda e: e.tensor_tensor(out=t1[:], in0=ps[:], in1=cosT[:, p0:p0 + 512], op=ALU.mult),
                              [cosT], [ps, t1])

                            def later():
                                ps2 = ps_next()
                                I("pe", lambda e: e.matmul(ps2[:], lhsT=perm_b[:], rhs=qs[:], start=True, stop=True), [perm_b, qs], [ps2])
                                I("dve", lambda e: e.tensor_tensor(out=t2[:], in0=ps2[:], in1=sinT[:, p0:p0 + 512], op=ALU.mult),
                                  [sinT], [ps2, t2])
                                I("pool", lambda e: e.tensor_tensor(out=sg[:], in0=t1[:], in1=t2[:], op=ALU.add), [t1, t2], [sg])
                                h = (g % 2) * 8 + ui
                                DMA("sp", sg, lambda e: [e.dma_start(out=dst[h, :, c * 512:(c + 1) * 512], in_=sg[:])], reads=[sg])
                            return later
                        proj_fm(wt, units, evac, hTp, mid)
                    elif g >= 6:
                        units = [(j * 128,) for j in range(8)]

                        def evac(ui, c, pss, g=g):
                            i = cnt[0]
                            cnt[0] += 1
                            sg = stg[i % 4]
                            I("act", lambda e: e.activation(out=sg[:], in_=pss[0][:], func=AF.Silu), [], [pss[0], sg])
                            h = (g - 6) * 8 + ui
                            DMA("sp", sg, lambda e: [e.dma_start(out=zT_s[h, :, c * 512:(c + 1) * 512], in_=sg[:])], reads=[sg])
                        proj_fm(wt, units, evac, hTp, mid)
                    else:
                        def evac(tt, half, ps, w, g=g):
                            i = cnt[0]
                            cnt[0] += 1
                            sg = stg[i % 4]
                            I("act", lambda e: e.activation(out=sg[:], in_=ps[:], func=AF.Copy), [], [ps, sg])
                            c0 = (g - 4) * 1024 + half * 512
                            DMA("sp", sg, lambda e: [e.dma_start(out=v_s[tt * 128:(tt + 1) * 128, c0:c0 + 512], in_=sg[:])],
                                reads=[sg])
                        proj_tm(wt, 1024, evac, hTp, mid)
                sc.barrier()

        def phase_attn(o, layer_no):
            li = lambda_init(layer_no)
            with ExitStack() as st:
                lv = sbt(st, "lv", [128, 4, 64], F32)
                sl = sbt(st, "sl", [128, 8], F32, False)
                gcol = sbt(st, "gcol", [128, 1], F32)
                lj = sbt(st, "lj", [128, 64], F32, False)
                DMA("sp", lv, lambda e: [e.dma_start(out=lv[:], in_=lamv[o])], writes=[lv])
                DMA("sp", gcol, lambda e: [e.dma_start(out=gcol[:], in_=subln[o])], writes=[gcol])
                for i2 in range(2):
                    I("dve", lambda e, i2=i2: e.tensor_tensor(out=lj[:], in0=lv[:, 2 * i2, :], in1=lv[:, 2 * i2 + 1, :], op=ALU.mult),
                      [lv], [lj])
                    I("dve", lambda e, i2=i2: e.reduce_sum(out=sl[:, i2:i2 + 1], in_=lj[:], axis=AX.X), [lj], [(sl, i2)])
                    I("act", lambda e, i2=i2: e.activation(out=sl[:, 2 + i2:3 + i2], in_=sl[:, i2:i2 + 1], func=AF.Exp),
                      [(sl, i2)], [(sl, 2 + i2)])
                I("dve", lambda e: e.tensor_tensor(out=sl[:, 5:6], in0=sl[:, 3:4], in1=sl[:, 2:3], op=ALU.subtract),
                  [(sl, 2), (sl, 3)], [(sl, 5)])
                I("dve", lambda e: e.tensor_scalar(out=sl[:, 4:5], in0=sl[:, 5:6], scalar1=-li, scalar2=None, op0=ALU.add),
                  [(sl, 5)], [(sl, 4)])
                I("dve", lambda e: e.tensor_scalar(out=gcol[:], in0=gcol[:], scalar1=1.0 - li, scalar2=None, op0=ALU.mult),
                  [gcol], [gcol])

                NG = S // 256
                qp = pool(st, "aqq", [128, NG, 2, 256], BF16, 2)
                for b_ in range(2):
                    I("dve", lambda e, b_=b_: e.memset(qp[b_][64:128, :, 0, :], 0.0), [], [(qp[b_], "z0")])
                    I("dve", lambda e, b_=b_: e.memset(qp[b_][0:64, :, 1, :], 0.0), [], [(qp[b_], "z1")])
                kp = pool(st, "ak", [128, S], BF16, 2)
                zp = pool(st, "az", [128, S], BF16, 2)
                vp = pool(st, "av", [128, CPS, 132], BF16, 2)
                yp = pool(st, "ay", [128, S], BF16, 2)
                pp = pool(st, "ap", [128, 512], BF16, 6, False)
                stp = pool(st, "ast", [128, 8], F32, 8, False)
                o1p = pool(st, "ao1", [128, 128], F32, 8, False)
                o2p = pool(st, "ao2", [128, 128], F32, 8, False)
                jkp = pool(st, "ajk", [128, 128], F32, 8, False)
                onp = pool(st, "aon", [128, 128], BF16, 8, False)
                for v_ in vp:
                    I("dve", lambda e, v_=v_: e.memset(v_[:, :, 128:129], 1.0), [], [(v_, "ones")])
                it = 0
                qb = 0
                epi_i = [0]
                gstep = [0]
                pending = []

                def defer(delay, fn):
                    pending.append([gstep[0] + delay, fn, it])

                def run_due(force=False, owner_lt=None):
                    keep = []
                    for item in list(pending):
                        if force or item[0] <= gstep[0] or (owner_lt is not None and item[2] < owner_lt):
                            item[1]()
                        else:
                            keep.append(item)
                    pending[:] = keep

                def issue_loads(idx):
                    s_, h = idx // 16, idx % 16
                    qq, k, z, v = qp[idx % 2], kp[idx % 2], zp[idx % 2], vp[idx % 2]
                    t0 = s_ * S
                    DMA("sp", qq, lambda e: [
                        e.dma_start(out=qq[0:64, :, 0, :], in_=qT_s[h, 0:64, t0:t0 + S].rearrange("p (g q) -> p g q", q=256)),
                        e.dma_start(out=qq[64:128, :, 1, :], in_=qT_s[h, 64:128, t0:t0 + S].rearrange("p (g q) -> p g q", q=256))],
                        writes=[qq], n=2)
                    DMA("sp", k, lambda e: [e.dma_start(out=k[:], in_=kT_s[h, :, t0:t0 + S])], writes=[k])
                    DMA("sp", z, lambda e: [e.dma_start(out=z[:], in_=zT_s[h, :, t0:t0 + S])], writes=[z])
                    nvp = (CPS + 7) // 8
                    DMA("sp", v, lambda e: [e.dma_start(
                        out=v[:, j8 * 8:min(CPS, j8 * 8 + 8), 0:128],
                        in_=v_s[t0 + j8 * 1024:min(t0 + S, t0 + j8 * 1024 + 1024), h * 128:(h + 1) * 128].rearrange(
                            "(c p) e -> p c e", p=128)) for j8 in range(nvp)], writes=[v], n=nvp)

                issue_loads(0)
                for s in range(NS):
                    for h in range(16):
                        qq, k, z, v, y = qp[it % 2], kp[it % 2], zp[it % 2], vp[it % 2], yp[it % 2]
                        it += 1
                        t0 = s * S
                        steps = [(G, i) for G in range(CPS // 2) for i in range(2 * G + 2)]
                        info = {}

                        def emit_qk(n, qq=qq, k=k):
                            nonlocal qb
                            G, i = steps[n]
                            r = 1 if i == 2 * G + 1 else 0
                            sps = psF[4 + qb % 3]
                            pt = pp[qb % 4]
                            qb += 1
                            info[n] = (r, sps, pt)
                            qdeps = [k, qq, (qq, "z0"), (qq, "z1")]
                            if r == 0:
                                I("pe", lambda e, sps=sps, i=i, G=G: e.matmul(
                                    sps[:, 0:512], lhsT=k[:, i * 128:(i + 1) * 128],
                                    rhs=qq[:, G, :, :].rearrange("p c q -> p (c q)"), start=True, stop=True), qdeps, [sps])
                            else:
                                for cmp_ in range(2):
                                    I("pe", lambda e, sps=sps, cmp_=cmp_, i=i, G=G: e.matmul(
                                        sps[:, cmp_ * 256:cmp_ * 256 + 128], lhsT=k[:, i * 128:(i + 1) * 128],
                                        rhs=qq[:, G, cmp_, 128:256], start=True, stop=True), qdeps, [sps])

                        def emit_rest(n, v=v):
                            G, i = steps[n]
                            r, sps, pt = info.pop(n)
                            if r == 0:
                                I("act", lambda e, sps=sps, pt=pt: e.activation(out=pt[:], in_=sps[:], func=AF.Exp, scale=0.125),
                                  [], [sps, pt])
                            else:
                                I("act", lambda e, sps=sps, pt=pt: e.activation(
                                    out=pt[:].rearrange("p (c q) -> p c q", c=2)[:, :, 0:128],
                                    in_=sps[:].rearrange("p (c q) -> p c q", c=2)[:, :, 0:128], func=AF.Exp, scale=0.125),
                                  [], [sps, pt])
                            if i >= 2 * G:
                                for cmp_ in range(2):
                                    ds_ = slice(cmp_ * 256, cmp_ * 256 + 128)
                                    I("dve", lambda e, pt=pt, ds_=ds_: e.tensor_tensor(
                                        out=pt[:, ds_], in0=pt[:, ds_], in1=tri_b[:], op=ALU.mult), [tri_b], [pt])
                            for ql in range(r, 2):
                                for cmp_ in range(2):
                                    acc = psF[(G % 2) * 2 + ql]
                                    col = cmp_ * 256 + (ql - r) * 128
                                    first = (i == 0 and cmp_ == 0)
                                    last = (i == 2 * G + ql)
                                    I("pe", lambda e, acc=acc, pt=pt, col=col, i=i, v=v, first=first, last=last, cmp_=cmp_: e.matmul(
                                        acc[:, cmp_ * 256:cmp_ * 256 + 129], lhsT=pt[:, col:col + 128], rhs=v[:, i, 0:129],
                                        start=first, stop=last, skip_group_check=True),
                                      [pt, v, (v, "ones")], [acc])

                        def emit_epilogue(G, y=y, z=z):
                            par = G % 2
                            bufs = []
                            for ql in range(2):
                                ab = psF[par * 2 + ql]
                                bi = epi_i[0] % 8
                                epi_i[0] += 1
                                stt, o1, o2, jk, on = stp[bi], o1p[bi], o2p[bi], jkp[bi], onp[bi]
                                bufs.append((stt, o2, on))
                                I("dve", lambda e, ab=ab, stt=stt: e.reciprocal(out=stt[:, 0:1], in_=ab[:, 128:129]), [], [ab, (stt, 0)])
                                I("dve", lambda e, ab=ab, stt=stt: e.reciprocal(out=stt[:, 2:3], in_=ab[:, 384:385]), [], [ab, (stt, 2)])
                                I("dve", lambda e, stt=stt: e.tensor_tensor(out=stt[:, 3:4], in0=stt[:, 2:3], in1=sl[:, 4:5], op=ALU.mult),
                                  [(stt, 2), (sl, 4)], [(stt, 3)])
                                I("dve", lambda e, ab=ab, stt=stt, o1=o1: e.tensor_scalar(
                                    out=o1[:], in0=ab[:, 0:128], scalar1=stt[:, 0:1], scalar2=None, op0=ALU.mult),
                                  [(stt, 0)], [ab, o1])
                                I("dve", lambda e, ab=ab, stt=stt, o1=o1, o2=o2: e.scalar_tensor_tensor(
                                    out=o2[:], in0=ab[:, 256:384], scalar=stt[:, 3:4], in1=o1[:], op0=ALU.mult, op1=ALU.add),
                                  [(stt, 3), o1], [ab, o2])
                                I("dve", lambda e, o2=o2, jk=jk, stt=stt: e.scalar_tensor_tensor(
                                    out=jk[:], in0=o2[:], scalar=1.0, in1=o2[:], op0=ALU.mult, op1=ALU.mult, accum_out=stt[:, 4:5]),
                                  [o2], [jk, (stt, 4)])
                                I("dve", lambda e, stt=stt: e.tensor_scalar(out=stt[:, 6:7], in0=stt[:, 4:5], scalar1=1.0 / 128,
                                                                            scalar2=EPS, op0=ALU.mult, op1=ALU.add), [(stt, 4)], [(stt, 6)])

                            def s2():
                                for (stt, o2, on) in bufs:
                                    I("act", lambda e, stt=stt: e.activation(out=stt[:, 7:8], in_=stt[:, 6:7], func=AF.Ln), [(stt, 6)], [(stt, 7)])
                                    I("act", lambda e, stt=stt: e.activation(out=stt[:, 5:6], in_=stt[:, 7:8], func=AF.Exp, scale=-0.5),
                                      [(stt, 7)], [(stt, 5)])

                            def s3():
                                for (stt, o2, on) in bufs:
                                    I("dve", lambda e, o2=o2, on=on, stt=stt: e.tensor_scalar(
                                        out=on[:], in0=o2[:], scalar1=stt[:, 5:6], scalar2=None, op0=ALU.mult), [o2, (stt, 5)], [on])

                            def s4():
                                for ql, (stt, o2, on) in enumerate(bufs):
                                    I("pe", lambda e, on=on, ql=ql: e.transpose(out=psB[:, ql * 128:(ql + 1) * 128], in_=on[:],
                                                                                identity=ident_b[:]), [on, ident_b], [psB])

                            def s5():
                                qcol = 2 * G * 128
                                I("dve", lambda e: e.scalar_tensor_tensor(
                                    out=y[:, qcol:qcol + 256], in0=psB[:, 0:256], scalar=gcol[:, 0:1], in1=z[:, qcol:qcol + 256],
                                    op0=ALU.mult, op1=ALU.mult), [gcol, z], [psB, y])

                            defer(4, s2)
                            defer(6, s3)
                            defer(8, s4)
                            defer(10, s5)

                        LA = 2
                        for n0 in range(min(LA, len(steps))):
                            emit_qk(n0)
                        for n in range(len(steps)):
                            if n + LA < len(steps):
                                emit_qk(n + LA)
                            emit_rest(n)
                            gstep[0] += 1
                            run_due()
                            if n == min(14, len(steps) - 1) and it < NS * 16:
                                run_due(owner_lt=it)
                                issue_loads(it)
                            G, i = steps[n]
                            if i == 2 * G + 1:
                                emit_epilogue(G)
                        defer(12, lambda y=y, h=h, t0=t0: DMA(
                            "sp", y, lambda e: [e.dma_start(out=yT[h, :, t0:t0 + S], in_=y[:])], reads=[y]))
                run_due(force=True)
                sc.barrier()


        def gelu_evac(ps, w, dstt, tmps):
            t1, t2 = tmps
            I("act", lambda e: e.activation(out=t1[:, 0:w], in_=ps[:, 0:w], func=AF.Square), [], [ps, t1])
            I("dve", lambda e: e.tensor_scalar(out=t1[:, 0:w], in0=t1[:, 0:w], scalar1=0.044715, scalar2=1.0,
                                               op0=ALU.mult, op1=ALU.add), [t1], [t1])
            I("dve", lambda e: e.tensor_tensor(out=t2[:, 0:w], in0=ps[:, 0:w], in1=t1[:, 0:w], op=ALU.mult), [t1], [ps, t2])
            I("act", lambda e: e.activation(out=t1[:, 0:w], in_=t2[:, 0:w], func=AF.Sigmoid, scale=1.5957691216057308),
              [t2], [t1])
            I("dve", lambda e: e.tensor_tensor(out=dstt[:, 0:w], in0=ps[:, 0:w], in1=t1[:, 0:w], op=ALU.mult), [t1], [ps, dstt])

        def phase_even_inproj(ei):
            W = ev_w_in[ei]
            with ExitStack() as st:
                wtp = pool(st, "eiw", [128, 8, 1024], BF16, 2, False)
                wstage = sbt(st, "eiws", [128, 8, 1024], F32)
                hTp = pool(st, "eih", [128, 8, 512], BF16, 3)
                t1p = pool(st, "eit1", [128, 512], F32, 2, False)
                t2p = pool(st, "eit2", [128, 512], F32, 2, False)
                stg = pool(st, "eist", [128, 512], BF16, 4)
                stgf = pool(st, "eisf", [128, 8], F32, 3)
                cnt = [0]
                groups = [(0, 1024, "tm", "gelu", u_s), (1024, 1024, "tm", "gelu", va_s), (2048, 1024, "fm", "silu", zaT_s),
                          (3072, 1024, "fm", "copy", xmT_s), (4096, 1024, "tm", "sigm", og_s), (5128, 1024, "fm", "silu", zbT_s),
                          (5120, 8, "tm", "gate", gt_s)]
                load_w_stage(W, groups[0][0], groups[0][1], wstage)
                cast_w(wtp[0], groups[0][1], wstage)
                for gi, (c0, ncols, mode, kind, dst) in enumerate(groups):
                    wt = wtp[gi % 2]
                    mid = None
                    if gi + 1 < len(groups):
                        nc0, nnc = groups[gi + 1][0], groups[gi + 1][1]
                        load_w_stage(W, nc0, nnc, wstage)
                        mid = (lambda gi=gi, nnc=nnc: cast_w(wtp[(gi + 1) % 2], nnc, wstage))
                    if mode == "fm":
                        units = [(j * 128,) for j in range(8)]

                        def evac(ui, c, pss, kind=kind, dst=dst):
                            i = cnt[0]
                            cnt[0] += 1
                            sg = stg[i % 4]
                            fn = AF.Silu if kind == "silu" else AF.Copy
                            I("act", lambda e: e.activation(out=sg[:], in_=pss[0][:], func=fn), [], [pss[0], sg])
                            DMA("sp", sg, lambda e: [e.dma_start(out=dst[ui, :, c * 512:(c + 1) * 512], in_=sg[:])], reads=[sg])
                        proj_fm(wt, units, evac, hTp, mid)
                    else:
                        def evac(tt, half, ps, w, kind=kind, dst=dst):
                            i = cnt[0]
                            cnt[0] += 1
                            if kind == "gate":
                                sf = stgf[i % 3]
                                I("act", lambda e: e.activation(out=sf[:], in_=ps[:, 0:8], func=AF.Copy), [], [ps, sf])
                                DMA("sp", sf, lambda e: [e.dma_start(out=dst[tt * 128:(tt + 1) * 128, :], in_=sf[:])], reads=[sf])
                                return
                            sg = stg[i % 4]
                            if kind == "gelu":
                                gelu_evac(ps, w, sg, (t1p[i % 2], t2p[i % 2]))
                            else:
                                I("act", lambda e: e.activation(out=sg[:], in_=ps[:], func=AF.Sigmoid), [], [ps, sg])
                            DMA("sp", sg, lambda e: [e.dma_start(out=dst[tt * 128:(tt + 1) * 128, half * 512:(half + 1) * 512],
                                                                 in_=sg[:])], reads=[sg])
                        proj_tm(wt, ncols, evac, hTp, mid)
                sc.barrier()

        def phase_even_mix(ei):
            with ExitStack() as st:
                alg = sbt(st, "alg", [128, D], F32)
                wsf = sbt(st, "wsf", [128, 8, 128], F32)
                wsb = sbt(st, "wsb", [128, 8, 128], BF16, False)
                bsT = sbt(st, "bsT", [128, 8], F32)
                cw = sbt(st, "cw", [128, 8, 4], F32)
                cv = sbt(st, "cv", [128, 3, 8], F32)
                gb = sbt(st, "gb", [128, 8], F32)
                Wb = sbt(st, "Wb", [128, 3, 8, 256], BF16, False)
                GN = sbt(st, "GN", [128, 8, 128], F32, False)
                SK = sbt(st, "SK", [128, 8, 128], F32, False)
                tri4 = sbt(st, "tri4", [128, 4, 128], F32, False)
                DMA("sp", alg, lambda e: [e.dma_start(out=alg[:], in_=a_ln_g[ei])], writes=[alg])
                DMA("sp", wsf, lambda e: [e.dma_start(out=wsf[:], in_=a_wsT[ei].rearrange("g s t -> s g t"))], writes=[wsf])
                DMA("sp", bsT, lambda e: [e.dma_start(out=bsT[:], in_=a_bsT[ei])], writes=[bsT])
                DMA("sp", cw, lambda e: [e.dma_start(out=cw[:], in_=convw[ei])], writes=[cw])
                DMA("sp", cv, lambda e: [e.dma_start(out=cv[:], in_=colv[ei])], writes=[cv])
                DMA("sp", gb, lambda e: [e.dma_start(out=gb[:], in_=gate_b[ei])], writes=[gb])
                Wbs = sbt(st, "Wbs", [128, 8, 256], F32)
                for j in range(3):
                    DMA("sp", Wbs, lambda e, j=j: [e.dma_start(out=Wbs[:], in_=b_wqkv[ei, j].rearrange("h (dc p) e -> p (h dc) e", p=128))],
                        writes=[Wbs])
                    I("act", lambda e, j=j: e.activation(out=Wb[:, j, :, :], in_=Wbs[:], func=AF.Copy), [Wbs], [(Wb, j)])
                Wb_keys = [(Wb, j) for j in range(3)]
                for g in range(8):
                    I("dve", lambda e, g=g: e.tensor_tensor(out=wsb[:, g, :], in0=wsf[:, g, :], in1=tri_f[:], op=ALU.mult),
                      [wsf, tri_f], [(wsb, g)])
                    I("dve", lambda e, g=g: e.tensor_scalar(out=GN[:, g, :], in0=ones_f[:], scalar1=cv[:, 1, g:g + 1], scalar2=None,
                                                            op0=ALU.mult), [ones_f, cv], [(GN, g)])
                    I("dve", lambda e, g=g: e.tensor_scalar(out=SK[:, g, :], in0=ones_f[:], scalar1=cv[:, 2, g:g + 1], scalar2=None,
                                                            op0=ALU.mult), [ones_f, cv], [(SK, g)])
                for h in range(4):
                    I("dve", lambda e, h=h: e.tensor_copy(out=tri4[:, h, :], in_=tri_f[:]), [tri_f], [(tri4, h)])
                dgw = sbt(st, "dgw", [128, 8, 4, 128], BF16, False)
                for j in range(8):
                    for k in range(4):
                        I("dve", lambda e, j=j, k=k: e.tensor_scalar(out=dgw[:, j, k, :], in0=ident_f[:], scalar1=cw[:, j, k:k + 1],
                                                                      scalar2=None, op0=ALU.mult), [ident_f, cw], [(dgw, j)])
                wsb_keys = [(wsb, g) for g in range(8)]
                GN_keys = [(GN, g) for g in range(8)]
                SK_keys = [(SK, g) for g in range(8)]
                tri4_keys = [(tri4, h) for h in range(4)]

                tmps = {}

                def xc_keys(xc):
                    return [(xc, j) for j in range(8)]

                def tmp(name, shape, dt, n=2):
                    if name not in tmps:
                        tmps[name] = [pool(st, "t" + name, shape, dt, n, False), 0]
                    p = tmps[name]
                    t = p[0][p[1] % n]
                    p[1] += 1
                    return t

                Cst = sbt(st, "Cst", [128, 4, 2, 260], F32, False)
                Cb = sbt(st, "Cb", [128, 4, 2, 260], BF16, False)

                up = pool(st, "mu", [128, D], BF16, 2)
                vap = pool(st, "mva", [128, D], BF16, 2)
                ogp = pool(st, "mog", [128, D], BF16, 2)
                gtp = pool(st, "mgt", [128, 8], F32, 2)
                zap = pool(st, "mza", [128, 8, 128], BF16, 2)
                zbp = pool(st, "mzb", [128, 8, 128], BF16, 2)
                xmp = pool(st, "mxm", [128, 8, 131], BF16, 2)
                yap = pool(st, "mya", [128, 8, 128], BF16, 2)
                ybp = pool(st, "myb", [128, 8, 128], BF16, 2)
                def tile_loads(tt):
                    c = tt % CPS
                    tok0 = tt * 128
                    b2 = tt % 2
                    ut, vat, ogt, gtt, zat, zbt, xmt = up[b2], vap[b2], ogp[b2], gtp[b2], zap[b2], zbp[b2], xmp[b2]
                    rows = slice(tok0, tok0 + 128)
                    DMA("sp", ut, lambda e, ut=ut, rows=rows: [e.dma_start(out=ut[:], in_=u_s[rows, :])], writes=[ut])
                    DMA("sp", vat, lambda e, vat=vat, rows=rows: [e.dma_start(out=vat[:], in_=va_s[rows, :])], writes=[vat])
                    DMA("sp", ogt, lambda e, ogt=ogt, rows=rows: [e.dma_start(out=ogt[:], in_=og_s[rows, :])], writes=[ogt])
                    DMA("sp", gtt, lambda e, gtt=gtt, rows=rows: [e.dma_start(out=gtt[:], in_=gt_s[rows, :])], writes=[gtt])
                    DMA("sp", zat, lambda e, zat=zat, rows=rows: [e.dma_start(
                        out=zat[:], in_=zaT_s[:, :, rows].rearrange("k p t -> p k t"))], writes=[zat])
                    DMA("sp", zbt, lambda e, zbt=zbt, rows=rows: [e.dma_start(
                        out=zbt[:], in_=zbT_s[:, :, rows].rearrange("k p t -> p k t"))], writes=[zbt])
                    if c == 0:
                        I("dve", lambda e, xmt=xmt: e.memset(xmt[:, :, 0:3], 0.0), [], [xmt])
                        DMA("sp", xmt, lambda e, xmt=xmt, rows=rows: [e.dma_start(
                            out=xmt[:, :, 3:131], in_=xmT_s[:, :, rows].rearrange("k p t -> p k t"))], writes=[xmt])
                    else:
                        DMA("sp", xmt, lambda e, xmt=xmt, tok0=tok0: [e.dma_start(
                            out=xmt[:], in_=xmT_s[:, :, tok0 - 3:tok0 + 128].rearrange("k p t -> p k t"))], writes=[xmt])


                def do_tile(tt):
                    c = tt % CPS
                    tok0 = tt * 128
                    b2 = tt % 2
                    ut, vat, ogt, gtt, zat, zbt, xmt = up[b2], vap[b2], ogp[b2], gtp[b2], zap[b2], zbp[b2], xmp[b2]
                    rows = slice(tok0, tok0 + 128)
                    if tt + 1 < NT:
                        tile_loads(tt + 1)
                    bst = tmp("bst", [128, 2, 6], F32)
                    mv = tmp("mv", [128, 8], F32)
                    for hf in range(2):
                        I("dve", lambda e, hf=hf, bst=bst, vat=vat: e.bn_stats(out=bst[:, hf, :], in_=vat[:, hf * 512:(hf + 1) * 512]),
                          [vat], [(bst, hf)])
                    I("dve", lambda e, bst=bst, mv=mv: e.bn_aggr(out=mv[:, 0:2], in_=bst[:].rearrange("p a b -> p (a b)")),
                      [(bst, 0), (bst, 1)], [(mv, 0), (mv, 1)])
                    rstd_chain(mv, 1, 1.0, 2)
                    vn = tmp("vn", [128, D], F32, 1)
                    vnb = tmp("vnb", [128, D], BF16)
                    I("dve", lambda e, vn=vn, vat=vat, mv=mv: e.tensor_scalar(out=vn[:], in0=vat[:], scalar1=mv[:, 0:1], scalar2=mv[:, 2:3],
                                                                              op0=ALU.subtract, op1=ALU.mult), [vat, (mv, 0), (mv, 2)], [vn])
                    I("pool", lambda e, vn=vn, vnb=vnb: e.tensor_tensor(out=vnb[:], in0=vn[:], in1=alg[:], op=ALU.mult), [vn, alg], [vnb])
                    psA = [ps_next(), ps_next()]
                    for g in range(8):
                        pg = psA[g // 4]
                        I("pe", lambda e, g=g, pg=pg, vnb=vnb: e.matmul(pg[:, (g % 4) * 128:(g % 4 + 1) * 128], lhsT=wsb[:, g, :],
                                                                        rhs=vnb[:, g * 128:(g + 1) * 128], start=True, stop=True),
                          [(wsb, g), vnb], [pg])
                    ya = tmp("ya", [128, D], BF16)
                    for g in range(8):
                        pg = psA[g // 4]
                        I("dve", lambda e, g=g, pg=pg, ya=ya, ut=ut: e.scalar_tensor_tensor(
                            out=ya[:, g * 128:(g + 1) * 128], in0=pg[:, (g % 4) * 128:(g % 4 + 1) * 128], scalar=bsT[:, g:g + 1],
                            in1=ut[:, g * 128:(g + 1) * 128], op0=ALU.add, op1=ALU.mult), [bsT, ut], [pg, (ya, g)])
                    for g in range(8):
                        I("pe", lambda e, g=g, ya=ya: e.transpose(out=psB[:, g * 128:(g + 1) * 128], in_=ya[:, g * 128:(g + 1) * 128],
                                                                  identity=ident_b[:]), [(ya, g), ident_b], [psB])
                    yat = yap[b2]
                    I("dve", lambda e, yat=yat, zat=zat: e.tensor_tensor(out=yat[:], in0=psB[:].rearrange("p (k t) -> p k t", k=8),
                                                                         in1=zat[:], op=ALU.mult), [zat], [psB, yat])
                    DMA("sp", yat, lambda e, yat=yat, rows=rows: [e.dma_start(out=yT[0:8, :, rows].rearrange("k p t -> p k t"), in_=yat[:])],
                        reads=[yat])

                    xc = tmp("xc", [128, 8, 128], BF16)
                    psc = [ps_next(), ps_next()]
                    for j in range(8):
                        pg = psc[j // 4]
                        for k in range(4):
                            I("pe", lambda e, j=j, k=k, pg=pg: e.matmul(
                                pg[:, (j % 4) * 128:(j % 4 + 1) * 128], lhsT=dgw[:, j, k, :], rhs=xmt[:, j, k:k + 128],
                                start=(k == 0), stop=(k == 3)), [(dgw, j), xmt], [pg])
                    for j in range(8):
                        pg = psc[j // 4]
                        I("act", lambda e, j=j, pg=pg: e.activation(
                            out=xc[:, j, :], in_=pg[:, (j % 4) * 128:(j % 4 + 1) * 128], func=AF.Silu, bias=cv[:, 0, j:j + 1]),
                          [cv], [pg, (xc, j)])
                    qb_ = tmp("qb", [128, 4, 2, 128], BF16)
                    kb_ = tmp("kb", [128, 4, 2, 128], BF16)
                    qs_ = tmp("qs", [128, 4, 2, 128], BF16)
                    psq = [ps_next(), ps_next()]
                    for (wi, pss, dstb) in ((0, psq, qb_), (1, [ps_next(), ps_next()], kb_)):
                        for h in range(4):
                            pg = pss[h // 2]
                            for ec in range(2):
                                o0 = (h % 2) * 256 + ec * 128
                                for dc in range(2):
                                    I("pe", lambda e, wi=wi, pg=pg, h=h, ec=ec, dc=dc, o0=o0, xc=xc: e.matmul(
                                        pg[:, o0:o0 + 128], lhsT=Wb[:, wi, h * 2 + dc, ec * 128:(ec + 1) * 128], rhs=xc[:, h * 2 + dc, :],
                                        start=(dc == 0), stop=(dc == 1)), Wb_keys + xc_keys(xc), [pg])
                        for bk in range(2):
                            I("act", lambda e, pss=pss, bk=bk, dstb=dstb, wi=wi: e.activation(
                                out=dstb[:, bk * 2:bk * 2 + 2, :, :], in_=pss[bk][:].rearrange("p (h c t) -> p h c t", h=2, c=2),
                                func=AF.Copy, scale=(1.0 if wi == 0 else 0.0625)), [], [pss[bk], (dstb, bk)])
                    psk = [ps_next(), ps_next()]
                    for h in range(4):
                        for dc in range(2):
                            I("pe", lambda e, h=h, dc=dc, xc=xc: e.matmul(
                                psk[h // 2][:, (h % 2) * 256:(h % 2 + 1) * 256], lhsT=xc[:, h * 2 + dc, :], rhs=Wb[:, 1, h * 2 + dc, :],
                                start=(dc == 0), stop=(dc == 1)), Wb_keys + xc_keys(xc), [psk[h // 2]])
                    ktm = tmp("ktm", [128, 4, 256], BF16)
                    for bk in range(2):
                        I("act", lambda e, bk=bk, ktm=ktm: e.activation(
                            out=ktm[:, bk * 2:bk * 2 + 2, :], in_=psk[bk][:].rearrange("p (h e) -> p h e", h=2), func=AF.Copy,
                            scale=0.0625), [], [psk[bk], (ktm, bk)])
                    va_ = tmp("vaug", [128, 4, 260], BF16)
                    I("pool", lambda e, va_=va_: e.memset(va_[:, :, 256:257], 1.0), [], [(va_, "o")])
                    for bk in range(2):
                        psv = ps_next()
                        for hh in range(2):
                            h = bk * 2 + hh
                            for dc in range(2):
                                I("pe", lambda e, h=h, hh=hh, dc=dc, psv=psv, xmt=xmt: e.matmul(
                                    psv[:, hh * 256:(hh + 1) * 256], lhsT=xmt[:, h * 2 + dc, 3:131], rhs=Wb[:, 2, h * 2 + dc, :],
                                    start=(dc == 0), stop=(dc == 1)), Wb_keys + [xmt], [psv])
                        I("act", lambda e, psv=psv, bk=bk, va_=va_: e.activation(
                            out=va_[:, bk * 2:bk * 2 + 2, 0:256], in_=psv[:].rearrange("p (h e) -> p h e", h=2), func=AF.Copy),
                          [], [psv, (va_, bk)])
                    gs = tmp("gs", [128, 40], F32)
                    I("dve", lambda e, gs=gs, gtt=gtt: e.tensor_tensor(out=gs[:, 0:8], in0=gtt[:], in1=gb[:], op=ALU.add), [gtt, gb], [(gs, 0)])
                    I("act", lambda e, gs=gs: e.activation(out=gs[:, 8:12], in_=gs[:, 4:8], func=AF.Exp, scale=-1.0), [(gs, 0)], [(gs, 1)])
                    I("dve", lambda e, gs=gs: e.tensor_scalar(out=gs[:, 12:16], in0=gs[:, 8:12], scalar1=1.0, scalar2=None, op0=ALU.add),
                      [(gs, 1)], [(gs, 2)])
                    I("act", lambda e, gs=gs: e.activation(out=gs[:, 16:20], in_=gs[:, 12:16], func=AF.Ln), [(gs, 2)], [(gs, 3)])
                    psg = ps_next()
                    I("pe", lambda e, gs=gs, psg=psg: e.matmul(psg[:, 0:4], lhsT=tri_f[:], rhs=gs[:, 16:20], start=True, stop=True),
                      [tri_f, (gs, 3)], [psg])
                    I("dve", lambda e, gs=gs, psg=psg: e.tensor_copy(out=gs[:, 20:24], in_=psg[:, 0:4]), [], [psg, (gs, 4)])
                    I("dve", lambda e, gs=gs: e.tensor_tensor(out=gs[:, 24:28], in0=gs[:, 0:4], in1=gs[:, 20:24], op=ALU.add),
                      [(gs, 0), (gs, 4)], [(gs, 5)])
                    dg = tmp("dg", [128, 4, 128], F32, 1)
                    for h in range(4):
                        I("dve", lambda e, h=h, dg=dg, gs=gs: e.tensor_scalar(out=dg[:, h, :], in0=ident_f[:], scalar1=gs[:, 20 + h:21 + h],
                                                                              scalar2=-1.0, op0=ALU.mult, op1=ALU.mult),
                          [ident_f, (gs, 4)], [(dg, h)])
                    psr = ps_next()
                    I("pe", lambda e, psr=psr, dg=dg: e.matmul(psr[:], lhsT=ones_f[:], rhs=dg[:].rearrange("p h t -> p (h t)"),
                                                               start=True, stop=True), [ones_f] + [(dg, h) for h in range(4)], [psr])
                    PT = tmp("PT", [128, 4, 128], F32, 1)
                    for h in range(4):
                        I("act", lambda e, h=h, PT=PT, psr=psr, gs=gs: e.activation(
                            out=PT[:, h, :], in_=psr[:, h * 128:(h + 1) * 128], func=AF.Exp, bias=gs[:, 24 + h:25 + h]),
                          [(gs, 5)], [psr, (PT, h)])
                    EB = tmp("EB", [128, 4, 128], F32, 1)
                    I("act", lambda e, EB=EB, psr=psr: e.activation(out=EB[:].rearrange("p h t -> p (h t)"), in_=psr[:], func=AF.Exp),
                      [], [psr, EB])
                    I("dve", lambda e, gs=gs, psr=psr: e.tensor_copy(
                        out=gs[:, 28:32], in_=psr[:].rearrange("p (h t) -> p h t", h=4)[:, :, 127]), [], [psr, (gs, 6)])
                    I("pool", lambda e, PT=PT: e.tensor_tensor(out=PT[:], in0=PT[:], in1=tri4[:], op=ALU.mult),
                      tri4_keys + [(PT, h) for h in range(4)], [(PT, h) for h in range(4)])
                    if c > 0:
                        for bk in range(2):
                            for ec in range(2):
                                I("dve", lambda e, bk=bk, ec=ec, qs_=qs_, EB=EB, qb_=qb_: e.tensor_tensor(
                                    out=qs_[:, bk * 2:bk * 2 + 2, ec, :], in0=qb_[:, bk * 2:bk * 2 + 2, ec, :],
                                    in1=EB[:, bk * 2:bk * 2 + 2, :], op=ALU.mult), [EB, (qb_, bk)], [(qs_, bk, ec)])
                    psqk = ps_next()
                    for h in range(4):
                        for ec in range(2):
                            I("pe", lambda e, h=h, ec=ec, psqk=psqk, kb_=kb_, qb_=qb_: e.matmul(
                                psqk[:, h * 128:(h + 1) * 128], lhsT=kb_[:, h, ec, :], rhs=qb_[:, h, ec, :],
                                start=(ec == 0), stop=(ec == 1)), [(kb_, 0), (kb_, 1), (qb_, 0), (qb_, 1)], [psqk])
                    scT = tmp("scT", [128, 4, 128], BF16)
                    I("dve", lambda e, scT=scT, psqk=psqk, PT=PT: e.tensor_tensor(
                        out=scT[:].rearrange("p h t -> p (h t)"), in0=psqk[:], in1=PT[:].rearrange("p h t -> p (h t)"), op=ALU.mult),
                      [(PT, h) for h in range(4)], [psqk, scT])
                    htn = tmp("htn", [128, D], BF16)
                    for h in range(4):
                        acc = ps_next()
                        I("pe", lambda e, h=h, acc=acc, scT=scT, va_=va_: e.matmul(
                            acc[:, 0:257], lhsT=scT[:, h, :], rhs=va_[:, h, 0:257], start=True, stop=(c == 0)),
                          [scT, (va_, "o"), (va_, h // 2)], [acc])
                        if c > 0:
                            for dc in range(2):
                                I("pe", lambda e, h=h, dc=dc, acc=acc, qs_=qs_: e.matmul(
                                    acc[:, 0:257], lhsT=qs_[:, h, dc, :], rhs=Cb[:, h, dc, 0:257], start=False, stop=(dc == 1)),
                                  [(qs_, h // 2, dc), (Cb, h, dc)], [acc])
                        hs = tmp("hs", [128, 16], F32)
                        ht = tmp("ht", [128, 256], F32)
                        I("act", lambda e, hs=hs, acc=acc: e.activation(out=hs[:, 8:9], in_=acc[:, 256:257], func=AF.Abs),
                          [], [acc, (hs, 8)])
                        I("dve", lambda e, hs=hs: e.tensor_scalar(out=hs[:, 10:11], in0=hs[:, 8:9], scalar1=1.0, scalar2=None,
                                                                  op0=ALU.max), [(hs, 8)], [(hs, 10)])
                        I("dve", lambda e, hs=hs: e.reciprocal(out=hs[:, 9:10], in_=hs[:, 10:11]), [(hs, 10)], [(hs, 9)])
                        I("dve", lambda e, hs=hs, acc=acc, ht=ht, h=h, ogt=ogt: e.scalar_tensor_tensor(
                            out=ht[:], in0=acc[:, 0:256], scalar=hs[:, 9:10], in1=ogt[:, h * 256:(h + 1) * 256],
                            op0=ALU.mult, op1=ALU.mult), [(hs, 9), ogt], [acc, ht])
                        bs2 = tmp("bs2", [128, 6], F32)
                        I("dve", lambda e, bs2=bs2, ht=ht: e.bn_stats(out=bs2[:], in_=ht[:]), [ht], [bs2])
                        I("dve", lambda e, bs2=bs2, hs=hs: e.bn_aggr(out=hs[:, 0:2], in_=bs2[:]), [bs2], [(hs, 0), (hs, 1)])
                        rstd_chain(hs, 1, 1.0, 2)
                        I("dve", lambda e, hs=hs, ht=ht, htn=htn, h=h: e.tensor_scalar(
                            out=htn[:, h * 256:(h + 1) * 256], in0=ht[:], scalar1=hs[:, 0:1], scalar2=hs[:, 2:3],
                            op0=ALU.subtract, op1=ALU.mult), [ht, (hs, 0), (hs, 2)], [(htn, h)])
                    for g in range(8):
                        I("pe", lambda e, g=g, htn=htn: e.transpose(out=psB[:, g * 128:(g + 1) * 128], in_=htn[:, g * 128:(g + 1) * 128],
                                                                    identity=ident_b[:]), [(htn, g // 2), ident_b], [psB])
                    y1 = tmp("y1", [128, 8, 128], F32, 1)
                    y2 = tmp("y2", [128, 8, 128], F32, 1)
                    I("dve", lambda e, y1=y1: e.tensor_tensor(out=y1[:], in0=psB[:].rearrange("p (k t) -> p k t", k=8), in1=GN[:],
                                                              op=ALU.mult), GN_keys, [psB, y1])
                    I("pool", lambda e, y2=y2, xc=xc: e.tensor_tensor(out=y2[:], in0=xc[:], in1=SK[:], op=ALU.mult), SK_keys + xc_keys(xc), [y2])
                    I("dve", lambda e, y1=y1, y2=y2: e.tensor_tensor(out=y1[:], in0=y1[:], in1=y2[:], op=ALU.add), [y1, y2], [y1])
                    ybt = ybp[b2]
                    I("dve", lambda e, y1=y1, ybt=ybt, zbt=zbt: e.tensor_tensor(out=ybt[:], in0=y1[:], in1=zbt[:], op=ALU.mult),
                      [y1, zbt], [ybt])
                    DMA("sp", ybt, lambda e, ybt=ybt, rows=rows: [e.dma_start(out=yT[8:16, :, rows].rearrange("k p t -> p k t"), in_=ybt[:])],
                        reads=[ybt])
                    if c < CPS - 1:
                        I("dve", lambda e, gs=gs: e.tensor_tensor(out=gs[:, 32:36], in0=gs[:, 24:28], in1=gs[:, 28:32], op=ALU.add),
                          [(gs, 5), (gs, 6)], [(gs, 7)])
                        I("act", lambda e, gs=gs: e.activation(out=gs[:, 32:36], in_=gs[:, 32:36], func=AF.Exp), [(gs, 7)], [(gs, 7)])
                        I("act", lambda e, gs=gs: e.activation(out=gs[:, 36:40], in_=gs[:, 28:32], func=AF.Exp), [(gs, 6)], [(gs, 8)])
                        kw = tmp("kw", [128, 4, 256], BF16)
                        for h in range(4):
                            I("dve", lambda e, h=h, kw=kw, gs=gs, ktm=ktm: e.tensor_scalar(
                                out=kw[:, h, :], in0=ktm[:, h, :], scalar1=gs[:, 32 + h:33 + h],
                                scalar2=None, op0=ALU.mult), [(gs, 7), (ktm, h // 2)], [(kw, h)])
                        for h in range(4):
                            for dc in range(2):
                                psu = ps_next()
                                I("pe", lambda e, h=h, dc=dc, psu=psu, kw=kw, va_=va_: e.matmul(
                                    psu[:, 0:257], lhsT=kw[:, h, dc * 128:(dc + 1) * 128], rhs=va_[:, h, 0:257], start=True, stop=True),
                                  [(kw, h), (va_, "o"), (va_, h // 2)], [psu])
                                if c == 0:
                                    I("dve", lambda e, h=h, dc=dc, psu=psu: e.tensor_copy(out=Cst[:, h, dc, 0:257], in_=psu[:, 0:257]),
                                      [], [psu, (Cst, h, dc)])
                                else:
                                    I("dve", lambda e, h=h, dc=dc, psu=psu, gs=gs: e.scalar_tensor_tensor(
                                        out=Cst[:, h, dc, 0:257], in0=Cst[:, h, dc, 0:257], scalar=gs[:, 36 + h:37 + h], in1=psu[:, 0:257],
                                        op0=ALU.mult, op1=ALU.add), [(gs, 8)], [psu, (Cst, h, dc)])
                                I("act", lambda e, h=h, dc=dc: e.activation(out=Cb[:, h, dc, 0:257], in_=Cst[:, h, dc, 0:257], func=AF.Copy),
                                  [(Cst, h, dc)], [(Cb, h, dc)])
                tile_loads(0)
                for tt in range(NT):
                    do_tile(tt)
                sc.barrier()

        import os as _os
        STOP = _os.environ.get("KSTOP", "")

        def phase_copyout():
            with ExitStack() as st:
                xp = pool(st, "cox", [128, D], F32, 2)
                for tt in range(NT):
                    xt = xp[tt % 2]
                    DMA("sp", xt, lambda e, xt=xt, tt=tt: [e.dma_start(out=xt[:], in_=xs[tt * 128:(tt + 1) * 128, :])], writes=[xt])
                    DMA("sp", xt, lambda e, xt=xt, tt=tt: [e.dma_start(out=out[tt * 128:(tt + 1) * 128, :], in_=xt[:])], reads=[xt])
                sc.barrier()

        def run_all():
            phase_norm0(layers[0][2])
            if STOP == "norm0":
                return phase_copyout()
            for li, (kind, pi, lno) in enumerate(layers):
                if kind == "odd":
                    phase_odd_inproj(pi)
                    if STOP == "inproj":
                        return phase_copyout()
                    phase_attn(pi, lno)
                    if STOP == "attn":
                        return phase_copyout()
                    phase_out(li, od_w_out[pi])
                else:
                    phase_even_inproj(pi)
                    if STOP == "inproj":
                        return phase_copyout()
                    phase_even_mix(pi)
                    if STOP == "attn":
                        return phase_copyout()
                    phase_out(li, ev_w_out[pi])

        run_all()
        sc.barrier(["sp"])
        sc.emit()
    return nc


def prep_inputs(inputs, S):
    f = lambda a: np.ascontiguousarray(np.asarray(a, dtype=np.float32))
    rep = lambda a: np.ascontiguousarray(np.broadcast_to(f(a)[None], (128,) + tuple(np.shape(a))))
    shared = {}
    shared["gvec"] = rep(np.concatenate([f(inputs["norm_g"]), f(inputs["final_g"])[None]], axis=0))
    shared["ident"] = np.eye(128, dtype=np.float32)
    shared["tri"] = np.triu(np.ones((128, 128), np.float32))
    cosT, sinT = rope_tables_T(S)
    shared["cosT"], shared["sinT"] = cosT, sinT
    shared["ev_w_in"] = f(inputs["ev_w_in"])
    shared["ev_w_out"] = f(inputs["ev_w_out"])
    shared["od_w_in"] = f(inputs["od_w_in"])
    pm = np.zeros((128, 128), np.float32)
    pm[np.arange(128), np.arange(128) ^ 32] = 1.0
    shared["perm"] = pm
    shared["od_w_out"] = f(inputs["od_w_out"])
    shared["a_ln_g"] = np.ascontiguousarray(np.broadcast_to(f(inputs["a_ln_g"])[:, None, :], (2, 128, D)))
    shared["a_wsT"] = np.ascontiguousarray(np.swapaxes(f(inputs["a_ws"]), 2, 3))
    shared["a_bsT"] = np.ascontiguousarray(np.swapaxes(f(inputs["a_bs"]), 1, 2))
    cw = f(inputs["b_conv_w"])
    shared["convw"] = np.ascontiguousarray(cw.reshape(2, 4, 8, 128).transpose(0, 3, 2, 1))
    colv = np.stack([f(inputs["b_conv_b"]), f(inputs["b_gn_g"]), f(inputs["b_skip"])], axis=1)
    shared["colv"] = np.ascontiguousarray(colv.reshape(2, 3, 8, 128).transpose(0, 3, 1, 2))
    shared["b_wqkv"] = np.ascontiguousarray(np.stack([f(inputs["b_wq"]), f(inputs["b_wk"]), f(inputs["b_wv"])], axis=1))
    gb = np.concatenate([f(inputs["b_ig_b"]), f(inputs["b_fg_b"])], axis=1)
    shared["gate_b"] = np.ascontiguousarray(np.broadcast_to(gb[:, None, :], (2, 128, 8)))
    lv = np.stack([f(inputs["c_lam_q1"]), f(inputs["c_lam_k1"]), f(inputs["c_lam_q2"]), f(inputs["c_lam_k2"])], axis=1)
    shared["lamv"] = np.ascontiguousarray(np.broadcast_to(lv[:, None], (2, 128, 4, 64)))
    shared["subln"] = np.ascontiguousarray(f(inputs["c_subln_g"]).reshape(2, 128, 1))
    return shared


_PROG_CACHE = {}


def kernel(**inputs):
    x = np.ascontiguousarray(inputs["x"], dtype=np.float32)
    B, S, _ = x.shape
    NS = B // NCORES
    key = (NS, S)
    if key not in _PROG_CACHE:
        _PROG_CACHE[key] = build_program(NS, S)
    nc = _PROG_CACHE[key]
    shared = prep_inputs(inputs, S)
    in_maps = []
    for c in range(NCORES):
        m = dict(shared)
        m["x"] = x[c * NS:(c + 1) * NS].reshape(NS * S, D)
        in_maps.append(m)
    res = run_bass_kernel_spmd(nc, in_maps, core_ids=list(range(NCORES)))
    outs = [r["out"].reshape(NS, S, D) for r in res.results]
    return np.concatenate(outs, axis=0)
```

```python
import math
from contextlib import ExitStack

import numpy as np
import concourse.bass as bass
import concourse.mybir as mybir
from concourse.bass_utils import run_bass_kernel_spmd

F32 = mybir.dt.float32
BF16 = mybir.dt.bfloat16
AF = mybir.ActivationFunctionType
ALU = mybir.AluOpType
AX = mybir.AxisListType

D = 1024
NCORES = 8
EPS = 1e-6
EVEN_IN = 6152
OD_EXT = 8192
N_DMA_SEMS = 84
FULL_LAYERS = (("even", 0, 0), ("odd", 0, 1), ("even", 1, 2), ("odd", 1, 3))


class Sched:
    ENGS = ("pe", "act", "dve", "pool", "sp")

    def __init__(self, nc, es):
        self.nc = nc
        self.es = es
        self.prog = {e: [] for e in self.ENGS}
        self.idx = {e: 0 for e in self.ENGS}
        self.seen = {e: {} for e in self.ENGS}
        self.cnt = {}
        self.last_w = {}
        self.readers = {}
        self.eng_sem = {}
        for e in ("pe", "act", "dve", "pool"):
            self.eng_sem[e] = self.new_sem("c_" + e)
        self.dma_sems = [self.new_sem(f"dq{i}") for i in range(N_DMA_SEMS)]
        self.dma_i = 0
        self.n_ops = 0

    def new_sem(self, name):
        s = self.es.enter_context(self.nc.semaphore(name))
        self.cnt[s] = 0
        return s

    def get_dma_sem(self):
        s = self.dma_sems[self.dma_i % N_DMA_SEMS]
        self.dma_i += 1
        return s

    def op(self, eng, fn, reads=(), writes=(), dma_sem=None, ndma=1):
        deps = []
        for k in reads:
            t = self.last_w.get(k)
            if t is not None:
                deps.append(t)
        for k in writes:
            t = self.last_w.get(k)
            if t is not None:
                deps.append(t)
            deps.extend(self.readers.get(k, ()))
        need = {}
        seen = self.seen[eng]
        cur = self.idx[eng]
        for (sem, val, teng, tidx, is_dma) in deps:
            if teng == eng and not is_dma:
                if eng == "pe":
                    continue
            if seen.get(sem, 0) >= val:
                continue
            if need.get(sem, 0) < val:
                need[sem] = val
        if dma_sem is not None:
            prev = self.cnt[dma_sem]
            if prev > 0 and seen.get(dma_sem, 0) < prev:
                need[dma_sem] = prev
        for sem, val in need.items():
            self.prog[eng].append(("w", sem, val))
            seen[sem] = val
        if dma_sem is None:
            sem = self.eng_sem[eng]
            self.cnt[sem] += 1
            is_dma = False
        else:
            sem = dma_sem
            self.cnt[sem] += 16 * ndma
            is_dma = True
        tok = (sem, self.cnt[sem], eng, cur, is_dma)
        self.prog[eng].append(("o", fn, sem))
        for k in writes:
            self.last_w[k] = tok
            self.readers[k] = []
        for k in reads:
            self.readers.setdefault(k, []).append(tok)
        self.idx[eng] = cur + 1
        self.n_ops += 1
        return tok

    def barrier(self, engs=None):
        for eng in (engs or self.ENGS):
            for sem, val in self.cnt.items():
                if val > 0 and self.seen[eng].get(sem, 0) < val:
                    self.prog[eng].append(("w", sem, val))
                    self.seen[eng][sem] = val
        if engs is None:
            self.last_w = {}
            self.readers = {}

    def emit(self):
        nc = self.nc
        engobj = {"pe": "tensor", "act": "scalar", "dve": "vector", "pool": "gpsimd", "sp": "sync"}
        with nc.Block() as block:
            for e in self.ENGS:
                prog = self.prog[e]

                def body(eng, prog=prog):
                    for it in prog:
                        if it[0] == "w":
                            eng.wait_ge(it[1], it[2])
                        else:
                            it[1](eng, it[2])

                getattr(block, engobj[e])(body)


class Tl:
    def __init__(self, t, sem=None):
        self.t = t
        self.sem = sem

    def __getitem__(self, k):
        return self.t[k]


def rope_tables_T(S):
    inv = (10000.0 ** (-np.arange(0, 64, 2, dtype=np.float32) / np.float32(64))).astype(np.float32)
    ang = (np.arange(S, dtype=np.float32)[:, None] * inv[None, :]).astype(np.float32)
    cos, sin = np.cos(ang).astype(np.float32), np.sin(ang).astype(np.float32)
    cosT = np.zeros((128, S), np.float32)
    sinT = np.zeros((128, S), np.float32)
    for p in range(128):
        d = p % 64
        cosT[p] = cos[:, d % 32]
        sinT[p] = -sin[:, d % 32] if d < 32 else sin[:, d % 32]
    return cosT, sinT


def lambda_init(layer):
    return 0.8 - 0.6 * math.exp(-0.3 * layer)


def build_program(NS, S, layers=FULL_LAYERS, dbg=False):
    T = NS * S
    NT = T // 128
    NC5 = T // 512
    CPS = S // 128
    n_layers = len(layers)
    nc = bass.Bass("TRN2", target_bir_lowering=False)
    es = ExitStack()

    def din(name, shape, dt=F32):
        return nc.dram_tensor(name, list(shape), dt, kind="ExternalInput").ap()

    def dscr(name, shape, dt):
        if dbg:
            return nc.dram_tensor(name, list(shape), dt, kind="ExternalOutput").ap()
        return nc.dram_tensor(name, list(shape), dt).ap()

    x_in = din("x", [T, D])
    gvec = din("gvec", [128, 5, D])
    ident_d = din("ident", [128, 128])
    tri_d = din("tri", [128, 128])
    perm_d = din("perm", [128, 128])
    cosT_d = din("cosT", [128, S])
    sinT_d = din("sinT", [128, S])
    ev_w_in = din("ev_w_in", [2, D, EVEN_IN])
    ev_w_out = din("ev_w_out", [2, 2048, D])
    od_w_in = din("od_w_in", [2, D, OD_EXT])
    od_w_out = din("od_w_out", [2, 2048, D])
    a_ln_g = din("a_ln_g", [2, 128, D])
    a_wsT = din("a_wsT", [2, 8, 128, 128])
    a_bsT = din("a_bsT", [2, 128, 8])
    convw = din("convw", [2, 128, 8, 4])
    colv = din("colv", [2, 128, 3, 8])
    b_wqkv = din("b_wqkv", [2, 3, 4, 256, 256])
    gate_b = din("gate_b", [2, 128, 8])
    lamv = din("lamv", [2, 128, 4, 64])
    subln = din("subln", [2, 128, 1])
    out = nc.dram_tensor("out", [T, D], F32, kind="ExternalOutput").ap()

    xs = dscr("xs", [T, D], F32)
    hnT = dscr("hnT", [8, 128, T], BF16)
    yT = dscr("yT", [16, 128, T], BF16)
    qT_s = dscr("qT_s", [16, 128, T], BF16)
    kT_s = dscr("kT_s", [16, 128, T], BF16)
    zT_s = dscr("zT_s", [16, 128, T], BF16)
    v_s = dscr("v_s", [T, 2048], BF16)
    u_s = dscr("u_s", [T, D], BF16)
    va_s = dscr("va_s", [T, D], BF16)
    og_s = dscr("og_s", [T, D], BF16)
    gt_s = dscr("gt_s", [T, 8], F32)
    zaT_s = dscr("zaT_s", [8, 128, T], BF16)
    xmT_s = dscr("xmT_s", [8, 128, T], BF16)
    zbT_s = dscr("zbT_s", [8, 128, T], BF16)

    with es:
        sc = Sched(nc, es)

        def I(eng, f, reads=(), writes=()):
            sc.op(eng, lambda e, s: f(e).then_inc(s, 1), reads, writes)

        def DMA(eng, tl, f, reads=(), writes=(), n=1):
            def g(e, s):
                r = f(e)
                assert len(r) == n
                for x in r:
                    x.then_inc(s, 16)
            sc.op(eng, g, reads, writes, dma_sem=tl.sem, ndma=n)

        uid = [0]

        def sbt(st, name, shape, dt, dma=True):
            uid[0] += 1
            t = st.enter_context(nc.sbuf_tensor(f"{name}_{uid[0]}", list(shape), dt))
            return Tl(t, sc.get_dma_sem() if dma else None)

        def pool(st, name, shape, dt, n, dma=True):
            return [sbt(st, f"{name}{i}", shape, dt, dma) for i in range(n)]

        ident_f = sbt(es, "ident_f", [128, 128], F32)
        ident_b = sbt(es, "ident_b", [128, 128], BF16)
        tri_f = sbt(es, "tri_f", [128, 128], F32)
        tri_b = sbt(es, "tri_b", [128, 128], BF16)
        ones_f = sbt(es, "ones_f", [128, 128], F32)
        perm_f = sbt(es, "perm_f", [128, 128], F32)
        perm_b = sbt(es, "perm_b", [128, 128], BF16)
        psF = [Tl(es.enter_context(nc.psum_tensor(f"psF{i}", [128, 512], F32))) for i in range(7)]
        psB = Tl(es.enter_context(nc.psum_tensor("psB", [128, 1024], BF16)))
        ps_i = [0]

        def ps_next():
            p = psF[ps_i[0] % len(psF)]
            ps_i[0] += 1
            return p

        DMA("sp", ident_f, lambda e: [e.dma_start(out=ident_f[:], in_=ident_d[:, :])], writes=[ident_f])
        DMA("sp", tri_f, lambda e: [e.dma_start(out=tri_f[:], in_=tri_d[:, :])], writes=[tri_f])
        I("dve", lambda e: e.tensor_copy(out=ident_b[:], in_=ident_f[:]), [ident_f], [ident_b])
        I("dve", lambda e: e.tensor_copy(out=tri_b[:], in_=tri_f[:]), [tri_f], [tri_b])
        I("dve", lambda e: e.memset(ones_f[:], 1.0), [], [ones_f])
        DMA("sp", perm_f, lambda e: [e.dma_start(out=perm_f[:], in_=perm_d[:, :])], writes=[perm_f])
        I("dve", lambda e: e.tensor_copy(out=perm_b[:], in_=perm_f[:]), [perm_f], [perm_b])

        def rstd_chain(st, ss_col, scale, out_col):
            I("dve", lambda e: e.tensor_scalar(out=st[:, 6:7], in0=st[:, ss_col:ss_col + 1], scalar1=scale,
                                               scalar2=EPS, op0=ALU.mult, op1=ALU.add),
              [(st, ss_col)], [(st, 6)])
            I("act", lambda e: e.activation(out=st[:, 7:8], in_=st[:, 6:7], func=AF.Ln), [(st, 6)], [(st, 7)])
            I("act", lambda e: e.activation(out=st[:, out_col:out_col + 1], in_=st[:, 7:8], func=AF.Exp, scale=-0.5),
              [(st, 7)], [(st, out_col)])

        def norm_to_hnT(st_pools, xt, tt, gt_, hT, final=False):
            junk, stat, hnp, otp = st_pools
            jt = junk[tt % len(junk)]
            st = stat[tt % len(stat)]
            I("act", lambda e: e.activation(out=jt[:], in_=xt[:], func=AF.Square, accum_out=st[:, 0:1]),
              [xt], [jt, (st, 0)])
            rstd_chain(st, 0, 1.0 / D, 1)
            if final:
                ot = otp[tt % len(otp)]
                I("dve", lambda e: e.scalar_tensor_tensor(out=ot[:], in0=xt[:], scalar=st[:, 1:2], in1=gt_[:],
                                                          op0=ALU.mult, op1=ALU.mult), [xt, (st, 1), gt_], [ot])
                DMA("sp", ot, lambda e: [e.dma_start(out=out[tt * 128:(tt + 1) * 128, :], in_=ot[:])], reads=[ot])
                return
            hn = hnp[tt % len(hnp)]
            I("dve", lambda e: e.scalar_tensor_tensor(out=hn[:], in0=xt[:], scalar=st[:, 1:2], in1=gt_[:],
                                                      op0=ALU.mult, op1=ALU.mult), [xt, (st, 1), gt_], [hn])
            for kc in range(8):
                I("pe", lambda e, kc=kc: e.transpose(out=psB[:, kc * 128:(kc + 1) * 128],
                                                     in_=hn[:, kc * 128:(kc + 1) * 128], identity=ident_b[:]),
                  [hn, ident_b], [psB])
            j = tt % 4
            I("act", lambda e: e.activation(out=hT[:, :, j * 128:(j + 1) * 128],
                                            in_=psB[:].rearrange("p (k t) -> p k t", k=8), func=AF.Copy),
              [], [psB, (hT, j)])
            if j == 3:
                c = tt // 4
                DMA("sp", hT, lambda e: [e.dma_start(out=hnT[:, :, c * 512:(c + 1) * 512].rearrange("k p t -> p k t"),
                                                     in_=hT[:])],
                    reads=[(hT, 0), (hT, 1), (hT, 2), (hT, 3)])

        def phase_norm0(gidx):
            with ExitStack() as st:
                xp = pool(st, "n0x", [128, D], F32, 3)
                pools = (pool(st, "n0j", [128, D], F32, 2, False), pool(st, "n0s", [128, 8], F32, 4, False),
                         pool(st, "n0h", [128, D], BF16, 2, False), None)
                hTp = pool(st, "n0T", [128, 8, 512], BF16, 2)
                gt_ = sbt(st, "gt0", [128, D], F32)
                DMA("sp", gt_, lambda e: [e.dma_start(out=gt_[:], in_=gvec[:, gidx, :])], writes=[gt_])
                for tt in range(NT):
                    xt = xp[tt % 3]
                    DMA("sp", xt, lambda e, xt=xt, tt=tt: [e.dma_start(out=xt[:], in_=x_in[tt * 128:(tt + 1) * 128, :])],
                        writes=[xt])
                    DMA("sp", xt, lambda e, xt=xt, tt=tt: [e.dma_start(out=xs[tt * 128:(tt + 1) * 128, :], in_=xt[:])],
                        reads=[xt])
                    norm_to_hnT(pools, xt, tt, gt_, hTp[(tt // 4) % 2])
                sc.barrier()

        def load_w_stage(w_dram_2d, c0, ncols, stage):
            DMA("sp", stage, lambda e: [e.dma_start(out=stage[:, :, 0:ncols],
                                                    in_=w_dram_2d[:, c0:c0 + ncols].rearrange("(kc p) c -> p kc c", p=128))],
                writes=[stage])

        def cast_w(wt, ncols, stage):
            I("act", lambda e: e.activation(out=wt[:, :, 0:ncols], in_=stage[:, :, 0:ncols], func=AF.Copy), [stage], [wt])

        def load_hT(hT, c):
            DMA("sp", hT, lambda e: [e.dma_start(out=hT[:], in_=hnT[:, :, c * 512:(c + 1) * 512].rearrange("k p t -> p k t"))],
                writes=[hT])

        def proj_fm(wt, units, evac, hTp, mid=None):
            pend_ = [None]
            load_hT(hTp[0], 0)
            for c in range(NC5):
                hT = hTp[c % len(hTp)]
                if c + 1 < NC5:
                    load_hT(hTp[(c + 1) % len(hTp)], c + 1)
                for ui, cols in enumerate(units):
                    pss = []
                    for co in cols:
                        ps = ps_next()
                        for kc in range(8):
                            I("pe", lambda e, ps=ps, kc=kc, co=co, hT=hT: e.matmul(
                                ps[:], lhsT=wt[:, kc, co:co + 128], rhs=hT[:, kc, :], start=(kc == 0), stop=(kc == 7)),
                              [wt, hT], [ps])
                        pss.append(ps)
                    r_ = evac(ui, c, pss)
                    if pend_[0] is not None:
                        pend_[0]()
                    pend_[0] = r_
                if mid is not None and c == min(4, NC5 - 1):
                    mid()
            if pend_[0] is not None:
                pend_[0]()

        def proj_tm(wt, ncols, evac, hTp, mid=None):
            load_hT(hTp[0], 0)
            for c in range(NC5):
                hT = hTp[c % len(hTp)]
                if c + 1 < NC5:
                    load_hT(hTp[(c + 1) % len(hTp)], c + 1)
                for j in range(4):
                    tt = c * 4 + j
                    for half in range((ncols + 511) // 512):
                        w = min(512, ncols - half * 512)
                        ps = ps_next()
                        for kc in range(8):
                            I("pe", lambda e, ps=ps, kc=kc, half=half, w=w, j=j, hT=hT: e.matmul(
                                ps[:, 0:w], lhsT=hT[:, kc, j * 128:(j + 1) * 128],
                                rhs=wt[:, kc, half * 512:half * 512 + w], start=(kc == 0), stop=(kc == 7)),
                              [wt, hT], [ps])
                        evac(tt, half, ps, w)
                if mid is not None and c == min(4, NC5 - 1):
                    mid()

        def phase_out(li, w_out_2d):
            final = (li == n_layers - 1)
            gidx = 4 if final else layers[li + 1][2]
            with ExitStack() as st:
                wo = sbt(st, "wo", [128, 16, D], BF16, False)
                wos = sbt(st, "wos", [128, 8, D], F32)
                for hf in range(2):
                    DMA("sp", wos, lambda e, hf=hf: [e.dma_start(
                        out=wos[:], in_=w_out_2d[hf * 1024:(hf + 1) * 1024, :].rearrange("(kc p) c -> p kc c", p=128))], writes=[wos])
                    I("act", lambda e, hf=hf: e.activation(out=wo[:, hf * 8:(hf + 1) * 8, :], in_=wos[:], func=AF.Copy), [wos], [(wo, hf)])
                gt_ = sbt(st, "gt", [128, D], F32)
                DMA("sp", gt_, lambda e: [e.dma_start(out=gt_[:], in_=gvec[:, gidx, :])], writes=[gt_])
                yp = pool(st, "poy", [128, 16, 512], BF16, 2)
                xp = pool(st, "pox", [128, D], F32, 4)
                pools = (pool(st, "poj", [128, D], F32, 2, False), pool(st, "pos", [128, 8], F32, 4, False),
                         pool(st, "poh", [128, D], BF16, 2, False), pool(st, "poo", [128, D], F32, 2))
                hTp = pool(st, "poT", [128, 8, 512], BF16, 2)
                def load_y(c):
                    yt = yp[c % 2]
                    DMA("sp", yt, lambda e: [e.dma_start(
                        out=yt[:, hf * 8:(hf + 1) * 8, :], in_=yT[hf * 8:(hf + 1) * 8, :, c * 512:(c + 1) * 512].rearrange("k p t -> p k t"))
                        for hf in range(2)], writes=[yt], n=2)

                load_y(0)

                def stage_a(tt):
                    c, j = tt // 4, tt % 4
                    yt = yp[c % 2]
                    if j == 1 and c + 1 < NC5:
                        load_y(c + 1)
                    xt = xp[tt % 4]
                    DMA("sp", xt, lambda e: [e.dma_start(out=xt[:], in_=xs[tt * 128:(tt + 1) * 128, :])], writes=[xt])
                    for half in range(2):
                        ps = ps_next()
                        for kc in range(16):
                            I("pe", lambda e, ps=ps, kc=kc, half=half: e.matmul(
                                ps[:], lhsT=yt[:, kc, j * 128:(j + 1) * 128],
                                rhs=wo[:, kc, half * 512:(half + 1) * 512], start=(kc == 0), stop=(kc == 15)),
                              [(wo, 0), (wo, 1), yt], [ps])
                        I("dve", lambda e, ps=ps, half=half: e.tensor_tensor(
                            out=xt[:, half * 512:(half + 1) * 512], in0=ps[:], in1=xt[:, half * 512:(half + 1) * 512],
                            op=ALU.add), [], [ps, xt])
                    if not final:
                        DMA("sp", xt, lambda e: [e.dma_start(out=xs[tt * 128:(tt + 1) * 128, :], in_=xt[:])], reads=[xt])

                def stage_b(tt):
                    norm_to_hnT(pools, xp[tt % 4], tt, gt_, hTp[(tt // 4) % 2], final=final)

                stage_a(0)
                for tt in range(NT):
                    if tt + 1 < NT:
                        stage_a(tt + 1)
                    stage_b(tt)
                sc.barrier()

        def phase_odd_inproj(o):
            W = od_w_in[o]
            with ExitStack() as st:
                wtp = pool(st, "oiw", [128, 8, 1024], BF16, 2, False)
                wstage = sbt(st, "oiws", [128, 8, 1024], F32)
                hTp = pool(st, "oih", [128, 8, 512], BF16, 3)
                cosT = sbt(st, "cosT", [128, S], F32)
                sinT = sbt(st, "sinT", [128, S], F32)
                DMA("sp", cosT, lambda e: [e.dma_start(out=cosT[:], in_=cosT_d[:, :])], writes=[cosT])
                DMA("sp", sinT, lambda e: [e.dma_start(out=sinT[:], in_=sinT_d[:, :])], writes=[sinT])
                t1p = pool(st, "oit1", [128, 512], F32, 4, False)
                t2p = pool(st, "oit2", [128, 512], F32, 4, False)
                stg = pool(st, "oist", [128, 512], BF16, 4)
                cnt = [0]
                qsp = pool(st, "oiq", [128, 512], BF16, 3, False)
                load_w_stage(W, 0, 1024, wstage)
                cast_w(wtp[0], 1024, wstage)
                NGRP = 8
                for g in range(NGRP):
                    wt = wtp[g % 2]
                    mid = None
                    if g + 1 < NGRP:
                        load_w_stage(W, (g + 1) * 1024, 1024, wstage)
                        mid = (lambda g=g: cast_w(wtp[(g + 1) % 2], 1024, wstage))
                    if g < 4:
                        dst = qT_s if g < 2 else kT_s
                        units = [(j * 128,) for j in range(8)]

                        def evac(ui, c, pss, g=g, dst=dst):
                            i = cnt[0]
                            cnt[0] += 1
                            t1, t2, sg, qs = t1p[i % 4], t2p[i % 4], stg[i % 4], qsp[i % 3]
                            ps = pss[0]
                            p0 = (c * 512) % S
                            I("act", lambda e: e.activation(out=qs[:], in_=ps[:], func=AF.Copy), [], [ps, qs])
                            I("dve", lambda e: e.tensor_tensor(out=t1[:], in0=ps[:], in1=cosT[:, p0:p0 + 512], op=ALU.mult),
                              [cosT], [ps, t1])

                            def later():
                                ps2 = ps_next()
                                I("pe", lambda e: e.matmul(ps2[:], lhsT=perm_b[:], rhs=qs[:], start=True, stop=True), [perm_b, qs], [ps2])
                                I("dve", lambda e: e.tensor_tensor(out=t2[:], in0=ps2[:], in1=sinT[:, p0:p0 + 512], op=ALU.mult),
                                  [sinT], [ps2, t2])
                                I("pool", lambda e: e.tensor_tensor(out=sg[:], in0=t1[:], in1=t2[:], op=ALU.add), [t1, t2], [sg])
                                h = (g % 2) * 8 + ui
                                DMA("sp", sg, lambda e: [e.dma_start(out=dst[h, :, c * 512:(c + 1) * 512], in_=sg[:])], reads=[sg])
                            return later
                        proj_fm(wt, units, evac, hTp, mid)
                    elif g >= 6:
                        units = [(j * 128,) for j in range(8)]

                        def evac(ui, c, pss, g=g):
                            i = cnt[0]
                            cnt[0] += 1
                            sg = stg[i % 4]
                            I("act", lambda e: e.activation(out=sg[:], in_=pss[0][:], func=AF.Silu), [], [pss[0], sg])
                            h = (g - 6) * 8 + ui
                            DMA("sp", sg, lambda e: [e.dma_start(out=zT_s[h, :, c * 512:(c + 1) * 512], in_=sg[:])], reads=[sg])
                        proj_fm(wt, units, evac, hTp, mid)
                    else:
                        def evac(tt, half, ps, w, g=g):
                            i = cnt[0]
                            cnt[0] += 1
                            sg = stg[i % 4]
                            I("act", lambda e: e.activation(out=sg[:], in_=ps[:], func=AF.Copy), [], [ps, sg])
                            c0 = (g - 4) * 1024 + half * 512
                            DMA("sp", sg, lambda e: [e.dma_start(out=v_s[tt * 128:(tt + 1) * 128, c0:c0 + 512], in_=sg[:])],
                                reads=[sg])
                        proj_tm(wt, 1024, evac, hTp, mid)
                sc.barrier()

        def phase_attn(o, layer_no):
            li = lambda_init(layer_no)
            with ExitStack() as st:
                lv = sbt(st, "lv", [128, 4, 64], F32)
                sl = sbt(st, "sl", [128, 8], F32, False)
                gcol = sbt(st, "gcol", [128, 1], F32)
                lj = sbt(st, "lj", [128, 64], F32, False)
                DMA("sp", lv, lambda e: [e.dma_start(out=lv[:], in_=lamv[o])], writes=[lv])
                DMA("sp", gcol, lambda e: [e.dma_start(out=gcol[:], in_=subln[o])], writes=[gcol])
                for i2 in range(2):
                    I("dve", lambda e, i2=i2: e.tensor_tensor(out=lj[:], in0=lv[:, 2 * i2, :], in1=lv[:, 2 * i2 + 1, :], op=ALU.mult),
                      [lv], [lj])
                    I("dve", lambda e, i2=i2: e.reduce_sum(out=sl[:, i2:i2 + 1], in_=lj[:], axis=AX.X), [lj], [(sl, i2)])
                    I("act", lambda e, i2=i2: e.activation(out=sl[:, 2 + i2:3 + i2], in_=sl[:, i2:i2 + 1], func=AF.Exp),
                      [(sl, i2)], [(sl, 2 + i2)])
                I("dve", lambda e: e.tensor_tensor(out=sl[:, 5:6], in0=sl[:, 3:4], in1=sl[:, 2:3], op=ALU.subtract),
                  [(sl, 2), (sl, 3)], [(sl, 5)])
                I("dve", lambda e: e.tensor_scalar(out=sl[:, 4:5], in0=sl[:, 5:6], scalar1=-li, scalar2=None, op0=ALU.add),
                  [(sl, 5)], [(sl, 4)])
                I("dve", lambda e: e.tensor_scalar(out=gcol[:], in0=gcol[:], scalar1=1.0 - li, scalar2=None, op0=ALU.mult),
                  [gcol], [gcol])

                NG = S // 256
                qp = pool(st, "aqq", [128, NG, 2, 256], BF16, 2)
                for b_ in range(2):
                    I("dve", lambda e, b_=b_: e.memset(qp[b_][64:128, :, 0, :], 0.0), [], [(qp[b_], "z0")])
                    I("dve", lambda e, b_=b_: e.memset(qp[b_][0:64, :, 1, :], 0.0), [], [(qp[b_], "z1")])
                kp = pool(st, "ak", [128, S], BF16, 2)
                zp = pool(st, "az", [128, S], BF16, 2)
                vp = pool(st, "av", [128, CPS, 132], BF16, 2)
                yp = pool(st, "ay", [128, S], BF16, 2)
                pp = pool(st, "ap", [128, 512], BF16, 6, False)
                stp = pool(st, "ast", [128, 8], F32, 8, False)
                o1p = pool(st, "ao1", [128, 128], F32, 8, False)
                o2p = pool(st, "ao2", [128, 128], F32, 8, False)
                jkp = pool(st, "ajk", [128, 128], F32, 8, False)
                onp = pool(st, "aon", [128, 128], BF16, 8, False)
                for v_ in vp:
                    I("dve", lambda e, v_=v_: e.memset(v_[:, :, 128:129], 1.0), [], [(v_, "ones")])
                it = 0
                qb = 0
                epi_i = [0]
                gstep = [0]
                pending = []

                def defer(delay, fn):
                    pending.append([gstep[0] + delay, fn, it])

                def run_due(force=False, owner_lt=None):
                    keep = []
                    for item in list(pending):
                        if force or item[0] <= gstep[0] or (owner_lt is not None and item[2] < owner_lt):
                            item[1]()
                        else:
                            keep.append(item)
                    pending[:] = keep

                def issue_loads(idx):
                    s_, h = idx // 16, idx % 16
                    qq, k, z, v = qp[idx % 2], kp[idx % 2], zp[idx % 2], vp[idx % 2]
                    t0 = s_ * S
                    DMA("sp", qq, lambda e: [
                        e.dma_start(out=qq[0:64, :, 0, :], in_=qT_s[h, 0:64, t0:t0 + S].rearrange("p (g q) -> p g q", q=256)),
                        e.dma_start(out=qq[64:128, :, 1, :], in_=qT_s[h, 64:128, t0:t0 + S].rearrange("p (g q) -> p g q", q=256))],
                        writes=[qq], n=2)
                    DMA("sp", k, lambda e: [e.dma_start(out=k[:], in_=kT_s[h, :, t0:t0 + S])], writes=[k])
                    DMA("sp", z, lambda e: [e.dma_start(out=z[:], in_=zT_s[h, :, t0:t0 + S])], writes=[z])
                    nvp = (CPS + 7) // 8
                    DMA("sp", v, lambda e: [e.dma_start(
                        out=v[:, j8 * 8:min(CPS, j8 * 8 + 8), 0:128],
                        in_=v_s[t0 + j8 * 1024:min(t0 + S, t0 + j8 * 1024 + 1024), h * 128:(h + 1) * 128].rearrange(
                            "(c p) e -> p c e", p=128)) for j8 in range(nvp)], writes=[v], n=nvp)

                issue_loads(0)
                for s in range(NS):
                    for h in range(16):
                        qq, k, z, v, y = qp[it % 2], kp[it % 2], zp[it % 2], vp[it % 2], yp[it % 2]
                        it += 1
                        t0 = s * S
                        steps = [(G, i) for G in range(CPS // 2) for i in range(2 * G + 2)]
                        info = {}

                        def emit_qk(n, qq=qq, k=k):
                            nonlocal qb
                            G, i = steps[n]
                            r = 1 if i == 2 * G + 1 else 0
                            sps = psF[4 + qb % 3]
                            pt = pp[qb % 4]
                            qb += 1
                            info[n] = (r, sps, pt)
                            qdeps = [k, qq, (qq, "z0"), (qq, "z1")]
                            if r == 0:
                                I("pe", lambda e, sps=sps, i=i, G=G: e.matmul(
                                    sps[:, 0:512], lhsT=k[:, i * 128:(i + 1) * 128],
                                    rhs=qq[:, G, :, :].rearrange("p c q -> p (c q)"), start=True, stop=True), qdeps, [sps])
                            else:
                                for cmp_ in range(2):
                                    I("pe", lambda e, sps=sps, cmp_=cmp_, i=i, G=G: e.matmul(
                                        sps[:, cmp_ * 256:cmp_ * 256 + 128], lhsT=k[:, i * 128:(i + 1) * 128],
                                        rhs=qq[:, G, cmp_, 128:256], start=True, stop=True), qdeps, [sps])

                        def emit_rest(n, v=v):
                            G, i = steps[n]
                            r, sps, pt = info.pop(n)
                            if r == 0:
                                I("act", lambda e, sps=sps, pt=pt: e.activation(out=pt[:], in_=sps[:], func=AF.Exp, scale=0.125),
                                  [], [sps, pt])
                            else:
                                I("act", lambda e, sps=sps, pt=pt: e.activation(
                                    out=pt[:].rearrange("p (c q) -> p c q", c=2)[:, :, 0:128],
                                    in_=sps[:].rearrange("p (c q) -> p c q", c=2)[:, :, 0:128], func=AF.Exp, scale=0.125),
                                  [], [sps, pt])
                            if i >= 2 * G:
                                for cmp_ in range(2):
                                    ds_ = slice(cmp_ * 256, cmp_ * 256 + 128)
                                    I("dve", lambda e, pt=pt, ds_=ds_: e.tensor_tensor(
                                        out=pt[:, ds_], in0=pt[:, ds_], in1=tri_b[:], op=ALU.mult), [tri_b], [pt])
                            for ql in range(r, 2):
                                for cmp_ in range(2):
                                    acc = psF[(G % 2) * 2 + ql]
                                    col = cmp_ * 256 + (ql - r) * 128
                                    first = (i == 0 and cmp_ == 0)
                                    last = (i == 2 * G + ql)
                                    I("pe", lambda e, acc=acc, pt=pt, col=col, i=i, v=v, first=first, last=last, cmp_=cmp_: e.matmul(
                                        acc[:, cmp_ * 256:cmp_ * 256 + 129], lhsT=pt[:, col:col + 128], rhs=v[:, i, 0:129],
                                        start=first, stop=last, skip_group_check=True),
                                      [pt, v, (v, "ones")], [acc])

                        def emit_epilogue(G, y=y, z=z):
                            par = G % 2
                            bufs = []
                            for ql in range(2):
                                ab = psF[par * 2 + ql]
                                bi = epi_i[0] % 8
                                epi_i[0] += 1
                                stt, o1, o2, jk, on = stp[bi], o1p[bi], o2p[bi], jkp[bi], onp[bi]
                                bufs.append((stt, o2, on))
                                I("dve", lambda e, ab=ab, stt=stt: e.reciprocal(out=stt[:, 0:1], in_=ab[:, 128:129]), [], [ab, (stt, 0)])
                                I("dve", lambda e, ab=ab, stt=stt: e.reciprocal(out=stt[:, 2:3], in_=ab[:, 384:385]), [], [ab, (stt, 2)])
                                I("dve", lambda e, stt=stt: e.tensor_tensor(out=stt[:, 3:4], in0=stt[:, 2:3], in1=sl[:, 4:5], op=ALU.mult),
                                  [(stt, 2), (sl, 4)], [(stt, 3)])
                                I("dve", lambda e, ab=ab, stt=stt, o1=o1: e.tensor_scalar(
                                    out=o1[:], in0=ab[:, 0:128], scalar1=stt[:, 0:1], scalar2=None, op0=ALU.mult),
                                  [(stt, 0)], [ab, o1])
                                I("dve", lambda e, ab=ab, stt=stt, o1=o1, o2=o2: e.scalar_tensor_tensor(
                                    out=o2[:], in0=ab[:, 256:384], scalar=stt[:, 3:4], in1=o1[:], op0=ALU.mult, op1=ALU.add),
                                  [(stt, 3), o1], [ab, o2])
                                I("dve", lambda e, o2=o2, jk=jk, stt=stt: e.scalar_tensor_tensor(
                                    out=jk[:], in0=o2[:], scalar=1.0, in1=o2[:], op0=ALU.mult, op1=ALU.mult, accum_out=stt[:, 4:5]),
                                  [o2], [jk, (stt, 4)])
                                I("dve", lambda e, stt=stt: e.tensor_scalar(out=stt[:, 6:7], in0=stt[:, 4:5], scalar1=1.0 / 128,
                                                                            scalar2=EPS, op0=ALU.mult, op1=ALU.add), [(stt, 4)], [(stt, 6)])

                            def s2():
                                for (stt, o2, on) in bufs:
                                    I("act", lambda e, stt=stt: e.activation(out=stt[:, 7:8], in_=stt[:, 6:7], func=AF.Ln), [(stt, 6)], [(stt, 7)])
                                    I("act", lambda e, stt=stt: e.activation(out=stt[:, 5:6], in_=stt[:, 7:8], func=AF.Exp, scale=-0.5),
                                      [(stt, 7)], [(stt, 5)])

                            def s3():
                                for (stt, o2, on) in bufs:
                                    I("dve", lambda e, o2=o2, on=on, stt=stt: e.tensor_scalar(
                                        out=on[:], in0=o2[:], scalar1=stt[:, 5:6], scalar2=None, op0=ALU.mult), [o2, (stt, 5)], [on])

                            def s4():
                                for ql, (stt, o2, on) in enumerate(bufs):
                                    I("pe", lambda e, on=on, ql=ql: e.transpose(out=psB[:, ql * 128:(ql + 1) * 128], in_=on[:],
                                                                                identity=ident_b[:]), [on, ident_b], [psB])

                            def s5():
                                qcol = 2 * G * 128
                                I("dve", lambda e: e.scalar_tensor_tensor(
                                    out=y[:, qcol:qcol + 256], in0=psB[:, 0:256], scalar=gcol[:, 0:1], in1=z[:, qcol:qcol + 256],
                                    op0=ALU.mult, op1=ALU.mult), [gcol, z], [psB, y])

                            defer(4, s2)
                            defer(6, s3)
                            defer(8, s4)
                            defer(10, s5)

                        LA = 2
                        for n0 in range(min(LA, len(steps))):
                            emit_qk(n0)
                        for n in range(len(steps)):
                            if n + LA < len(steps):
                                emit_qk(n + LA)
                            emit_rest(n)
                            gstep[0] += 1
                            run_due()
                            if n == min(14, len(steps) - 1) and it < NS * 16:
                                run_due(owner_lt=it)
                                issue_loads(it)
                            G, i = steps[n]
                            if i == 2 * G + 1:
                                emit_epilogue(G)
                        defer(12, lambda y=y, h=h, t0=t0: DMA(
                            "sp", y, lambda e: [e.dma_start(out=yT[h, :, t0:t0 + S], in_=y[:])], reads=[y]))
                run_due(force=True)
                sc.barrier()


        def gelu_evac(ps, w, dstt, tmps):
            t1, t2 = tmps
            I("act", lambda e: e.activation(out=t1[:, 0:w], in_=ps[:, 0:w], func=AF.Square), [], [ps, t1])
            I("pool", lambda e: e.tensor_scalar(out=t1[:, 0:w], in0=t1[:, 0:w], scalar1=0.044715, scalar2=1.0,
                                                op0=ALU.mult, op1=ALU.add), [t1], [t1])
            I("dve", lambda e: e.tensor_tensor(out=t2[:, 0:w], in0=ps[:, 0:w], in1=t1[:, 0:w], op=ALU.mult), [t1], [ps, t2])
            I("act", lambda e: e.activation(out=t1[:, 0:w], in_=t2[:, 0:w], func=AF.Sigmoid, scale=1.5957691216057308),
              [t2], [t1])
            I("dve", lambda e: e.tensor_tensor(out=dstt[:, 0:w], in0=ps[:, 0:w], in1=t1[:, 0:w], op=ALU.mult), [t1], [ps, dstt])

        def phase_even_inproj(ei):
            W = ev_w_in[ei]
            with ExitStack() as st:
                wtp = pool(st, "eiw", [128, 8, 1024], BF16, 2, False)
                wstage = sbt(st, "eiws", [128, 8, 1024], F32)
                hTp = pool(st, "eih", [128, 8, 512], BF16, 3)
                t1p = pool(st, "eit1", [128, 512], F32, 4, False)
                t2p = pool(st, "eit2", [128, 512], F32, 4, False)
                stg = pool(st, "eist", [128, 512], BF16, 4)
                stgf = pool(st, "eisf", [128, 8], F32, 3)
                cnt = [0]
                groups = [(0, 1024, "tm", "gelu", u_s), (1024, 1024, "tm", "gelu", va_s), (2048, 1024, "fm", "silu", zaT_s),
                          (3072, 1024, "fm", "copy", xmT_s), (4096, 1024, "tm", "sigm", og_s), (5128, 1024, "fm", "silu", zbT_s),
                          (5120, 8, "tm", "gate", gt_s)]
                load_w_stage(W, groups[0][0], groups[0][1], wstage)
                cast_w(wtp[0], groups[0][1], wstage)
                for gi, (c0, ncols, mode, kind, dst) in enumerate(groups):
                    wt = wtp[gi % 2]
                    mid = None
                    if gi + 1 < len(groups):
                        nc0, nnc = groups[gi + 1][0], groups[gi + 1][1]
                        load_w_stage(W, nc0, nnc, wstage)
                        mid = (lambda gi=gi, nnc=nnc: cast_w(wtp[(gi + 1) % 2], nnc, wstage))
                    if mode == "fm":
                        units = [(j * 128,) for j in range(8)]

                        def evac(ui, c, pss, kind=kind, dst=dst):
                            i = cnt[0]
                            cnt[0] += 1
                            sg = stg[i % 4]
                            fn = AF.Silu if kind == "silu" else AF.Copy
                            I("act", lambda e: e.activation(out=sg[:], in_=pss[0][:], func=fn), [], [pss[0], sg])
                            DMA("sp", sg, lambda e: [e.dma_start(out=dst[ui, :, c * 512:(c + 1) * 512], in_=sg[:])], reads=[sg])
                        proj_fm(wt, units, evac, hTp, mid)
                    else:
                        def evac(tt, half, ps, w, kind=kind, dst=dst):
                            i = cnt[0]
                            cnt[0] += 1
                            if kind == "gate":
                                sf = stgf[i % 3]
                                I("act", lambda e: e.activation(out=sf[:], in_=ps[:, 0:8], func=AF.Copy), [], [ps, sf])
                                DMA("sp", sf, lambda e: [e.dma_start(out=dst[tt * 128:(tt + 1) * 128, :], in_=sf[:])], reads=[sf])
                                return
                            sg = stg[i % 4]
                            if kind == "gelu":
                                gelu_evac(ps, w, sg, (t1p[i % 4], t2p[i % 4]))
                            else:
                                I("act", lambda e: e.activation(out=sg[:], in_=ps[:], func=AF.Sigmoid), [], [ps, sg])
                            DMA("sp", sg, lambda e: [e.dma_start(out=dst[tt * 128:(tt + 1) * 128, half * 512:(half + 1) * 512],
                                                                 in_=sg[:])], reads=[sg])
                        proj_tm(wt, ncols, evac, hTp, mid)
                sc.barrier()

        def phase_even_mix(ei):
            with ExitStack() as st:
                alg = sbt(st, "alg", [128, D], F32)
                wsf = sbt(st, "wsf", [128, 8, 128], F32)
                wsb = sbt(st, "wsb", [128, 8, 128], BF16, False)
                bsT = sbt(st, "bsT", [128, 8], F32)
                cw = sbt(st, "cw", [128, 8, 4], F32)
                cv = sbt(st, "cv", [128, 3, 8], F32)
                gb = sbt(st, "gb", [128, 8], F32)
                Wb = sbt(st, "Wb", [128, 3, 8, 256], BF16, False)
                GN = sbt(st, "GN", [128, 8, 128], F32, False)
                SK = sbt(st, "SK", [128, 8, 128], F32, False)
                tri4 = sbt(st, "tri4", [128, 4, 128], F32, False)
                DMA("sp", alg, lambda e: [e.dma_start(out=alg[:], in_=a_ln_g[ei])], writes=[alg])
                DMA("sp", wsf, lambda e: [e.dma_start(out=wsf[:], in_=a_wsT[ei].rearrange("g s t -> s g t"))], writes=[wsf])
                DMA("sp", bsT, lambda e: [e.dma_start(out=bsT[:], in_=a_bsT[ei])], writes=[bsT])
                DMA("sp", cw, lambda e: [e.dma_start(out=cw[:], in_=convw[ei])], writes=[cw])
                DMA("sp", cv, lambda e: [e.dma_start(out=cv[:], in_=colv[ei])], writes=[cv])
                DMA("sp", gb, lambda e: [e.dma_start(out=gb[:], in_=gate_b[ei])], writes=[gb])
                Wbs = sbt(st, "Wbs", [128, 8, 256], F32)
                for j in range(3):
                    DMA("sp", Wbs, lambda e, j=j: [e.dma_start(out=Wbs[:], in_=b_wqkv[ei, j].rearrange("h (dc p) e -> p (h dc) e", p=128))],
                        writes=[Wbs])
                    I("act", lambda e, j=j: e.activation(out=Wb[:, j, :, :], in_=Wbs[:], func=AF.Copy), [Wbs], [(Wb, j)])
                Wb_keys = [(Wb, j) for j in range(3)]
                for g in range(8):
                    I("dve", lambda e, g=g: e.tensor_tensor(out=wsb[:, g, :], in0=wsf[:, g, :], in1=tri_f[:], op=ALU.mult),
                      [wsf, tri_f], [(wsb, g)])
                    I("dve", lambda e, g=g: e.tensor_scalar(out=GN[:, g, :], in0=ones_f[:], scalar1=cv[:, 1, g:g + 1], scalar2=None,
                                                            op0=ALU.mult), [ones_f, cv], [(GN, g)])
                    I("dve", lambda e, g=g: e.tensor_scalar(out=SK[:, g, :], in0=ones_f[:], scalar1=cv[:, 2, g:g + 1], scalar2=None,
                                                            op0=ALU.mult), [ones_f, cv], [(SK, g)])
                for h in range(4):
                    I("dve", lambda e, h=h: e.tensor_copy(out=tri4[:, h, :], in_=tri_f[:]), [tri_f], [(tri4, h)])
                dgw = sbt(st, "dgw", [128, 8, 4, 128], BF16, False)
                for j in range(8):
                    for k in range(4):
                        I("dve", lambda e, j=j, k=k: e.tensor_scalar(out=dgw[:, j, k, :], in0=ident_f[:], scalar1=cw[:, j, k:k + 1],
                                                                      scalar2=None, op0=ALU.mult), [ident_f, cw], [(dgw, j)])
                wsb_keys = [(wsb, g) for g in range(8)]
                GN_keys = [(GN, g) for g in range(8)]
                SK_keys = [(SK, g) for g in range(8)]
                tri4_keys = [(tri4, h) for h in range(4)]

                tmps = {}

                def xc_keys(xc):
                    return [(xc, j) for j in range(8)]

                def tmp(name, shape, dt, n=2):
                    if name not in tmps:
                        tmps[name] = [pool(st, "t" + name, shape, dt, n, False), 0]
                    p = tmps[name]
                    t = p[0][p[1] % n]
                    p[1] += 1
                    return t

                Cst = sbt(st, "Cst", [128, 4, 2, 260], F32, False)
                Cb = sbt(st, "Cb", [128, 4, 2, 260], BF16, False)

                up = pool(st, "mu", [128, D], BF16, 2)
                vap = pool(st, "mva", [128, D], BF16, 2)
                ogp = pool(st, "mog", [128, D], BF16, 2)
                gtp = pool(st, "mgt", [128, 8], F32, 2)
                zap = pool(st, "mza", [128, 8, 128], BF16, 2)
                zbp = pool(st, "mzb", [128, 8, 128], BF16, 2)
                xmp = pool(st, "mxm", [128, 8, 131], BF16, 2)
                yap = pool(st, "mya", [128, 8, 128], BF16, 2)
                ybp = pool(st, "myb", [128, 8, 128], BF16, 2)
                def tile_loads(tt):
                    c = tt % CPS
                    tok0 = tt * 128
                    b2 = tt % 2
                    ut, vat, ogt, gtt, zat, zbt, xmt = up[b2], vap[b2], ogp[b2], gtp[b2], zap[b2], zbp[b2], xmp[b2]
                    rows = slice(tok0, tok0 + 128)
                    DMA("sp", ut, lambda e, ut=ut, rows=rows: [e.dma_start(out=ut[:], in_=u_s[rows, :])], writes=[ut])
                    DMA("sp", vat, lambda e, vat=vat, rows=rows: [e.dma_start(out=vat[:], in_=va_s[rows, :])], writes=[vat])
                    DMA("sp", ogt, lambda e, ogt=ogt, rows=rows: [e.dma_start(out=ogt[:], in_=og_s[rows, :])], writes=[ogt])
                    DMA("sp", gtt, lambda e, gtt=gtt, rows=rows: [e.dma_start(out=gtt[:], in_=gt_s[rows, :])], writes=[gtt])
                    DMA("sp", zat, lambda e, zat=zat, rows=rows: [e.dma_start(
                        out=zat[:], in_=zaT_s[:, :, rows].rearrange("k p t -> p k t"))], writes=[zat])
                    DMA("sp", zbt, lambda e, zbt=zbt, rows=rows: [e.dma_start(
                        out=zbt[:], in_=zbT_s[:, :, rows].rearrange("k p t -> p k t"))], writes=[zbt])
                    if c == 0:
                        I("dve", lambda e, xmt=xmt: e.memset(xmt[:, :, 0:3], 0.0), [], [xmt])
                        DMA("sp", xmt, lambda e, xmt=xmt, rows=rows: [e.dma_start(
                            out=xmt[:, :, 3:131], in_=xmT_s[:, :, rows].rearrange("k p t -> p k t"))], writes=[xmt])
                    else:
                        DMA("sp", xmt, lambda e, xmt=xmt, tok0=tok0: [e.dma_start(
                            out=xmt[:], in_=xmT_s[:, :, tok0 - 3:tok0 + 128].rearrange("k p t -> p k t"))], writes=[xmt])


                def do_tile(tt):
                    c = tt % CPS
                    tok0 = tt * 128
                    b2 = tt % 2
                    ut, vat, ogt, gtt, zat, zbt, xmt = up[b2], vap[b2], ogp[b2], gtp[b2], zap[b2], zbp[b2], xmp[b2]
                    rows = slice(tok0, tok0 + 128)
                    if tt + 1 < NT:
                        tile_loads(tt + 1)
                    bst = tmp("bst", [128, 2, 6], F32)
                    mv = tmp("mv", [128, 8], F32)
                    for hf in range(2):
                        I("dve", lambda e, hf=hf, bst=bst, vat=vat: e.bn_stats(out=bst[:, hf, :], in_=vat[:, hf * 512:(hf + 1) * 512]),
                          [vat], [(bst, hf)])
                    I("dve", lambda e, bst=bst, mv=mv: e.bn_aggr(out=mv[:, 0:2], in_=bst[:].rearrange("p a b -> p (a b)")),
                      [(bst, 0), (bst, 1)], [(mv, 0), (mv, 1)])
                    rstd_chain(mv, 1, 1.0, 2)
                    vn = tmp("vn", [128, D], F32, 1)
                    vnb = tmp("vnb", [128, D], BF16)
                    I("dve", lambda e, vn=vn, vat=vat, mv=mv: e.tensor_scalar(out=vn[:], in0=vat[:], scalar1=mv[:, 0:1], scalar2=mv[:, 2:3],
                                                                              op0=ALU.subtract, op1=ALU.mult), [vat, (mv, 0), (mv, 2)], [vn])
                    I("pool", lambda e, vn=vn, vnb=vnb: e.tensor_tensor(out=vnb[:], in0=vn[:], in1=alg[:], op=ALU.mult), [vn, alg], [vnb])
                    psA = [ps_next(), ps_next()]
                    for g in range(8):
                        pg = psA[g // 4]
                        I("pe", lambda e, g=g, pg=pg, vnb=vnb: e.matmul(pg[:, (g % 4) * 128:(g % 4 + 1) * 128], lhsT=wsb[:, g, :],
                                                                        rhs=vnb[:, g * 128:(g + 1) * 128], start=True, stop=True),
                          [(wsb, g), vnb], [pg])
                    ya = tmp("ya", [128, D], BF16)
                    for g in range(8):
                        pg = psA[g // 4]
                        I("dve", lambda e, g=g, pg=pg, ya=ya, ut=ut: e.scalar_tensor_tensor(
                            out=ya[:, g * 128:(g + 1) * 128], in0=pg[:, (g % 4) * 128:(g % 4 + 1) * 128], scalar=bsT[:, g:g + 1],
                            in1=ut[:, g * 128:(g + 1) * 128], op0=ALU.add, op1=ALU.mult), [bsT, ut], [pg, (ya, g)])
                    for g in range(8):
                        I("pe", lambda e, g=g, ya=ya: e.transpose(out=psB[:, g * 128:(g + 1) * 128], in_=ya[:, g * 128:(g + 1) * 128],
                                                                  identity=ident_b[:]), [(ya, g), ident_b], [psB])
                    yat = yap[b2]
                    I("dve", lambda e, yat=yat, zat=zat: e.tensor_tensor(out=yat[:], in0=psB[:].rearrange("p (k t) -> p k t", k=8),
                                                                         in1=zat[:], op=ALU.mult), [zat], [psB, yat])
                    DMA("sp", yat, lambda e, yat=yat, rows=rows: [e.dma_start(out=yT[0:8, :, rows].rearrange("k p t -> p k t"), in_=yat[:])],
                        reads=[yat])

                    xc = tmp("xc", [128, 8, 128], BF16)
                    psc = [ps_next(), ps_next()]
                    for j in range(8):
                        pg = psc[j // 4]
                        for k in range(4):
                            I("pe", lambda e, j=j, k=k, pg=pg: e.matmul(
                                pg[:, (j % 4) * 128:(j % 4 + 1) * 128], lhsT=dgw[:, j, k, :], rhs=xmt[:, j, k:k + 128],
                                start=(k == 0), stop=(k == 3)), [(dgw, j), xmt], [pg])
                    for j in range(8):
                        pg = psc[j // 4]
                        I("act", lambda e, j=j, pg=pg: e.activation(
                            out=xc[:, j, :], in_=pg[:, (j % 4) * 128:(j % 4 + 1) * 128], func=AF.Silu, bias=cv[:, 0, j:j + 1]),
                          [cv], [pg, (xc, j)])
                    qb_ = tmp("qb", [128, 4, 2, 128], BF16)
                    kb_ = tmp("kb", [128, 4, 2, 128], BF16)
                    qs_ = tmp("qs", [128, 4, 2, 128], BF16)
                    psq = [ps_next(), ps_next()]
                    for (wi, pss, dstb) in ((0, psq, qb_), (1, [ps_next(), ps_next()], kb_)):
                        for h in range(4):
                            pg = pss[h // 2]
                            for ec in range(2):
                                o0 = (h % 2) * 256 + ec * 128
                                for dc in range(2):
                                    I("pe", lambda e, wi=wi, pg=pg, h=h, ec=ec, dc=dc, o0=o0, xc=xc: e.matmul(
                                        pg[:, o0:o0 + 128], lhsT=Wb[:, wi, h * 2 + dc, ec * 128:(ec + 1) * 128], rhs=xc[:, h * 2 + dc, :],
                                        start=(dc == 0), stop=(dc == 1)), Wb_keys + xc_keys(xc), [pg])
                        for bk in range(2):
                            I("act", lambda e, pss=pss, bk=bk, dstb=dstb, wi=wi: e.activation(
                                out=dstb[:, bk * 2:bk * 2 + 2, :, :], in_=pss[bk][:].rearrange("p (h c t) -> p h c t", h=2, c=2),
                                func=AF.Copy, scale=(1.0 if wi == 0 else 0.0625)), [], [pss[bk], (dstb, bk)])
                    psk = [ps_next(), ps_next()]
                    for h in range(4):
                        for dc in range(2):
                            I("pe", lambda e, h=h, dc=dc, xc=xc: e.matmul(
                                psk[h // 2][:, (h % 2) * 256:(h % 2 + 1) * 256], lhsT=xc[:, h * 2 + dc, :], rhs=Wb[:, 1, h * 2 + dc, :],
                                start=(dc == 0), stop=(dc == 1)), Wb_keys + xc_keys(xc), [psk[h // 2]])
                    ktm = tmp("ktm", [128, 4, 256], BF16)
                    for bk in range(2):
                        I("act", lambda e, bk=bk, ktm=ktm: e.activation(
                            out=ktm[:, bk * 2:bk * 2 + 2, :], in_=psk[bk][:].rearrange("p (h e) -> p h e", h=2), func=AF.Copy,
                            scale=0.0625), [], [psk[bk], (ktm, bk)])
                    va_ = tmp("vaug", [128, 4, 260], BF16)
                    I("pool", lambda e, va_=va_: e.memset(va_[:, :, 256:257], 1.0), [], [(va_, "o")])
                    for bk in range(2):
                        psv = ps_next()
                        for hh in range(2):
                            h = bk * 2 + hh
                            for dc in range(2):
                                I("pe", lambda e, h=h, hh=hh, dc=dc, psv=psv, xmt=xmt: e.matmul(
                                    psv[:, hh * 256:(hh + 1) * 256], lhsT=xmt[:, h * 2 + dc, 3:131], rhs=Wb[:, 2, h * 2 + dc, :],
                                    start=(dc == 0), stop=(dc == 1)), Wb_keys + [xmt], [psv])
                        I("act", lambda e, psv=psv, bk=bk, va_=va_: e.activation(
                            out=va_[:, bk * 2:bk * 2 + 2, 0:256], in_=psv[:].rearrange("p (h e) -> p h e", h=2), func=AF.Copy),
                          [], [psv, (va_, bk)])
                    gs = tmp("gs", [128, 40], F32)
                    I("dve", lambda e, gs=gs, gtt=gtt: e.tensor_tensor(out=gs[:, 0:8], in0=gtt[:], in1=gb[:], op=ALU.add), [gtt, gb], [(gs, 0)])
                    I("act", lambda e, gs=gs: e.activation(out=gs[:, 8:12], in_=gs[:, 4:8], func=AF.Exp, scale=-1.0), [(gs, 0)], [(gs, 1)])
                    I("dve", lambda e, gs=gs: e.tensor_scalar(out=gs[:, 12:16], in0=gs[:, 8:12], scalar1=1.0, scalar2=None, op0=ALU.add),
                      [(gs, 1)], [(gs, 2)])
                    I("act", lambda e, gs=gs: e.activation(out=gs[:, 16:20], in_=gs[:, 12:16], func=AF.Ln), [(gs, 2)], [(gs, 3)])
                    psg = ps_next()
                    I("pe", lambda e, gs=gs, psg=psg: e.matmul(psg[:, 0:4], lhsT=tri_f[:], rhs=gs[:, 16:20], start=True, stop=True),
                      [tri_f, (gs, 3)], [psg])
                    I("dve", lambda e, gs=gs, psg=psg: e.tensor_copy(out=gs[:, 20:24], in_=psg[:, 0:4]), [], [psg, (gs, 4)])
                    I("dve", lambda e, gs=gs: e.tensor_tensor(out=gs[:, 24:28], in0=gs[:, 0:4], in1=gs[:, 20:24], op=ALU.add),
                      [(gs, 0), (gs, 4)], [(gs, 5)])
                    dg = tmp("dg", [128, 4, 128], F32, 1)
                    for h in range(4):
                        I("dve", lambda e, h=h, dg=dg, gs=gs: e.tensor_scalar(out=dg[:, h, :], in0=ident_f[:], scalar1=gs[:, 20 + h:21 + h],
                                                                              scalar2=-1.0, op0=ALU.mult, op1=ALU.mult),
                          [ident_f, (gs, 4)], [(dg, h)])
                    psr = ps_next()
                    I("pe", lambda e, psr=psr, dg=dg: e.matmul(psr[:], lhsT=ones_f[:], rhs=dg[:].rearrange("p h t -> p (h t)"),
                                                               start=True, stop=True), [ones_f] + [(dg, h) for h in range(4)], [psr])
                    PT = tmp("PT", [128, 4, 128], F32, 1)
                    for h in range(4):
                        I("act", lambda e, h=h, PT=PT, psr=psr, gs=gs: e.activation(
                            out=PT[:, h, :], in_=psr[:, h * 128:(h + 1) * 128], func=AF.Exp, bias=gs[:, 24 + h:25 + h]),
                          [(gs, 5)], [psr, (PT, h)])
                    EB = tmp("EB", [128, 4, 128], F32, 1)
                    I("act", lambda e, EB=EB, psr=psr: e.activation(out=EB[:].rearrange("p h t -> p (h t)"), in_=psr[:], func=AF.Exp),
                      [], [psr, EB])
                    I("dve", lambda e, gs=gs, psr=psr: e.tensor_copy(
                        out=gs[:, 28:32], in_=psr[:].rearrange("p (h t) -> p h t", h=4)[:, :, 127]), [], [psr, (gs, 6)])
                    I("pool", lambda e, PT=PT: e.tensor_tensor(out=PT[:], in0=PT[:], in1=tri4[:], op=ALU.mult),
                      tri4_keys + [(PT, h) for h in range(4)], [(PT, h) for h in range(4)])
                    if c > 0:
                        for bk in range(2):
                            for ec in range(2):
                                I("dve", lambda e, bk=bk, ec=ec, qs_=qs_, EB=EB, qb_=qb_: e.tensor_tensor(
                                    out=qs_[:, bk * 2:bk * 2 + 2, ec, :], in0=qb_[:, bk * 2:bk * 2 + 2, ec, :],
                                    in1=EB[:, bk * 2:bk * 2 + 2, :], op=ALU.mult), [EB, (qb_, bk)], [(qs_, bk, ec)])
                    psqk = ps_next()
                    for h in range(4):
                        for ec in range(2):
                            I("pe", lambda e, h=h, ec=ec, psqk=psqk, kb_=kb_, qb_=qb_: e.matmul(
                                psqk[:, h * 128:(h + 1) * 128], lhsT=kb_[:, h, ec, :], rhs=qb_[:, h, ec, :],
                                start=(ec == 0), stop=(ec == 1)), [(kb_, 0), (kb_, 1), (qb_, 0), (qb_, 1)], [psqk])
                    scT = tmp("scT", [128, 4, 128], BF16)
                    I("dve", lambda e, scT=scT, psqk=psqk, PT=PT: e.tensor_tensor(
                        out=scT[:].rearrange("p h t -> p (h t)"), in0=psqk[:], in1=PT[:].rearrange("p h t -> p (h t)"), op=ALU.mult),
                      [(PT, h) for h in range(4)], [psqk, scT])
                    htn = tmp("htn", [128, D], BF16)
                    for h in range(4):
                        acc = ps_next()
                        I("pe", lambda e, h=h, acc=acc, scT=scT, va_=va_: e.matmul(
                            acc[:, 0:257], lhsT=scT[:, h, :], rhs=va_[:, h, 0:257], start=True, stop=(c == 0)),
                          [scT, (va_, "o"), (va_, h // 2)], [acc])
                        if c > 0:
                            for dc in range(2):
                                I("pe", lambda e, h=h, dc=dc, acc=acc, qs_=qs_: e.matmul(
                                    acc[:, 0:257], lhsT=qs_[:, h, dc, :], rhs=Cb[:, h, dc, 0:257], start=False, stop=(dc == 1)),
                                  [(qs_, h // 2, dc), (Cb, h, dc)], [acc])
                        hs = tmp("hs", [128, 16], F32)
                        ht = tmp("ht", [128, 256], F32)
                        I("act", lambda e, hs=hs, acc=acc: e.activation(out=hs[:, 8:9], in_=acc[:, 256:257], func=AF.Abs),
                          [], [acc, (hs, 8)])
                        I("dve", lambda e, hs=hs: e.tensor_scalar(out=hs[:, 10:11], in0=hs[:, 8:9], scalar1=1.0, scalar2=None,
                                                                  op0=ALU.max), [(hs, 8)], [(hs, 10)])
                        I("dve", lambda e, hs=hs: e.reciprocal(out=hs[:, 9:10], in_=hs[:, 10:11]), [(hs, 10)], [(hs, 9)])
                        I("dve", lambda e, hs=hs, acc=acc, ht=ht, h=h, ogt=ogt: e.scalar_tensor_tensor(
                            out=ht[:], in0=acc[:, 0:256], scalar=hs[:, 9:10], in1=ogt[:, h * 256:(h + 1) * 256],
                            op0=ALU.mult, op1=ALU.mult), [(hs, 9), ogt], [acc, ht])
                        bs2 = tmp("bs2", [128, 6], F32)
                        I("dve", lambda e, bs2=bs2, ht=ht: e.bn_stats(out=bs2[:], in_=ht[:]), [ht], [bs2])
                        I("dve", lambda e, bs2=bs2, hs=hs: e.bn_aggr(out=hs[:, 0:2], in_=bs2[:]), [bs2], [(hs, 0), (hs, 1)])
                        rstd_chain(hs, 1, 1.0, 2)
                        I("dve", lambda e, hs=hs, ht=ht, htn=htn, h=h: e.tensor_scalar(
                            out=htn[:, h * 256:(h + 1) * 256], in0=ht[:], scalar1=hs[:, 0:1], scalar2=hs[:, 2:3],
                            op0=ALU.subtract, op1=ALU.mult), [ht, (hs, 0), (hs, 2)], [(htn, h)])
                    for g in range(8):
                        I("pe", lambda e, g=g, htn=htn: e.transpose(out=psB[:, g * 128:(g + 1) * 128], in_=htn[:, g * 128:(g + 1) * 128],
                                                                    identity=ident_b[:]), [(htn, g // 2), ident_b], [psB])
                    y1 = tmp("y1", [128, 8, 128], F32, 1)
                    y2 = tmp("y2", [128, 8, 128], F32, 1)
                    I("dve", lambda e, y1=y1: e.tensor_tensor(out=y1[:], in0=psB[:].rearrange("p (k t) -> p k t", k=8), in1=GN[:],
                                                              op=ALU.mult), GN_keys, [psB, y1])
                    I("pool", lambda e, y2=y2, xc=xc: e.tensor_tensor(out=y2[:], in0=xc[:], in1=SK[:], op=ALU.mult), SK_keys + xc_keys(xc), [y2])
                    I("dve", lambda e, y1=y1, y2=y2: e.tensor_tensor(out=y1[:], in0=y1[:], in1=y2[:], op=ALU.add), [y1, y2], [y1])
                    ybt = ybp[b2]
                    I("dve", lambda e, y1=y1, ybt=ybt, zbt=zbt: e.tensor_tensor(out=ybt[:], in0=y1[:], in1=zbt[:], op=ALU.mult),
                      [y1, zbt], [ybt])
                    DMA("sp", ybt, lambda e, ybt=ybt, rows=rows: [e.dma_start(out=yT[8:16, :, rows].rearrange("k p t -> p k t"), in_=ybt[:])],
                        reads=[ybt])
                    if c < CPS - 1:
                        I("dve", lambda e, gs=gs: e.tensor_tensor(out=gs[:, 32:36], in0=gs[:, 24:28], in1=gs[:, 28:32], op=ALU.add),
                          [(gs, 5), (gs, 6)], [(gs, 7)])
                        I("act", lambda e, gs=gs: e.activation(out=gs[:, 32:36], in_=gs[:, 32:36], func=AF.Exp), [(gs, 7)], [(gs, 7)])
                        I("act", lambda e, gs=gs: e.activation(out=gs[:, 36:40], in_=gs[:, 28:32], func=AF.Exp), [(gs, 6)], [(gs, 8)])
                        kw = tmp("kw", [128, 4, 256], BF16)
                        for h in range(4):
                            I("dve", lambda e, h=h, kw=kw, gs=gs, ktm=ktm: e.tensor_scalar(
                                out=kw[:, h, :], in0=ktm[:, h, :], scalar1=gs[:, 32 + h:33 + h],
                                scalar2=None, op0=ALU.mult), [(gs, 7), (ktm, h // 2)], [(kw, h)])
                        for h in range(4):
                            for dc in range(2):
                                psu = ps_next()
                                I("pe", lambda e, h=h, dc=dc, psu=psu, kw=kw, va_=va_: e.matmul(
                                    psu[:, 0:257], lhsT=kw[:, h, dc * 128:(dc + 1) * 128], rhs=va_[:, h, 0:257], start=True, stop=True),
                                  [(kw, h), (va_, "o"), (va_, h // 2)], [psu])
                                if c == 0:
                                    I("dve", lambda e, h=h, dc=dc, psu=psu: e.tensor_copy(out=Cst[:, h, dc, 0:257], in_=psu[:, 0:257]),
                                      [], [psu, (Cst, h, dc)])
                                else:
                                    I("dve", lambda e, h=h, dc=dc, psu=psu, gs=gs: e.scalar_tensor_tensor(
                                        out=Cst[:, h, dc, 0:257], in0=Cst[:, h, dc, 0:257], scalar=gs[:, 36 + h:37 + h], in1=psu[:, 0:257],
                                        op0=ALU.mult, op1=ALU.add), [(gs, 8)], [psu, (Cst, h, dc)])
                                I("act", lambda e, h=h, dc=dc: e.activation(out=Cb[:, h, dc, 0:257], in_=Cst[:, h, dc, 0:257], func=AF.Copy),
                                  [(Cst, h, dc)], [(Cb, h, dc)])
                tile_loads(0)
                for tt in range(NT):
                    do_tile(tt)
                sc.barrier()

        import os as _os
        STOP = _os.environ.get("KSTOP", "")

        def phase_copyout():
            with ExitStack() as st:
                xp = pool(st, "cox", [128, D], F32, 2)
                for tt in range(NT):
                    xt = xp[tt % 2]
                    DMA("sp", xt, lambda e, xt=xt, tt=tt: [e.dma_start(out=xt[:], in_=xs[tt * 128:(tt + 1) * 128, :])], writes=[xt])
                    DMA("sp", xt, lambda e, xt=xt, tt=tt: [e.dma_start(out=out[tt * 128:(tt + 1) * 128, :], in_=xt[:])], reads=[xt])
                sc.barrier()

        def run_all():
            phase_norm0(layers[0][2])
            if STOP == "norm0":
                return phase_copyout()
            for li, (kind, pi, lno) in enumerate(layers):
                if kind == "odd":
                    phase_odd_inproj(pi)
                    if STOP == "inproj":
                        return phase_copyout()
                    phase_attn(pi, lno)
                    if STOP == "attn":
                        return phase_copyout()
                    phase_out(li, od_w_out[pi])
                else:
                    phase_even_inproj(pi)
                    if STOP == "inproj":
                        return phase_copyout()
                    phase_even_mix(pi)
                    if STOP == "attn":
                        return phase_copyout()
                    phase_out(li, ev_w_out[pi])

        run_all()
        sc.barrier(["sp"])
        sc.emit()
    return nc


def prep_inputs(inputs, S):
    f = lambda a: np.ascontiguousarray(np.asarray(a, dtype=np.float32))
    rep = lambda a: np.ascontiguousarray(np.broadcast_to(f(a)[None], (128,) + tuple(np.shape(a))))
    shared = {}
    shared["gvec"] = rep(np.concatenate([f(inputs["norm_g"]), f(inputs["final_g"])[None]], axis=0))
    shared["ident"] = np.eye(128, dtype=np.float32)
    shared["tri"] = np.triu(np.ones((128, 128), np.float32))
    cosT, sinT = rope_tables_T(S)
    shared["cosT"], shared["sinT"] = cosT, sinT
    shared["ev_w_in"] = f(inputs["ev_w_in"])
    shared["ev_w_out"] = f(inputs["ev_w_out"])
    shared["od_w_in"] = f(inputs["od_w_in"])
    pm = np.zeros((128, 128), np.float32)
    pm[np.arange(128), np.arange(128) ^ 32] = 1.0
    shared["perm"] = pm
    shared["od_w_out"] = f(inputs["od_w_out"])
    shared["a_ln_g"] = np.ascontiguousarray(np.broadcast_to(f(inputs["a_ln_g"])[:, None, :], (2, 128, D)))
    shared["a_wsT"] = np.ascontiguousarray(np.swapaxes(f(inputs["a_ws"]), 2, 3))
    shared["a_bsT"] = np.ascontiguousarray(np.swapaxes(f(inputs["a_bs"]), 1, 2))
    cw = f(inputs["b_conv_w"])
    shared["convw"] = np.ascontiguousarray(cw.reshape(2, 4, 8, 128).transpose(0, 3, 2, 1))
    colv = np.stack([f(inputs["b_conv_b"]), f(inputs["b_gn_g"]), f(inputs["b_skip"])], axis=1)
    shared["colv"] = np.ascontiguousarray(colv.reshape(2, 3, 8, 128).transpose(0, 3, 1, 2))
    shared["b_wqkv"] = np.ascontiguousarray(np.stack([f(inputs["b_wq"]), f(inputs["b_wk"]), f(inputs["b_wv"])], axis=1))
    gb = np.concatenate([f(inputs["b_ig_b"]), f(inputs["b_fg_b"])], axis=1)
    shared["gate_b"] = np.ascontiguousarray(np.broadcast_to(gb[:, None, :], (2, 128, 8)))
    lv = np.stack([f(inputs["c_lam_q1"]), f(inputs["c_lam_k1"]), f(inputs["c_lam_q2"]), f(inputs["c_lam_k2"])], axis=1)
    shared["lamv"] = np.ascontiguousarray(np.broadcast_to(lv[:, None], (2, 128, 4, 64)))
    shared["subln"] = np.ascontiguousarray(f(inputs["c_subln_g"]).reshape(2, 128, 1))
    return shared


_PROG_CACHE = {}


def kernel(**inputs):
    x = np.ascontiguousarray(inputs["x"], dtype=np.float32)
    B, S, _ = x.shape
    NS = B // NCORES
    key = (NS, S)
    if key not in _PROG_CACHE:
        _PROG_CACHE[key] = build_program(NS, S)
    nc = _PROG_CACHE[key]
    shared = prep_inputs(inputs, S)
    in_maps = []
    for c in range(NCORES):
        m = dict(shared)
        m["x"] = x[c * NS:(c + 1) * NS].reshape(NS * S, D)
        in_maps.append(m)
    res = run_bass_kernel_spmd(nc, in_maps, core_ids=list(range(NCORES)))
    outs = [r["out"].reshape(NS, S, D) for r in res.results]
    return np.concatenate(outs, axis=0)
```
